# Optimizing a Trainium2 kernel written in Bass

```python
import jax, jax.numpy as jnp
from jax import lax
import numpy as np

D_MODEL = 1024
BATCH = 16
SEQ = 2048
DEPTH = 2

EPS = 1e-6
D_FF = 2816
CONV_WIDTH = 4
D_RG = D_MODEL // 2
RG_BLOCKS = 8
RG_C = 8.0
ML_HEADS = 4
D_ML = D_MODEL // 2
ML_HEAD_DIM = D_ML // ML_HEADS
ML_CHUNK = 128
D_AB_IN = 2 * D_RG + 4 * D_ML + 2 * ML_HEADS
NSA_HEADS = 16
NSA_GROUPS = 4
NSA_HEAD_DIM = 64
NSA_KV = NSA_GROUPS * NSA_HEAD_DIM
NSA_WIDTH = NSA_HEADS * NSA_HEAD_DIM
CMP_BLOCK = 32
CMP_STRIDE = 16
CMP_HIDDEN = 256
SEL_BLOCK = 64
SEL_TOPN = 8
WINDOW = 256
NSA_QBLOCK = 64
NSA_BRANCHES = 3
D_NSA_IN = NSA_WIDTH + 6 * NSA_KV + NSA_BRANCHES * NSA_HEADS
FORCED_SCORE = 1e6
N_EVEN = (DEPTH + 1) // 2
N_ODD = DEPTH // 2

kernel_name = "hybrid_rglru_mlstm_nsa_macaron"


def rmsnorm(x, g):
    xf = x.astype(jnp.float32)
    y = xf * lax.rsqrt(jnp.mean(xf * xf, axis=-1, keepdims=True) + EPS)
    return (y * g.astype(jnp.float32)).astype(x.dtype)


def swiglu_ffn(x, w_gu, w_down):
    gate, up = jnp.split(x @ w_gu, 2, axis=-1)
    return (jax.nn.silu(gate) * up) @ w_down


def causal_dwconv(x, w, b):
    k, c = w.shape
    y = lax.conv_general_dilated(x, w[:, None, :], window_strides=(1,), padding=[(k - 1, 0)],
                                 dimension_numbers=('NWC', 'WIO', 'NWC'), feature_group_count=c)
    return y + b


def masked_softmax(s, mask):
    s = jnp.where(mask, s.astype(jnp.float32), -1e30)
    return jax.nn.softmax(s, axis=-1) * mask


def rglru(x, w_r, b_r, w_i, b_i, lam):
    bsz, s, c = x.shape
    xb = x.reshape(bsz, s, RG_BLOCKS, c // RG_BLOCKS)
    r = jax.nn.sigmoid(jnp.einsum('bsnc,ncd->bsnd', xb, w_r).reshape(bsz, s, c) + b_r)
    ig = jax.nn.sigmoid(jnp.einsum('bsnc,ncd->bsnd', xb, w_i).reshape(bsz, s, c) + b_i)
    log_a = -RG_C * r.astype(jnp.float32) * jax.nn.softplus(-lam.astype(jnp.float32))
    a = jnp.exp(log_a)
    u = jnp.sqrt(-jnp.expm1(2.0 * log_a)) * (ig * x).astype(jnp.float32)

    def combine(left, right):
        a1, b1 = left
        a2, b2 = right
        return a1 * a2, a2 * b1 + b2

    _, h = lax.associative_scan(combine, (a, u), axis=1)
    return h.astype(x.dtype)


def mlstm_chunkwise(q, k, v, log_i, log_f):
    bsz, nh, s, dh = q.shape
    nc = s // ML_CHUNK
    q = q.reshape(bsz, nh, nc, ML_CHUNK, dh) * (dh ** -0.5)
    k = k.reshape(bsz, nh, nc, ML_CHUNK, dh)
    v = v.reshape(bsz, nh, nc, ML_CHUNK, dh)
    log_i = log_i.reshape(bsz, nh, nc, ML_CHUNK)
    b = jnp.cumsum(log_f.reshape(bsz, nh, nc, ML_CHUNK), axis=-1)
    g = b[..., -1]
    w = g[..., None] - b + log_i
    m_loc = jnp.max(w, axis=-1)
    wk = jnp.exp(w - m_loc[..., None])
    c_loc = jnp.einsum('bhcl,bhclv,bhclk->bhcvk', wk, v, k)
    n_loc = jnp.einsum('bhcl,bhclk->bhck', wk, k)

    def step(carry, xs):
        c_st, n_st, m_st = carry
        c_l, n_l, m_l, g_c = xs
        m_new = jnp.maximum(g_c + m_st, m_l)
        sa = jnp.exp(g_c + m_st - m_new)
        sb = jnp.exp(m_l - m_new)
        new = (sa[..., None, None] * c_st + sb[..., None, None] * c_l,
               sa[..., None] * n_st + sb[..., None] * n_l, m_new)
        return new, carry

    init = (jnp.zeros((bsz, nh, dh, dh), jnp.float32), jnp.zeros((bsz, nh, dh), jnp.float32),
            jnp.full((bsz, nh), -1e30, jnp.float32))
    xs = (jnp.moveaxis(c_loc, 2, 0), jnp.moveaxis(n_loc, 2, 0),
          jnp.moveaxis(m_loc, 2, 0), jnp.moveaxis(g, 2, 0))
    _, (c0, n0, m0) = lax.scan(step, init, xs)
    c0 = jnp.moveaxis(c0, 0, 2)
    n0 = jnp.moveaxis(n0, 0, 2)
    m0 = jnp.moveaxis(m0, 0, 2)
    causal = jnp.tril(jnp.ones((ML_CHUNK, ML_CHUNK), bool))
    d = jnp.where(causal, b[..., :, None] - b[..., None, :] + log_i[..., None, :], -jnp.inf)
    m_inter = b + m0[..., None]
    m = jnp.maximum(m_inter, jnp.max(d, axis=-1))
    p = jnp.exp(d - m[..., None]) * jnp.einsum('bhcjd,bhckd->bhcjk', q, k)
    sc = jnp.exp(m_inter - m)
    num = sc[..., None] * jnp.einsum('bhcvk,bhcjk->bhcjv', c0, q) + jnp.einsum('bhcjk,bhckv->bhcjv', p, v)
    den = sc * jnp.einsum('bhck,bhcjk->bhcj', n0, q) + jnp.sum(p, axis=-1)
    h = num / jnp.maximum(jnp.abs(den), jnp.exp(-m))[..., None]
    return h.reshape(bsz, nh, s, dh)


def mlstm_block(qk, v, o, i_pre, f_pre, conv_w, conv_b, b_i, b_f, norm_g):
    bsz, s, _ = v.shape
    qk = jax.nn.silu(causal_dwconv(qk, conv_w, conv_b))
    q, k = jnp.split(qk, 2, axis=-1)
    heads = lambda t: t.reshape(bsz, s, ML_HEADS, ML_HEAD_DIM).transpose(0, 2, 1, 3).astype(jnp.float32)
    log_i = (i_pre + b_i).astype(jnp.float32).transpose(0, 2, 1)
    log_f = jax.nn.log_sigmoid((f_pre + b_f).astype(jnp.float32)).transpose(0, 2, 1)
    h = mlstm_chunkwise(heads(q), heads(k), heads(v), log_i, log_f)
    h = h * lax.rsqrt(jnp.mean(h * h, axis=-1, keepdims=True) + EPS)
    h = h.transpose(0, 2, 1, 3).reshape(bsz, s, D_ML) * norm_g.astype(jnp.float32)
    return (jax.nn.sigmoid(o.astype(jnp.float32)) * h).astype(v.dtype)


def ab_mixer(xn, w_in, rg_conv_w, rg_conv_b, rg_w_r, rg_b_r, rg_w_i, rg_b_i, rg_lambda,
             ml_conv_w, ml_conv_b, ml_b_i, ml_b_f, ml_norm, w_out):
    proj = xn @ w_in
    cuts = [D_RG, 2 * D_RG, 2 * D_RG + 2 * D_ML, 2 * D_RG + 3 * D_ML, 2 * D_RG + 4 * D_ML,
            2 * D_RG + 4 * D_ML + ML_HEADS]
    xa, ga, qk, v, o, i_pre, f_pre = jnp.split(proj, cuts, axis=-1)
    ya = jax.nn.gelu(ga) * rglru(causal_dwconv(xa, rg_conv_w, rg_conv_b),
                                 rg_w_r, rg_b_r, rg_w_i, rg_b_i, rg_lambda)
    yb = mlstm_block(qk, v, o, i_pre, f_pre, ml_conv_w, ml_conv_b, ml_b_i, ml_b_f, ml_norm)
    return jnp.concatenate([ya, yb], axis=-1) @ w_out


def compress_blocks(kv, pe, w1, b1, w2):
    bsz, s, g, dk = kv.shape
    nb = (s - CMP_BLOCK) // CMP_STRIDE + 1
    idx = np.arange(nb)[:, None] * CMP_STRIDE + np.arange(CMP_BLOCK)[None, :]
    blk = kv[:, idx] + pe[:, None, :]
    blk = blk.transpose(0, 1, 3, 2, 4).reshape(bsz, nb, g, CMP_BLOCK * dk)
    return jax.nn.gelu(blk @ w1 + b1) @ w2


def cmp_to_sel_matrix(n_cmp, n_sel):
    cs = np.arange(n_cmp)[:, None] * CMP_STRIDE
    ss = np.arange(n_sel)[None, :] * SEL_BLOCK
    ov = np.clip(np.minimum(cs + CMP_BLOCK, ss + SEL_BLOCK) - np.maximum(cs, ss), 0, None)
    return jnp.asarray(ov / CMP_BLOCK, dtype=jnp.float32)


def nsa_mixer(xn, w_in, pe_k, k_w1, k_b1, k_w2, pe_v, v_w1, v_b1, v_w2, b_gate, w_out):
    bsz, s, _ = xn.shape
    g, r, dk = NSA_GROUPS, NSA_HEADS // NSA_GROUPS, NSA_HEAD_DIM
    proj = xn @ w_in
    cuts = list(np.cumsum([NSA_WIDTH] + [NSA_KV] * 6))
    q, kc, vc, ks, vs, kw, vw, gates = jnp.split(proj, cuts, axis=-1)
    q = q.reshape(bsz, s, g, r, dk) * (dk ** -0.5)
    kvh = lambda t: t.reshape(bsz, s, g, dk)
    kc = compress_blocks(kvh(kc), pe_k, k_w1, k_b1, k_w2)
    vc = compress_blocks(kvh(vc), pe_v, v_w1, v_b1, v_w2)
    n_cmp = kc.shape[1]
    n_sel = s // SEL_BLOCK
    top_n = min(SEL_TOPN, n_sel)
    m_sel = cmp_to_sel_matrix(n_cmp, n_sel)
    cmp_end = jnp.arange(n_cmp) * CMP_STRIDE + CMP_BLOCK - 1
    sel_blk = jnp.arange(n_sel)
    ks = kvh(ks).transpose(0, 2, 1, 3)
    vs = kvh(vs).transpose(0, 2, 1, 3)
    pad = jnp.zeros((bsz, WINDOW, g, dk), xn.dtype)
    kw = jnp.concatenate([pad, kvh(kw)], axis=1)
    vw = jnp.concatenate([pad, kvh(vw)], axis=1)
    gates = jax.nn.sigmoid(gates.reshape(bsz, s, g, r, NSA_BRANCHES) + b_gate.reshape(g, r, NSA_BRANCHES))

    def query_block(s0):
        t = s0 + jnp.arange(NSA_QBLOCK)
        qb = lax.dynamic_slice_in_dim(q, s0, NSA_QBLOCK, axis=1)
        gb = lax.dynamic_slice_in_dim(gates, s0, NSA_QBLOCK, axis=1)
        sc = jnp.einsum('bqgrd,bngd->bgrqn', qb, kc)
        p_cmp = masked_softmax(sc, cmp_end[None, :] <= t[:, None])
        o_cmp = jnp.einsum('bgrqn,bngd->bqgrd', p_cmp.astype(vc.dtype), vc)
        score = jnp.einsum('bgqn,nj->bgqj', jnp.sum(p_cmp, axis=2), m_sel)
        cur = (t // SEL_BLOCK)[:, None]
        valid = sel_blk[None, :] * SEL_BLOCK <= t[:, None]
        forced = (sel_blk[None, :] == 0) | (sel_blk[None, :] == cur) | (sel_blk[None, :] == cur - 1)
        score = jnp.where(forced & valid, FORCED_SCORE, jnp.where(valid, score, -1.0))
        _, idx = lax.top_k(score, top_n)
        key_idx = (idx[..., None] * SEL_BLOCK + jnp.arange(SEL_BLOCK)).reshape(bsz, g, -1)
        n_keys = top_n * SEL_BLOCK
        k_sel = jnp.take_along_axis(ks, key_idx[..., None], axis=2).reshape(bsz, g, NSA_QBLOCK, n_keys, dk)
        v_sel = jnp.take_along_axis(vs, key_idx[..., None], axis=2).reshape(bsz, g, NSA_QBLOCK, n_keys, dk)
        ss = jnp.einsum('bqgrd,bgqkd->bgrqk', qb, k_sel)
        sel_mask = key_idx.reshape(bsz, g, 1, NSA_QBLOCK, n_keys) <= t[:, None]
        p_slc = masked_softmax(ss, sel_mask)
        o_slc = jnp.einsum('bgrqk,bgqkd->bqgrd', p_slc.astype(v_sel.dtype), v_sel)
        k_win = lax.dynamic_slice_in_dim(kw, s0, WINDOW + NSA_QBLOCK, axis=1)
        v_win = lax.dynamic_slice_in_dim(vw, s0, WINDOW + NSA_QBLOCK, axis=1)
        pos = s0 - WINDOW + jnp.arange(WINDOW + NSA_QBLOCK)
        win_mask = (pos[None, :] <= t[:, None]) & (pos[None, :] > t[:, None] - WINDOW) & (pos[None, :] >= 0)
        sw = jnp.einsum('bqgrd,bkgd->bgrqk', qb, k_win)
        p_win = masked_softmax(sw, win_mask)
        o_win = jnp.einsum('bgrqk,bkgd->bqgrd', p_win.astype(v_win.dtype), v_win)
        out = gb[..., 0:1] * o_cmp + gb[..., 1:2] * o_slc + gb[..., 2:3] * o_win
        return out.reshape(bsz, NSA_QBLOCK, NSA_WIDTH)

    starts = jnp.arange(s // NSA_QBLOCK) * NSA_QBLOCK
    out = lax.map(query_block, starts)
    out = out.transpose(1, 0, 2, 3).reshape(bsz, s, NSA_WIDTH)
    return out @ w_out


def setup_inputs(seed: int = 0) -> dict:
    key = jax.random.key(seed)
    keys = iter(jax.random.split(key, 40))
    nrm = lambda shape, scale: scale * jax.random.normal(next(keys), shape, jnp.float32)
    gain = lambda shape: 1.0 + 0.05 * jax.random.normal(next(keys), shape, jnp.float32)
    a8 = jax.random.uniform(next(keys), (N_EVEN, D_RG), jnp.float32, 0.9, 0.999)
    a0 = a8 ** (1.0 / RG_C)
    rg_lambda = jnp.log(a0) - jnp.log1p(-a0)
    ml_b_f = jnp.linspace(3.0, 6.0, ML_HEADS, dtype=jnp.float32)[None, :] + nrm((N_EVEN, ML_HEADS), 0.1)
    flat = CMP_BLOCK * NSA_HEAD_DIM
    return {
        "x": nrm((BATCH, SEQ, D_MODEL), 1.0),
        "ffn1_norm": gain((DEPTH, D_MODEL)),
        "ffn1_w_gu": nrm((DEPTH, D_MODEL, 2 * D_FF), D_MODEL ** -0.5),
        "ffn1_w_down": nrm((DEPTH, D_FF, D_MODEL), D_FF ** -0.5),
        "mix_norm": gain((DEPTH, D_MODEL)),
        "ffn2_norm": gain((DEPTH, D_MODEL)),
        "ffn2_w_gu": nrm((DEPTH, D_MODEL, 2 * D_FF), D_MODEL ** -0.5),
        "ffn2_w_down": nrm((DEPTH, D_FF, D_MODEL), D_FF ** -0.5),
        "ab_w_in": nrm((N_EVEN, D_MODEL, D_AB_IN), D_MODEL ** -0.5),
        "rg_conv_w": nrm((N_EVEN, CONV_WIDTH, D_RG), CONV_WIDTH ** -0.5),
        "rg_conv_b": nrm((N_EVEN, D_RG), 0.02),
        "rg_w_r": nrm((N_EVEN, RG_BLOCKS, D_RG // RG_BLOCKS, D_RG // RG_BLOCKS), (D_RG // RG_BLOCKS) ** -0.5),
        "rg_b_r": nrm((N_EVEN, D_RG), 0.02),
        "rg_w_i": nrm((N_EVEN, RG_BLOCKS, D_RG // RG_BLOCKS, D_RG // RG_BLOCKS), (D_RG // RG_BLOCKS) ** -0.5),
        "rg_b_i": nrm((N_EVEN, D_RG), 0.02),
        "rg_lambda": rg_lambda,
        "ml_conv_w": nrm((N_EVEN, CONV_WIDTH, 2 * D_ML), CONV_WIDTH ** -0.5),
        "ml_conv_b": nrm((N_EVEN, 2 * D_ML), 0.02),
        "ml_b_i": nrm((N_EVEN, ML_HEADS), 0.1),
        "ml_b_f": ml_b_f,
        "ml_norm": gain((N_EVEN, D_ML)),
        "ab_w_out": nrm((N_EVEN, D_RG + D_ML, D_MODEL), (D_RG + D_ML) ** -0.5),
        "nsa_w_in": nrm((N_ODD, D_MODEL, D_NSA_IN), D_MODEL ** -0.5),
        "nsa_pe_k": nrm((N_ODD, CMP_BLOCK, NSA_HEAD_DIM), 0.1),
        "nsa_k_w1": nrm((N_ODD, flat, CMP_HIDDEN), flat ** -0.5),
        "nsa_k_b1": nrm((N_ODD, CMP_HIDDEN), 0.02),
        "nsa_k_w2": nrm((N_ODD, CMP_HIDDEN, NSA_HEAD_DIM), CMP_HIDDEN ** -0.5),
        "nsa_pe_v": nrm((N_ODD, CMP_BLOCK, NSA_HEAD_DIM), 0.1),
        "nsa_v_w1": nrm((N_ODD, flat, CMP_HIDDEN), flat ** -0.5),
        "nsa_v_b1": nrm((N_ODD, CMP_HIDDEN), 0.02),
        "nsa_v_w2": nrm((N_ODD, CMP_HIDDEN, NSA_HEAD_DIM), CMP_HIDDEN ** -0.5),
        "nsa_b_gate": nrm((N_ODD, NSA_BRANCHES * NSA_HEADS), 0.1),
        "nsa_w_out": nrm((N_ODD, NSA_WIDTH, D_MODEL), NSA_WIDTH ** -0.5),
        "final_norm": gain((D_MODEL,)),
    }


def reference(x, ffn1_norm, ffn1_w_gu, ffn1_w_down, mix_norm, ffn2_norm, ffn2_w_gu, ffn2_w_down,
              ab_w_in, rg_conv_w, rg_conv_b, rg_w_r, rg_b_r, rg_w_i, rg_b_i, rg_lambda,
              ml_conv_w, ml_conv_b, ml_b_i, ml_b_f, ml_norm, ab_w_out,
              nsa_w_in, nsa_pe_k, nsa_k_w1, nsa_k_b1, nsa_k_w2, nsa_pe_v, nsa_v_w1, nsa_v_b1, nsa_v_w2,
              nsa_b_gate, nsa_w_out, final_norm):
    for i in range(DEPTH):
        j = i // 2
        x = x + 0.5 * swiglu_ffn(rmsnorm(x, ffn1_norm[i]), ffn1_w_gu[i], ffn1_w_down[i])
        xn = rmsnorm(x, mix_norm[i])
        if i % 2 == 0:
            x = x + ab_mixer(xn, ab_w_in[j], rg_conv_w[j], rg_conv_b[j], rg_w_r[j], rg_b_r[j],
                             rg_w_i[j], rg_b_i[j], rg_lambda[j], ml_conv_w[j], ml_conv_b[j],
                             ml_b_i[j], ml_b_f[j], ml_norm[j], ab_w_out[j])
        else:
            x = x + nsa_mixer(xn, nsa_w_in[j], nsa_pe_k[j], nsa_k_w1[j], nsa_k_b1[j], nsa_k_w2[j],
                              nsa_pe_v[j], nsa_v_w1[j], nsa_v_b1[j], nsa_v_w2[j], nsa_b_gate[j], nsa_w_out[j])
        x = x + 0.5 * swiglu_ffn(rmsnorm(x, ffn2_norm[i]), ffn2_w_gu[i], ffn2_w_down[i])
    return rmsnorm(x, final_norm)
```

```python
import os
from contextlib import ExitStack
import numpy as np
import concourse.bass as bass
import concourse.mybir as mybir
from concourse.bass_utils import run_bass_kernel_spmd

F32 = mybir.dt.float32
BF16 = mybir.dt.bfloat16
ALU = mybir.AluOpType
AF = mybir.ActivationFunctionType
AX = mybir.AxisListType

D = 1024
S = 2048
NSEQ = 2
NTOK = NSEQ * S
DFF = 2816
NF = DFF // 128
NCORES = 8
EPS = 1e-6


class Buf:
    __slots__ = ("name", "w", "rd", "sem", "cnt", "keep", "q")

    def __init__(self, name, keep=False):
        self.name = name
        self.keep = keep
        self.w = None
        self.rd = []
        self.sem = None
        self.cnt = 0


class Op:
    __slots__ = ("eng", "fn", "waits", "marked", "semval", "dma")

    def __init__(self, eng, fn):
        self.eng = eng
        self.fn = fn
        self.waits = []
        self.marked = False
        self.semval = None
        self.dma = None


class Prog:
    ENGS = ("pe", "act", "dve", "pool", "sp")
    CENGS = ("pe", "act", "dve", "pool")

    def __init__(self, nc):
        self.nc = nc
        self.lists = {e: [] for e in self.ENGS}
        self.esem = {e: nc.alloc_semaphore("es_" + e) for e in self.CENGS}
        self.lastc = {e: None for e in self.CENGS}
        self.bufs = []
        self.nsem = 0
        self.sempool = {}

    def buf(self, name="b", keep=False):
        b = Buf(name, keep)
        self.bufs.append(b)
        return b

    def bufs_n(self, n, name="b"):
        return [self.buf("%s%d" % (name, i)) for i in range(n)]

    def _deps(self, o, reads, writes):
        toks = []
        for b in reads:
            if b.w is not None:
                toks.append(b.w)
        for b in writes:
            if b.w is not None:
                toks.append(b.w)
            toks.extend(b.rd)
        seen = set()
        for t in toks:
            if id(t) in seen:
                continue
            seen.add(id(t))
            if t[0] == "e":
                p = t[1]
                if p is o:
                    continue
                if p.eng == "pe" and o.eng == "pe":
                    continue
                p.marked = True
            o.waits.append(t)

    def op(self, eng, fn, reads=(), writes=()):
        o = Op(eng, fn)
        self._deps(o, reads, writes)
        tok = ("e", o)
        for b in reads:
            b.rd.append(tok)
        for b in writes:
            b.w = tok
            b.rd = []
        self.lists[eng].append(o)
        self.lastc[eng] = o
        return o

    def dma(self, q, out, in_, dst, src, join=False, **kw):
        o = Op(q, None)
        if dst.sem is None:
            pool_ = self.sempool.setdefault(q, [])
            if pool_ and not os.environ.get("NOPOOL"):
                dst.sem, dst.cnt = pool_.pop()
                dst.q = q
            else:
                dst.sem = self.nc.alloc_semaphore("ds_%d" % self.nsem)
                self.nsem += 1
                dst.q = q
        assert dst.q == q, "one DMA queue per buffer"
        srcs = [src] if src is not None else []
        if join and dst.w is not None and dst.w[0] == "d" and dst.w[1] is dst:
            saved = dst.w
            dst.w = None
            self._deps(o, srcs, [dst])
            dst.w = saved
        else:
            self._deps(o, srcs, [dst])
        dst.cnt += 16
        tok = ("d", dst, dst.cnt)
        dst.w = tok
        dst.rd = []
        if src is not None:
            src.rd.append(tok)
        o.dma = (out, in_, dst, kw)
        self.lists[q].append(o)
        return o

    def barrier(self):
        toks = []
        for e in self.CENGS:
            if self.lastc[e] is not None:
                self.lastc[e].marked = True
                toks.append(("e", self.lastc[e]))
        for b in self.bufs:
            if b.sem is not None and b.cnt > 0:
                toks.append(("d", b, b.cnt))
        for e in self.ENGS:
            o = Op(e, None)
            o.waits = [t for t in toks if not (t[0] == "e" and t[1].eng == e)]
            self.lists[e].append(o)
        kept = []
        for b in self.bufs:
            if b.keep:
                kept.append(b)
            elif b.sem is not None:
                self.sempool.setdefault(b.q, []).append((b.sem, b.cnt))
        self.bufs = kept

    def emit(self, final_bufs=()):
        nc = self.nc
        for e in self.CENGS:
            c = 0
            for o in self.lists[e]:
                if o.marked:
                    c += 1
                    o.semval = c
        fin = Op("sp", None)
        for b in final_bufs:
            if b.w is not None:
                fin.waits.append(b.w)
        self.lists["sp"].append(fin)

        def run(engname, eng):
            seen = {}
            for o in self.lists[engname]:
                for t in o.waits:
                    if t[0] == "e":
                        sem, val = self.esem[t[1].eng], t[1].semval
                    else:
                        sem, val = t[1].sem, t[2]
                    if seen.get(sem.num, 0) < val:
                        eng.wait_ge(sem, val)
                        seen[sem.num] = val
                if o.dma is not None:
                    out, in_, dst, kw = o.dma
                    eng.dma_start(out=out, in_=in_, **kw).then_inc(dst.sem, 16)
                elif o.fn is not None:
                    ins = o.fn(eng)
                    if o.marked:
                        ins.then_inc(self.esem[engname], 1)

        with nc.Block() as block:
            @block.tensor
            def _(eng):
                run("pe", eng)

            @block.scalar
            def _(eng):
                run("act", eng)

            @block.vector
            def _(eng):
                run("dve", eng)

            @block.gpsimd
            def _(eng):
                run("pool", eng)

            @block.sync
            def _(eng):
                run("sp", eng)

    def stats(self):
        return {e: len(l) for e, l in self.lists.items()}


class Ctx:
    pass


_SBN = [0]


def sb(es, nc, name, shape, dt):
    _SBN[0] += 1
    return es.enter_context(nc.sbuf_tensor("s%d_%s" % (_SBN[0], name), list(shape), dt)).ap()


def rmsnorm_fm(C, x3, xB, g2, out3, outB, sq3, sqB, T):
    P, ps, pb = C.P, C.ps, C.pb
    rs, rstd, rsB, rstdB = C.rs, C.rstd, C.rsB, C.rstdB
    nh = T // 512
    for kc in range(8):
        P.op("act", lambda e, kc=kc: e.activation(out=sq3[:, kc, 0:T], in_=x3[:, kc, 0:T], func=AF.Square),
             [xB], [sqB[kc]])
    for hf in range(nh):
        bank = 6 + (hf % 2)
        for kc in range(8):
            P.op("pe", lambda e, kc=kc, hf=hf, bank=bank: e.matmul(
                ps[bank], lhsT=C.ones_bf, rhs=sq3[:, kc, hf * 512:(hf + 1) * 512], start=(kc == 0), stop=(kc == 7)),
                [sqB[kc], C.constB], [pb[bank]])
        P.op("act", lambda e, hf=hf, bank=bank: e.activation(
            out=rs[:, hf * 512:(hf + 1) * 512], in_=ps[bank], func=AF.Sqrt, scale=1.0 / D, bias=C.eps_t[:, 0:1]),
            [pb[bank], C.constB], [rsB])
    P.op("dve", lambda e: e.reciprocal(out=rstd[:, 0:T], in_=rs[:, 0:T]), [rsB], [rstdB])
    for kc in range(8):
        P.op("dve", lambda e, kc=kc: e.scalar_tensor_tensor(
            out=out3[:, kc, 0:T], in0=x3[:, kc, 0:T], scalar=g2[:, kc:kc + 1], in1=rstd[:, 0:T],
            op0=ALU.mult, op1=ALU.mult), [xB, rstdB, C.constB], [outB[kc] if isinstance(outB, list) else outB])


def ffn_phase(C, es0, gain, wgu_d, wd_d, final=None):
    P, nc, ps, pb = C.P, C.nc, C.ps, C.pb
    T = 1024
    NT = NTOK // T
    with ExitStack() as es:
        xt = [sb(es, nc, "xt%d" % i, [128, 8, T], F32) for i in range(2)]
        xtB = P.bufs_n(2, "xt")
        xn = sb(es, nc, "xn", [128, 8, T], BF16)
        xnB = P.bufs_n(8, "xn")
        h = sb(es, nc, "h", [128, NF, T], BF16)
        hB = P.bufs_n(NF, "h")
        wd = sb(es, nc, "wd", [128, NF, D], BF16)
        wdB = P.bufs_n(NF, "wd")
        NR = 4
        wg = [sb(es, nc, "wg%d" % i, [128, 2048], BF16) for i in range(NR)]
        wgB = P.bufs_n(NR, "wg")
        sl = [sb(es, nc, "sl%d" % i, [128, 512], F32) for i in range(2)]
        slB = P.bufs_n(2, "sl")
        C.rs = sb(es, nc, "rs", [128, T], F32)
        C.rstd = sb(es, nc, "rstd", [128, T], F32)
        C.rsB, C.rstdB = P.buf("rs"), P.buf("rstd")

        Xv = C.X.rearrange("c p n -> p c n")
        Ov = C.out.rearrange("c p n -> p c n")
        for fi in range(NF):
            P.dma("pool", wd[:, fi, :], wd_d[fi], wdB[fi], None)

        def load(t):
            P.dma("sp", xt[t % 2], Xv[:, :, t * T:(t + 1) * T], xtB[t % 2], C.XB)

        load(0)
        load(1)
        gi = 0
        for t in range(NT):
            x3, xB = xt[t % 2], xtB[t % 2]
            rmsnorm_fm(C, x3, xB, gain, xn, xnB, h, hB, T)
            for fi in range(NF):
                slot = gi % NR
                gi += 1
                P.dma("pool", wg[slot], wgu_d[fi], wgB[slot], None)
                for hf in range(2):
                    pg, pu = 2 * hf, 2 * hf + 1
                    cs = slice(hf * 512, (hf + 1) * 512)
                    for kc in range(8):
                        P.op("pe", lambda e, kc=kc, slot=slot, cs=cs, pg=pg: e.matmul(
                            ps[pg], lhsT=wg[slot][:, kc * 256:kc * 256 + 128], rhs=xn[:, kc, cs],
                            start=(kc == 0), stop=(kc == 7)), [wgB[slot], xnB[kc]], [pb[pg]])
                    for kc in range(8):
                        P.op("pe", lambda e, kc=kc, slot=slot, cs=cs, pu=pu: e.matmul(
                            ps[pu], lhsT=wg[slot][:, kc * 256 + 128:kc * 256 + 256], rhs=xn[:, kc, cs],
                            start=(kc == 0), stop=(kc == 7)), [wgB[slot], xnB[kc]], [pb[pu]])
                    P.op("act", lambda e, hf=hf, pg=pg: e.activation(out=sl[hf], in_=ps[pg], func=AF.Silu),
                         [pb[pg]], [slB[hf]])
                    P.op("dve", lambda e, hf=hf, pu=pu, fi=fi, cs=cs: e.tensor_tensor(
                        out=h[:, fi, cs], in0=sl[hf], in1=ps[pu], op=ALU.mult), [slB[hf], pb[pu]], [hB[fi]])
            k = 0
            for dc in range(8):
                for hf in range(2):
                    po = 4 + (k % 2)
                    k += 1
                    cs = slice(hf * 512, (hf + 1) * 512)
                    for fi in range(NF):
                        P.op("pe", lambda e, fi=fi, dc=dc, cs=cs, po=po: e.matmul(
                            ps[po], lhsT=wd[:, fi, dc * 128:(dc + 1) * 128], rhs=h[:, fi, cs],
                            start=(fi == 0), stop=(fi == NF - 1)), [wdB[fi], hB[fi]], [pb[po]])
                    P.op("dve", lambda e, dc=dc, cs=cs, po=po, x3=x3: e.scalar_tensor_tensor(
                        out=x3[:, dc, cs], in0=ps[po], scalar=0.5, in1=x3[:, dc, cs], op0=ALU.mult, op1=ALU.add),
                        [pb[po], xB], [xB])
            if final is None:
                P.dma("sp", Xv[:, :, t * T:(t + 1) * T], x3, C.XB, xB, join=True)
            else:
                rmsnorm_fm(C, x3, xB, final, x3, xB, h, hB, T)
                P.dma("sp", Ov[:, :, t * T:(t + 1) * T], x3, C.outB, xB, join=True)
            if t + 2 < NT:
                load(t + 2)
        P.barrier()


def ab_phase(C, gain, D_):
    P, nc, ps, pb = C.P, C.nc, C.ps, C.pb
    T = 512
    NT = NTOK // T
    TPS = S // T
    QS = 128.0 ** -0.5
    with ExitStack() as es:
        A = lambda name, shape, dt: sb(es, nc, name, shape, dt)
        win = A("win", [128, 8, 3080], BF16)
        wout = A("wout", [128, 8, D], BF16)
        rgw = A("rgw", [128, 4, 2, 128], BF16)
        rgc = A("rgc", [128, 4, 8], F32)
        mlc = A("mlc", [128, 8, 5], F32)
        mlrow = A("mlrow", [128, 8 + 512], F32)
        cneg = A("cneg", [128, 4], F32)
        U = A("U", [128, 128], BF16)
        mneg = A("mneg", [128, 128], BF16)
        ident = A("ident", [128, 128], BF16)
        onesf = A("onesf", [128, 128], F32)
        zerof = A("zerof", [128, 128], F32)
        wB = P.buf("abw")
        for kc in range(8):
            P.dma("pool", win[:, kc, :], D_["ab_win"][:, kc, :], wB, None, join=True)
        P.dma("pool", wout, D_["ab_wout"], wB, None, join=True)
        P.dma("pool", rgw, D_["rgw"], wB, None, join=True)
        cB = P.buf("abc")
        P.dma("sp", rgc, D_["rgc"], cB, None, join=True)
        P.dma("sp", mlc, D_["mlc"], cB, None, join=True)
        P.dma("sp", mlrow, D_["mlrow"], cB, None, join=True)
        kB = P.buf("abk")
        P.op("pool", lambda e: e.memset(onesf, 1.0), [], [kB])
        P.op("pool", lambda e: e.memset(zerof, 0.0), [kB], [kB])
        P.op("pool", lambda e: e.affine_select(out=U, in_=onesf, pattern=[[1, 128]], compare_op=ALU.is_ge, fill=0.0,
                                              base=0, channel_multiplier=-1), [kB], [kB])
        P.op("pool", lambda e: e.affine_select(out=mneg, in_=zerof, pattern=[[1, 128]], compare_op=ALU.is_ge,
                                              fill=-30000.0, base=0, channel_multiplier=-1), [kB], [kB])
        P.op("pool", lambda e: e.affine_select(out=ident, in_=onesf, pattern=[[1, 128]], compare_op=ALU.is_equal,
                                              fill=0.0, base=0, channel_multiplier=-1), [kB], [kB])
        P.op("act", lambda e: e.activation(out=cneg, in_=rgc[:, :, 7], func=AF.Exp, scale=-1.0), [cB], [kB])
        P.op("act", lambda e: e.activation(out=cneg, in_=cneg, func=AF.Ln, bias=1.0), [kB], [kB])
        P.op("dve", lambda e: e.tensor_scalar(out=cneg, in0=cneg, scalar1=-8.0, scalar2=None, op0=ALU.mult), [kB], [kB])

        xt = A("xt", [128, 8, T], F32); xtB = P.buf("xt")
        xn = A("xn", [128, 8, T], BF16); xnB = P.bufs_n(8, "xn")
        yT = A("yT", [128, 8, T], BF16); yTB = P.bufs_n(8, "yT")
        C.rs = A("rs", [128, T], F32); C.rstd = A("rstd", [128, T], F32)
        C.rsB, C.rstdB = P.buf("rs"), P.buf("rstd")
        xa = A("xa", [128, 4, T + 3], F32); xaB = P.bufs_n(4, "xa")
        qk = A("qk", [128, 8, T + 3], F32); qkB = P.bufs_n(8, "qk")
        xc = A("xc", [128, T], F32); xcB = P.buf("xc")
        xcb = A("xcb", [128, T], BF16); xcbB = P.buf("xcb")
        rr = A("rr", [128, T], F32); rrB = P.buf("rr")
        ig = A("ig", [128, T], F32); igB = P.buf("ig")
        aa = A("aa", [128, T], F32); aaB = P.buf("aa")
        uu = A("uu", [128, T], F32); uuB = P.buf("uu")
        hs = A("hs", [128, T], F32); hsB = P.buf("hs")
        gg = A("gg", [128, T], F32); ggB = P.buf("gg")
        hst = A("hst", [128, 4], F32); hstB = P.bufs_n(4, "hst")
        qc = A("qc", [128, T], F32); qcB = P.buf("qc")
        qT = A("qT", [128, 4, T], BF16); qTB = P.bufs_n(4, "qT")
        kT = A("kT", [128, 4, T], BF16); kTB = P.bufs_n(4, "kT")
        va = A("va", [128, 4, 4, 129], BF16); vaB = P.bufs_n(4, "va")
        og = A("og", [128, 4, 512], F32); ogB = P.bufs_n(4, "og")
        gt = A("gt", [128, 4, 8], F32); gtB = P.bufs_n(4, "gt")
        lhi = A("lhi", [128, 4], BF16); llo = A("llo", [128, 4], BF16); lhf = A("lhf", [128, 4], F32)
        lB = P.buf("lhl")
        Lh = A("Lh", [128, 4, 128], BF16); Ll = A("Ll", [128, 4, 128], BF16); LB = P.buf("L")
        sm = A("sm", [128, 24], F32); smB = P.buf("sm")
        DT = A("DT", [128, 4, 128], F32); DTB = P.buf("DT")
        AT = A("AT", [128, 4, 128], BF16); ATB = P.buf("AT")
        kw = A("kw", [128, 4, 128], BF16); kwB = P.buf("kw")
        itr = A("itr", [128, 4, 129], F32); itrB = P.buf("itr")
        tot = A("tot", [128, 4, 129], F32); totB = P.buf("tot")
        hh = A("hh", [128, 4, 128], F32); hhB = P.buf("hh")
        h2 = A("h2", [128, 4, 128], F32); h2B = P.buf("h2")
        ytk = A("ytk", [128, 512], BF16); ytkB = P.buf("ytk")
        Cst = A("Cst", [128, 4, 129], F32); CstB = P.buf("Cst")
        Cbf = A("Cbf", [128, 4, 129], BF16); CbfB = P.buf("Cbf")
        P.op("pool", lambda e: e.memset(va, 1.0), [], vaB)
        Xv = C.X.rearrange("c p n -> p c n")
        bank_rr = [0]

        def nb():
            bank_rr[0] = (bank_rr[0] + 1) % 6
            return bank_rr[0]

        def proj_fm(col0, t):
            b = nb()
            for kc in range(8):
                P.op("pe", lambda e, kc=kc, b=b: e.matmul(ps[b], lhsT=win[:, kc, col0:col0 + 128], rhs=xn[:, kc, :],
                                                          start=(kc == 0), stop=(kc == 7)), [wB, xnB[kc]], [pb[b]])
            return b

        for t in range(NT):
            first = (t % TPS == 0)
            P.dma("sp", xt, Xv[:, :, t * T:(t + 1) * T], xtB, C.XB)
            rmsnorm_fm(C, xt, xtB, gain, xn, xnB, yT, yTB, T)
            if first:
                P.op("pool", lambda e: e.memset(Cst, 0.0), [], [CstB])
                P.op("pool", lambda e: e.memset(Cbf, 0.0), [], [CbfB])
            for c in range(4):
                if first:
                    P.op("pool", lambda e, c=c: e.memset(xa[:, c, 0:3], 0.0), [], [xaB[c]])
                else:
                    P.op("pool", lambda e, c=c: e.tensor_copy(out=xa[:, c, 0:3], in_=xa[:, c, T:T + 3]), [xaB[c]], [xaB[c]])
                b = proj_fm(c * 128, t)
                P.op("act", lambda e, c=c, b=b: e.activation(out=xa[:, c, 3:T + 3], in_=ps[b], func=AF.Copy), [pb[b]], [xaB[c]])
                P.op("dve", lambda e, c=c: e.tensor_scalar(out=xc, in0=xa[:, c, 0:T], scalar1=rgc[:, c, 0:1],
                                                          scalar2=rgc[:, c, 4:5], op0=ALU.mult, op1=ALU.add), [xaB[c], cB], [xcB])
                for j in range(1, 4):
                    P.op("dve", lambda e, c=c, j=j: e.scalar_tensor_tensor(
                        out=xc, in0=xa[:, c, j:j + T], scalar=rgc[:, c, j:j + 1], in1=xc, op0=ALU.mult, op1=ALU.add),
                        [xaB[c], cB, xcB], [xcB])
                P.op("act", lambda e: e.activation(out=xcb, in_=xc, func=AF.Copy), [xcB], [xcbB])
                br = nb()
                P.op("pe", lambda e, c=c, br=br: e.matmul(ps[br], lhsT=rgw[:, c, 0, :], rhs=xcb, start=True, stop=True),
                     [wB, xcbB], [pb[br]])
                bi = nb()
                P.op("pe", lambda e, c=c, bi=bi: e.matmul(ps[bi], lhsT=rgw[:, c, 1, :], rhs=xcb, start=True, stop=True),
                     [wB, xcbB], [pb[bi]])
                P.op("act", lambda e, c=c, br=br: e.activation(out=rr, in_=ps[br], func=AF.Sigmoid, bias=rgc[:, c, 5:6]),
                     [pb[br], cB], [rrB])
                P.op("act", lambda e, c=c, bi=bi: e.activation(out=ig, in_=ps[bi], func=AF.Sigmoid, bias=rgc[:, c, 6:7]),
                     [pb[bi], cB], [igB])
                P.op("act", lambda e, c=c: e.activation(out=aa, in_=rr, func=AF.Exp, scale=cneg[:, c:c + 1]), [rrB, kB], [aaB])
                P.op("act", lambda e: e.activation(out=rr, in_=aa, func=AF.Square), [aaB], [rrB])
                P.op("act", lambda e: e.activation(out=rr, in_=rr, func=AF.Sqrt, scale=-1.0, bias=onesf[:, 0:1]), [rrB, kB], [rrB])
                P.op("dve", lambda e: e.tensor_tensor(out=uu, in0=ig, in1=xc, op=ALU.mult), [igB, xcB], [uuB])
                P.op("dve", lambda e: e.tensor_tensor(out=uu, in0=uu, in1=rr, op=ALU.mult), [uuB, rrB], [uuB])
                if first:
                    P.op("dve", lambda e: e.tensor_tensor_scan(out=hs, data0=aa, data1=uu, initial=0.0, op0=ALU.mult, op1=ALU.add),
                         [aaB, uuB], [hsB])
                else:
                    P.op("dve", lambda e, c=c: e.tensor_tensor_scan(out=hs, data0=aa, data1=uu, initial=hst[:, c:c + 1],
                                                                   op0=ALU.mult, op1=ALU.add), [aaB, uuB, hstB[c]], [hsB])
                P.op("dve", lambda e, c=c: e.tensor_copy(out=hst[:, c:c + 1], in_=hs[:, T - 1:T]), [hsB], [hstB[c]])
                bg = proj_fm(512 + c * 128, t)
                P.op("act", lambda e, bg=bg: e.activation(out=gg, in_=ps[bg], func=AF.Gelu_apprx_tanh), [pb[bg]], [ggB])
                P.op("dve", lambda e, c=c: e.tensor_tensor(out=yT[:, c, :], in0=gg, in1=hs, op=ALU.mult), [ggB, hsB], [yTB[c]])
            for c in range(8):
                if first:
                    P.op("pool", lambda e, c=c: e.memset(qk[:, c, 0:3], 0.0), [], [qkB[c]])
                else:
                    P.op("pool", lambda e, c=c: e.tensor_copy(out=qk[:, c, 0:3], in_=qk[:, c, T:T + 3]), [qkB[c]], [qkB[c]])
                b = proj_fm(1024 + c * 128, t)
                P.op("act", lambda e, c=c, b=b: e.activation(out=qk[:, c, 3:T + 3], in_=ps[b], func=AF.Copy), [pb[b]], [qkB[c]])
                P.op("dve", lambda e, c=c: e.tensor_scalar(out=qc, in0=qk[:, c, 0:T], scalar1=mlc[:, c, 0:1],
                                                          scalar2=mlc[:, c, 4:5], op0=ALU.mult, op1=ALU.add), [qkB[c], cB], [qcB])
                for j in range(1, 4):
                    P.op("dve", lambda e, c=c, j=j: e.scalar_tensor_tensor(
                        out=qc, in0=qk[:, c, j:j + T], scalar=mlc[:, c, j:j + 1], in1=qc, op0=ALU.mult, op1=ALU.add),
                        [qkB[c], cB, qcB], [qcB])
                if c < 4:
                    P.op("act", lambda e: e.activation(out=qc, in_=qc, func=AF.Silu), [qcB], [qcB])
                    P.op("dve", lambda e, c=c: e.tensor_scalar(out=qT[:, c, :], in0=qc, scalar1=QS, scalar2=None, op0=ALU.mult),
                         [qcB], [qTB[c]])
                else:
                    P.op("act", lambda e, c=c: e.activation(out=kT[:, c - 4, :], in_=qc, func=AF.Silu), [qcB], [kTB[c - 4]])
            for j in range(4):
                ts_ = slice(j * 128, (j + 1) * 128)
                b = nb()
                for kc in range(8):
                    P.op("pe", lambda e, kc=kc, b=b, ts_=ts_: e.matmul(ps[b], lhsT=xn[:, kc, ts_], rhs=win[:, kc, 2048:2560],
                                                                     start=(kc == 0), stop=(kc == 7)), [wB, xnB[kc]], [pb[b]])
                P.op("act", lambda e, j=j, b=b: e.activation(out=va[:, j, :, 0:128], in_=ps[b].rearrange("p (h v) -> p h v", h=4),
                                                             func=AF.Copy), [pb[b]], [vaB[j]])
                b = nb()
                for kc in range(8):
                    P.op("pe", lambda e, kc=kc, b=b, ts_=ts_: e.matmul(ps[b], lhsT=xn[:, kc, ts_], rhs=win[:, kc, 2560:3072],
                                                                     start=(kc == 0), stop=(kc == 7)), [wB, xnB[kc]], [pb[b]])
                P.op("act", lambda e, j=j, b=b: e.activation(out=og[:, j, :], in_=ps[b], func=AF.Sigmoid), [pb[b]], [ogB[j]])
                b = nb()
                for kc in range(8):
                    P.op("pe", lambda e, kc=kc, b=b, ts_=ts_: e.matmul(ps[b][:, 0:8], lhsT=xn[:, kc, ts_], rhs=win[:, kc, 3072:3080],
                                                                     start=(kc == 0), stop=(kc == 7)), [wB, xnB[kc]], [pb[b]])
                P.op("dve", lambda e, j=j, b=b: e.tensor_tensor(out=gt[:, j, :], in0=ps[b][:, 0:8], in1=mlrow[:, 0:8], op=ALU.add),
                     [pb[b], cB], [gtB[j]])
                P.op("act", lambda e, j=j: e.activation(out=gt[:, j, 4:8], in_=gt[:, j, 4:8], func=AF.Exp, scale=-1.0), [gtB[j]], [gtB[j]])
                P.op("act", lambda e, j=j: e.activation(out=gt[:, j, 4:8], in_=gt[:, j, 4:8], func=AF.Ln, bias=1.0), [gtB[j]], [gtB[j]])
                P.op("dve", lambda e, j=j: e.tensor_scalar(out=gt[:, j, 4:8], in0=gt[:, j, 4:8], scalar1=-1.0, scalar2=None, op0=ALU.mult),
                     [gtB[j]], [gtB[j]])
            for j in range(4):
                ts_ = slice(j * 128, (j + 1) * 128)
                logi = gt[:, j, 0:4]
                logf = gt[:, j, 4:8]
                P.op("dve", lambda e, logf=logf: e.tensor_copy(out=lhi, in_=logf), [gtB[j]], [lB])
                P.op("dve", lambda e: e.tensor_copy(out=lhf, in_=lhi), [lB], [lB])
                P.op("dve", lambda e, logf=logf: e.tensor_tensor(out=lhf, in0=logf, in1=lhf, op=ALU.subtract), [gtB[j], lB], [lB])
                P.op("dve", lambda e: e.tensor_copy(out=llo, in_=lhf), [lB], [lB])
                P.op("dve", lambda e: e.tensor_copy(out=Lh, in_=lhi.unsqueeze(2).broadcast_to([128, 4, 128])), [lB], [LB])
                P.op("dve", lambda e: e.tensor_copy(out=Ll, in_=llo.unsqueeze(2).broadcast_to([128, 4, 128])), [lB, LB], [LB])
                P.op("pe", lambda e: e.matmul(ps[7][:, 264:268], lhsT=U, rhs=lhi, start=True, stop=False), [kB, lB], [pb[7]])
                P.op("pe", lambda e: e.matmul(ps[7][:, 264:268], lhsT=U, rhs=llo, start=False, stop=True), [kB, lB], [pb[7]])
                P.op("pe", lambda e: e.matmul(ps[7][:, 268:272], lhsT=C.ones_bf, rhs=lhi, start=True, stop=False), [kB, lB], [pb[7]])
                P.op("pe", lambda e: e.matmul(ps[7][:, 268:272], lhsT=C.ones_bf, rhs=llo, start=False, stop=True), [kB, lB], [pb[7]])
                for hd in range(4):
                    o_ = ps[0][:, hd * 128:(hd + 1) * 128]
                    P.op("pe", lambda e, hd=hd, o_=o_: e.matmul(o_, lhsT=Lh[:, hd, :], rhs=U, start=True, stop=False), [LB, kB], [pb[0]])
                    P.op("pe", lambda e, hd=hd, o_=o_: e.matmul(o_, lhsT=Ll[:, hd, :], rhs=U, start=False, stop=False), [LB, kB], [pb[0]])
                    P.op("pe", lambda e, hd=hd, o_=o_: e.matmul(o_, lhsT=ident, rhs=mneg, start=False, stop=True), [kB], [pb[0]])
                    P.op("pe", lambda e, hd=hd, ts_=ts_: e.matmul(ps[1][:, hd * 128:(hd + 1) * 128], lhsT=kT[:, hd, ts_], rhs=qT[:, hd, ts_],
                                                                 start=True, stop=True), [kTB[hd], qTB[hd]], [pb[1]])
                bcol = ps[7][:, 264:268]
                gcol = ps[7][:, 268:272]
                P.op("dve", lambda e, logi=logi, bcol=bcol: e.tensor_tensor(out=sm[:, 0:4], in0=logi, in1=bcol, op=ALU.subtract),
                     [gtB[j], pb[7]], [smB])
                P.op("dve", lambda e, gcol=gcol: e.tensor_tensor(out=sm[:, 8:12], in0=sm[:, 0:4], in1=gcol, op=ALU.add),
                     [smB, pb[7]], [smB])
                P.op("act", lambda e, bcol=bcol: e.activation(out=sm[:, 4:8], in_=bcol, func=AF.Exp), [pb[7], smB], [smB])
                P.op("act", lambda e: e.activation(out=sm[:, 8:12], in_=sm[:, 8:12], func=AF.Exp), [smB], [smB])
                P.op("act", lambda e, gcol=gcol: e.activation(out=sm[:, 12:16], in_=gcol, func=AF.Exp), [pb[7], smB], [smB])
                for hd in range(4):
                    P.op("act", lambda e, hd=hd: e.activation(out=DT[:, hd, :], in_=ps[0][:, hd * 128:(hd + 1) * 128], func=AF.Exp,
                                                              bias=sm[:, hd:hd + 1]), [pb[0], smB], [DTB])
                P.op("dve", lambda e: e.tensor_tensor(out=AT, in0=DT, in1=ps[1].rearrange("p (h k) -> p h k", h=4), op=ALU.mult),
                     [DTB, pb[1]], [ATB])
                for hd in range(4):
                    bi_, bo_ = 2 + hd // 2, 4 + hd // 2
                    cs = slice((hd % 2) * 129, (hd % 2) * 129 + 129)
                    P.op("pe", lambda e, hd=hd, bi_=bi_, cs=cs, ts_=ts_: e.matmul(ps[bi_][:, cs], lhsT=qT[:, hd, ts_], rhs=Cbf[:, hd, :],
                                                                                start=True, stop=True), [qTB[hd], CbfB], [pb[bi_]])
                    P.op("pe", lambda e, hd=hd, bo_=bo_, cs=cs, j=j: e.matmul(ps[bo_][:, cs], lhsT=AT[:, hd, :], rhs=va[:, j, hd, :],
                                                                            start=True, stop=True), [ATB, vaB[j]], [pb[bo_]])
                for hp in range(2):
                    P.op("act", lambda e, hp=hp: e.activation(out=itr[:, 2 * hp:2 * hp + 2, :],
                                                              in_=ps[4 + hp][:, 0:258].rearrange("p (h v) -> p h v", h=2), func=AF.Copy),
                         [pb[4 + hp]], [itrB])
                for hd in range(4):
                    bi_ = 2 + hd // 2
                    cs = slice((hd % 2) * 129, (hd % 2) * 129 + 129)
                    P.op("dve", lambda e, hd=hd, bi_=bi_, cs=cs: e.scalar_tensor_tensor(
                        out=tot[:, hd, :], in0=ps[bi_][:, cs], scalar=sm[:, 4 + hd:5 + hd], in1=itr[:, hd, :], op0=ALU.mult, op1=ALU.add),
                        [pb[bi_], smB, itrB], [totB])
                P.op("dve", lambda e: e.tensor_scalar(out=sm[:, 16:20], in0=tot[:, :, 128], scalar1=-1.0, scalar2=1.0,
                                                      op0=ALU.mult, op1=ALU.max), [totB, smB], [smB])
                P.op("dve", lambda e: e.tensor_tensor(out=sm[:, 16:20], in0=sm[:, 16:20], in1=tot[:, :, 128], op=ALU.max),
                     [totB, smB], [smB])
                P.op("dve", lambda e: e.reciprocal(out=sm[:, 16:20], in_=sm[:, 16:20]), [smB], [smB])
                P.op("dve", lambda e: e.tensor_tensor(out=hh, in0=tot[:, :, 0:128],
                                                      in1=sm[:, 16:20].unsqueeze(2).broadcast_to([128, 4, 128]), op=ALU.mult),
                     [totB, smB], [hhB])
                P.op("dve", lambda e: e.tensor_tensor(out=h2, in0=hh, in1=hh, op=ALU.mult), [hhB], [h2B])
                P.op("dve", lambda e: e.tensor_reduce(out=sm[:, 20:24], in_=h2, axis=AX.X, op=ALU.add), [h2B, smB], [smB])
                P.op("act", lambda e: e.activation(out=sm[:, 20:24], in_=sm[:, 20:24], func=AF.Sqrt, scale=1.0 / 128, bias=C.eps_t[:, 0:1]),
                     [smB], [smB])
                P.op("dve", lambda e: e.reciprocal(out=sm[:, 20:24], in_=sm[:, 20:24]), [smB], [smB])
                P.op("dve", lambda e: e.tensor_tensor(out=hh, in0=hh, in1=sm[:, 20:24].unsqueeze(2).broadcast_to([128, 4, 128]),
                                                      op=ALU.mult), [hhB, smB], [hhB])
                P.op("dve", lambda e: e.tensor_tensor(out=hh, in0=hh, in1=mlrow[:, 8:520].rearrange("p (h v) -> p h v", h=4),
                                                      op=ALU.mult), [hhB, cB], [hhB])
                P.op("dve", lambda e, j=j: e.tensor_tensor(out=ytk.rearrange("p (h v) -> p h v", h=4), in0=hh,
                                                           in1=og[:, j, :].rearrange("p (h v) -> p h v", h=4), op=ALU.mult),
                     [hhB, ogB[j]], [ytkB])
                p6b = ps[6].bitcast(BF16)
                for hd in range(4):
                    P.op("pe", lambda e, hd=hd, p6b=p6b: e.transpose(out=p6b[:, hd * 128:(hd + 1) * 128], in_=ytk[:, hd * 128:(hd + 1) * 128],
                                                                   identity=ident), [ytkB, kB], [pb[6]])
                for hd in range(4):
                    P.op("act", lambda e, hd=hd, p6b=p6b, ts_=ts_: e.activation(out=yT[:, 4 + hd, ts_], in_=p6b[:, hd * 128:(hd + 1) * 128],
                                                                              func=AF.Copy), [pb[6]], [yTB[4 + hd]])
                p7b = ps[7].bitcast(BF16)
                for hd in range(4):
                    P.op("pe", lambda e, hd=hd, p7b=p7b, ts_=ts_: e.transpose(out=p7b[:, 512 + hd * 128:512 + (hd + 1) * 128],
                                                                            in_=kT[:, hd, ts_], identity=ident), [kTB[hd], kB], [pb[7]])
                for hd in range(4):
                    P.op("dve", lambda e, hd=hd, p7b=p7b: e.tensor_scalar(out=kw[:, hd, :], in0=p7b[:, 512 + hd * 128:512 + (hd + 1) * 128],
                                                                        scalar1=sm[:, 8 + hd:9 + hd], scalar2=None, op0=ALU.mult),
                         [pb[7], smB], [kwB])
                for hd in range(4):
                    bs_ = 2 + hd // 2
                    cs = slice((hd % 2) * 129, (hd % 2) * 129 + 129)
                    P.op("pe", lambda e, hd=hd, bs_=bs_, cs=cs, j=j: e.matmul(ps[bs_][:, cs], lhsT=kw[:, hd, :], rhs=va[:, j, hd, :],
                                                                            start=True, stop=True), [kwB, vaB[j]], [pb[bs_]])
                for hd in range(4):
                    bs_ = 2 + hd // 2
                    cs = slice((hd % 2) * 129, (hd % 2) * 129 + 129)
                    P.op("dve", lambda e, hd=hd, bs_=bs_, cs=cs: e.scalar_tensor_tensor(
                        out=Cst[:, hd, :], in0=Cst[:, hd, :], scalar=sm[:, 12 + hd:13 + hd], in1=ps[bs_][:, cs], op0=ALU.mult, op1=ALU.add),
                        [CstB, smB, pb[bs_]], [CstB])
                P.op("act", lambda e: e.activation(out=Cbf, in_=Cst, func=AF.Copy), [CstB], [CbfB])
            for dc in range(8):
                b = nb()
                for yc in range(8):
                    P.op("pe", lambda e, yc=yc, dc=dc, b=b: e.matmul(ps[b], lhsT=wout[:, yc, dc * 128:(dc + 1) * 128], rhs=yT[:, yc, :],
                                                                   start=(yc == 0), stop=(yc == 7)), [wB, yTB[yc]], [pb[b]])
                P.op("dve", lambda e, dc=dc, b=b: e.tensor_tensor(out=xt[:, dc, :], in0=xt[:, dc, :], in1=ps[b], op=ALU.add),
                     [xtB, pb[b]], [xtB])
            P.dma("sp", Xv[:, :, t * T:(t + 1) * T], xt, C.XB, xtB, join=True)
        P.barrier()


def final_phase(C, gain):
    P, nc = C.P, C.nc
    T = 512
    with ExitStack() as es:
        xt = sb(es, nc, "fxt", [128, 8, T], F32); xtB = P.buf("fxt")
        sq = sb(es, nc, "fsq", [128, 8, T], BF16); sqB = P.bufs_n(8, "fsq")
        C.rs = sb(es, nc, "rs", [128, T], F32); C.rstd = sb(es, nc, "rstd", [128, T], F32)
        C.rsB, C.rstdB = P.buf("rs"), P.buf("rstd")
        Xv = C.X.rearrange("c p n -> p c n")
        Ov = C.out.rearrange("c p n -> p c n")
        for t in range(NTOK // T):
            P.dma("sp", xt, Xv[:, :, t * T:(t + 1) * T], xtB, C.XB)
            rmsnorm_fm(C, xt, xtB, gain, xt, xtB, sq, sqB, T)
            P.dma("sp", Ov[:, :, t * T:(t + 1) * T], xt, C.outB, xtB, join=True)
        P.barrier()


NCOLN = 3120


def nsa_proj_phase(C, gain, D_):
    P, nc, ps, pb = C.P, C.nc, C.ps, C.pb
    T = 512
    NT = NTOK // T
    TPS = S // T
    with ExitStack() as es:
        A = lambda name, shape, dt: sb(es, nc, name, shape, dt)
        win = A("nwin", [128, 8, NCOLN], BF16)
        wB = P.buf("nw")
        for kc in range(8):
            P.dma("pool", win[:, kc, :], D_["nsa_win"][:, kc, :], wB, None, join=True)
        bg = A("bg", [128, 48], F32)
        cB = P.buf("nc")
        P.dma("sp", bg, D_["bgrow"], cB, None)
        xt = A("xt", [128, 8, T], F32); xtB = P.buf("xt")
        xn = A("xn", [128, 8, T], BF16); xnB = P.bufs_n(8, "xn")
        sq = A("sq", [128, 8, T], BF16); sqB = P.bufs_n(8, "sq")
        C.rs = A("rs", [128, T], F32); C.rstd = A("rstd", [128, T], F32)
        C.rsB, C.rstdB = P.buf("rs"), P.buf("rstd")
        stg = A("stg", [128, 20, T], BF16); stgB = P.bufs_n(5, "stg")
        vtk = A("vtk", [128, 4, 512], BF16); vtkB = P.buf("vtk")
        gtk = A("gtk", [128, 4, 48], F32); gtkB = P.buf("gtk")
        Xv = C.X.rearrange("c p n -> p c n")
        k = 0
        groups = [("Qs", 0, 8, 0.125), ("KsT", 8, 4, 1.0), ("KwT", 12, 4, 1.0), ("KcT", 16, 2, 1.0), ("VcT", 18, 2, 1.0)]
        for t in range(NT):
            sq_, t0 = t // TPS, (t % TPS) * T
            P.dma("sp", xt, Xv[:, :, t * T:(t + 1) * T], xtB, C.XB)
            rmsnorm_fm(C, xt, xtB, gain, xn, xnB, sq, sqB, T)
            for gi, (nm, c0, ncnk, scl) in enumerate(groups):
                for cc in range(ncnk):
                    b = k % 6
                    k += 1
                    col0 = (c0 + cc) * 128
                    for kc in range(8):
                        P.op("pe", lambda e, kc=kc, b=b, col0=col0: e.matmul(ps[b], lhsT=win[:, kc, col0:col0 + 128], rhs=xn[:, kc, :],
                                                                          start=(kc == 0), stop=(kc == 7)), [wB, xnB[kc]], [pb[b]])
                    if (c0 + cc) % 2 == 0:
                        P.op("act", lambda e, b=b, c=c0 + cc, scl=scl: e.activation(out=stg[:, c, :], in_=ps[b], func=AF.Copy, scale=scl),
                             [pb[b]], [stgB[gi]])
                    else:
                        P.op("dve", lambda e, b=b, c=c0 + cc, scl=scl: e.tensor_scalar(out=stg[:, c, :], in0=ps[b], scalar1=scl, scalar2=None,
                                                                                   op0=ALU.mult), [pb[b]], [stgB[gi]])
                P.dma("sp", D_[nm][sq_, :, :, t0:t0 + T], stg[:, c0:c0 + ncnk, :], D_[nm + "B"], stgB[gi], join=True)
            for j in range(4):
                ts_ = slice(j * 128, (j + 1) * 128)
                b = k % 6
                k += 1
                for kc in range(8):
                    P.op("pe", lambda e, kc=kc, b=b, ts_=ts_: e.matmul(ps[b], lhsT=xn[:, kc, ts_], rhs=win[:, kc, 2560:3072],
                                                                     start=(kc == 0), stop=(kc == 7)), [wB, xnB[kc]], [pb[b]])
                P.op("act", lambda e, b=b, j=j: e.activation(out=vtk[:, j, :], in_=ps[b], func=AF.Copy), [pb[b]], [vtkB])
                b = k % 6
                k += 1
                for kc in range(8):
                    P.op("pe", lambda e, kc=kc, b=b, ts_=ts_: e.matmul(ps[b][:, 0:48], lhsT=xn[:, kc, ts_], rhs=win[:, kc, 3072:3120],
                                                                     start=(kc == 0), stop=(kc == 7)), [wB, xnB[kc]], [pb[b]])
                P.op("dve", lambda e, b=b, j=j: e.tensor_tensor(out=gtk[:, j, :], in0=ps[b][:, 0:48], in1=bg, op=ALU.add),
                     [pb[b], cB], [gtkB])
            P.op("act", lambda e: e.activation(out=gtk, in_=gtk, func=AF.Sigmoid), [gtkB], [gtkB])
            P.dma("sp", D_["Vs"][sq_, t0:t0 + T, :].rearrange("(j p) c -> p j c", p=128), vtk[:, :, 0:256], D_["VsB"], vtkB, join=True)
            P.dma("sp", D_["Vw"][sq_, t0:t0 + T, :].rearrange("(j p) c -> p j c", p=128), vtk[:, :, 256:512], D_["VwB"], vtkB, join=True)
            P.dma("sp", D_["G"][sq_, t0:t0 + T, :].rearrange("(j p) c -> p j c", p=128), gtk, D_["GB"], gtkB, join=True)
        P.barrier()


def nsa_core_phase(C, D_):
    P, nc, ps, pb = C.P, C.nc, C.ps, C.pb
    NQ = S // 128
    NEG = -30000.0
    with ExitStack() as es:
        A = lambda name, shape, dt: sb(es, nc, name, shape, dt)
        wout = A("nwout", [128, 8, D], BF16)
        w1 = A("w1", [128, 2, 32, 256], BF16)
        w2k = A("w2k", [128, 2, 128], BF16)
        w2v = A("w2v", [128, 2, 64], BF16)
        peT = A("peT", [128, 2, 32], BF16)
        b1 = A("b1", [128, 2, 2], F32)
        selA = A("selA", [128, 16, 32], F32)
        selB = A("selB", [128, 16, 32], F32)
        msel = A("msel", [128, 32], BF16)
        wB = P.buf("n2w")
        P.dma("pool", wout, D_["nsa_wout"], wB, None, join=True)
        P.dma("pool", w1, D_["w1d"], wB, None, join=True)
        P.dma("pool", w2k, D_["w2kd"], wB, None, join=True)
        P.dma("pool", w2v, D_["w2vd"], wB, None, join=True)
        P.dma("pool", peT, D_["peTd"], wB, None, join=True)
        P.dma("pool", msel, D_["mseld"], wB, None, join=True)
        cB = P.buf("n2c")
        P.dma("sp", b1, D_["b1d"], cB, None, join=True)
        P.dma("sp", selA, D_["selA"], cB, None, join=True)
        P.dma("sp", selB, D_["selB"], cB, None, join=True)
        kB = P.buf("n2k")
        onesf = A("onesf", [128, 2048], F32)
        zerof = A("zerof", [128, 2048], F32)
        ident = A("ident", [128, 128], BF16)
        mneg = A("mneg", [128, 128], BF16)
        mneg2 = A("mneg2", [128, 128], BF16)
        cmask = A("cmask", [128, 16, 128], BF16)
        E = A("E", [32, 16, 128], BF16)
        E0 = A("E0", [32, 16, 128], F32)
        P.op("pool", lambda e: e.memset(onesf, 1.0), [], [kB])
        P.op("pool", lambda e: e.memset(zerof, 0.0), [kB], [kB])
        P.op("pool", lambda e: e.affine_select(out=ident, in_=onesf[:, 0:128], pattern=[[1, 128]], compare_op=ALU.is_equal, fill=0.0,
                                              base=0, channel_multiplier=-1), [kB], [kB])
        P.op("pool", lambda e: e.affine_select(out=mneg, in_=zerof[:, 0:128], pattern=[[1, 128]], compare_op=ALU.is_ge, fill=NEG,
                                              base=0, channel_multiplier=-1), [kB], [kB])
        P.op("pool", lambda e: e.affine_select(out=mneg2, in_=zerof[:, 0:128], pattern=[[-1, 128]], compare_op=ALU.is_ge, fill=NEG,
                                              base=-1, channel_multiplier=1), [kB], [kB])
        for i in range(16):
            P.op("pool", lambda e, i=i: e.affine_select(out=cmask[:, i, :], in_=zerof[:, 0:128], pattern=[[1, 128]], compare_op=ALU.is_ge,
                                                       fill=NEG, base=128 * i - 31, channel_multiplier=-16), [kB], [kB])
        P.op("pool", lambda e: e.affine_select(out=E0, in_=onesf[0:32, :].rearrange("p (a b) -> p a b", a=16), pattern=[[128, 16], [1, 128]],
                                              compare_op=ALU.is_ge, fill=0.0, base=0, channel_multiplier=-64), [kB], [kB])
        P.op("pool", lambda e: e.affine_select(out=E, in_=E0, pattern=[[-128, 16], [-1, 128]], compare_op=ALU.is_ge, fill=0.0,
                                              base=63, channel_multiplier=64), [kB], [kB])

        ksT = A("ksT", [128, 4, S], BF16)
        kwT = A("kwT", [128, 4, S], BF16)
        kcT = A("kcT", [128, 2, S], BF16)
        vcT = A("vcT", [128, 2, S], BF16)
        vsa = A("vsa", [128, 16, 4, 65], BF16)
        vwa = A("vwa", [128, 16, 4, 65], BF16)
        gts = A("gts", [128, 16, 48], F32)
        sqB_ = P.buf("seqdata")
        hT = A("hT", [128, 2, 128], BF16); hTB = P.buf("hT")
        btot = A("btot", [128, 2, 2], F32); btB = P.buf("btot")
        kcmp = A("kcmp", [128, 4, 128], BF16); kcmpB = P.buf("kcmp")
        vcmp = A("vcmp", [128, 4, 97], BF16); vcmpB = P.buf("vcmp")
        qt = A("qt", [128, 8, 128], BF16); qtB = P.buf("qt")
        xt = A("xt2", [128, 8, 128], F32); xtB = P.buf("xt2")
        pT = [A("pT%d" % i, [128, 4, 128], BF16) for i in range(2)]; pTB = P.bufs_n(2, "pT")
        sc4 = A("sc4", [128, 4, 32], F32); sc4B = P.buf("sc4")
        scr = A("scr", [128, 32], F32); scrB = P.buf("scr")
        top8 = A("top8", [128, 8], F32)
        seln = A("seln", [128, 32], BF16); selnB = P.buf("seln")
        selT = A("selT", [32, 128], BF16); selTB = P.buf("selT")
        zz = A("zz", [128, 3, 4], F32); zzB = P.buf("zz")
        ocs = A("ocs", [128, 4, 64], F32); ocsB = P.buf("ocs")
        otk = A("otk", [128, 1024], F32); otkB = P.buf("otk")
        tmp = A("tmpo", [128, 4, 64], F32); tmpB = P.buf("tmpo")
        otb = A("otb", [128, 1024], BF16); otbB = P.buf("otb")
        oT = A("oT", [128, 8, 128], BF16); oTB = P.buf("oT")
        P.op("dve", lambda e: e.memset(vsa, 1.0), [], [sqB_])
        P.op("dve", lambda e: e.memset(vwa, 1.0), [sqB_], [sqB_])
        P.op("dve", lambda e: e.memset(vcmp, 1.0), [], [vcmpB])
        Xv = C.X.rearrange("c p n -> p c n")
        p6b = ps[6].bitcast(BF16)
        pp = [0]

        for sq_ in range(NSEQ):
            P.dma("sp", ksT, D_["KsT"][sq_], sqB_, D_["KsTB"], join=True)
            P.dma("sp", kwT, D_["KwT"][sq_], sqB_, D_["KwTB"], join=True)
            P.dma("sp", kcT, D_["KcT"][sq_], sqB_, D_["KcTB"], join=True)
            P.dma("sp", vcT, D_["VcT"][sq_], sqB_, D_["VcTB"], join=True)
            for g in range(4):
                P.dma("sp", vsa[:, :, g, 0:64], D_["Vs"][sq_, :, g * 64:(g + 1) * 64].rearrange("(k p) d -> p k d", p=128), sqB_, D_["VsB"], join=True)
                P.dma("sp", vwa[:, :, g, 0:64], D_["Vw"][sq_, :, g * 64:(g + 1) * 64].rearrange("(k p) d -> p k d", p=128), sqB_, D_["VwB"], join=True)
            P.dma("sp", gts, D_["G"][sq_].rearrange("(k p) c -> p k c", p=128), sqB_, D_["GB"], join=True)
            if sq_ == 0:
                for kv in range(2):
                    for hc in range(2):
                        for l in range(32):
                            P.op("pe", lambda e, kv=kv, hc=hc, l=l: e.matmul(ps[7][:, 2 * kv + hc:2 * kv + hc + 1],
                                                                           lhsT=w1[0:64, kv, l, hc * 128:(hc + 1) * 128], rhs=peT[0:64, kv, l:l + 1],
                                                                           start=(l == 0), stop=(l == 31)), [wB], [pb[7]])
                P.op("dve", lambda e: e.tensor_tensor(out=btot, in0=b1, in1=ps[7][:, 0:4].rearrange("p (a b) -> p a b", a=2), op=ALU.add),
                     [pb[7], cB], [btB])
                for g in range(4):
                    P.dma("pool", vcmp[:, g, 0:32], D_["mseld"], vcmpB, None, join=True)
            for kv, src in enumerate([kcT, vcT]):
                for g in range(4):
                    r0 = (g % 2) * 64
                    ch = g // 2
                    for hc in range(2):
                        b = 4 + hc
                        for l in range(32):
                            P.op("pe", lambda e, kv=kv, hc=hc, l=l, r0=r0, ch=ch, src=src, b=b: e.matmul(
                                ps[b][:, 0:127], lhsT=w1[r0:r0 + 64, kv, l, hc * 128:(hc + 1) * 128],
                                rhs=src[r0:r0 + 64, ch, l:l + 16 * 126 + 1:16], start=(l == 0), stop=(l == 31)), [wB, sqB_], [pb[b]])
                        P.op("act", lambda e, kv=kv, hc=hc, b=b: e.activation(out=hT[:, hc, 0:127], in_=ps[b][:, 0:127], func=AF.Gelu_apprx_tanh,
                                                                           bias=btot[:, kv, hc:hc + 1]), [pb[b], btB], [hTB])
                    if kv == 0:
                        for hc in range(2):
                            P.op("pe", lambda e, hc=hc: e.matmul(ps[7][:, 0:127], lhsT=w2k[:, hc, :], rhs=hT[:, hc, 0:127],
                                                                 start=(hc == 0), stop=(hc == 1)), [wB, hTB], [pb[7]])
                        P.op("act", lambda e, g=g: e.activation(out=kcmp[:, g, 0:127], in_=ps[7][:, 0:127], func=AF.Copy), [pb[7]], [kcmpB])
                    else:
                        for hc in range(2):
                            P.op("pe", lambda e, hc=hc: e.matmul(ps[7][0:127, 0:64], lhsT=hT[:, hc, 0:127], rhs=w2v[:, hc, :],
                                                                 start=(hc == 0), stop=(hc == 1)), [wB, hTB], [pb[7]])
                        P.op("act", lambda e, g=g: e.activation(out=vcmp[0:127, g, 32:96], in_=ps[7][0:127, 0:64], func=AF.Copy), [pb[7]], [vcmpB])
            for i in range(NQ):
                tok0 = sq_ * S + i * 128
                P.dma("sp", qt, D_["Qs"][sq_, :, :, i * 128:(i + 1) * 128], qtB, D_["QsB"])
                P.dma("sp", xt, Xv[:, :, tok0:tok0 + 128], xtB, C.XB)
                for g in range(4):
                    qlo = qt[0:64, 2 * g:2 * g + 2, :]
                    qhi = qt[64:128, 2 * g:2 * g + 2, :]

                    def scores(klo, khi, nk, extra):
                        b0 = (pp[0] % 2) * 2
                        pi = pp[0] % 2
                        pp[0] += 1
                        for b, kk, qq in ((b0, klo, qlo), (b0 + 1, khi, qhi)):
                            P.op("pe", lambda e, b=b, kk=kk, qq=qq, nk=nk: e.matmul(ps[b][0:nk, 0:256], lhsT=kk, rhs=qq, start=True,
                                                                                  stop=(len(extra) == 0)), [sqB_, kcmpB, qtB], [pb[b]])
                            for ei, (ml, mr, mB) in enumerate(extra):
                                P.op("pe", lambda e, b=b, ml=ml, mr=mr, nk=nk, last=(ei == len(extra) - 1): e.matmul(
                                    ps[b][0:nk, 0:256], lhsT=ml, rhs=mr.unsqueeze(1).broadcast_to([mr.shape[0], 2, 128]), start=False, stop=last),
                                    [kB, mB], [pb[b]])
                        for hf, b in enumerate((b0, b0 + 1)):
                            P.op("act", lambda e, b=b, hf=hf, pi=pi, nk=nk: e.activation(
                                out=pT[pi][0:nk, 2 * hf:2 * hf + 2, :], in_=ps[b][0:nk, 0:256].rearrange("p (r q) -> p r q", r=2), func=AF.Exp),
                                [pb[b]], [pTB[pi]])
                        return pi

                    pi = scores(kcmp[0:64, g, 0:127], kcmp[64:128, g, 0:127], 127, [(ident[0:127, 0:127], cmask[0:127, i, :], kB)])
                    for r in range(4):
                        P.op("pe", lambda e, r=r, pi=pi, g=g: e.matmul(ps[4][:, r * 97:(r + 1) * 97], lhsT=pT[pi][0:127, r, :], rhs=vcmp[0:127, g, :],
                                                                      start=(r == 0), stop=(r == 3), skip_group_check=True), [pTB[pi], vcmpB], [pb[4]])
                    c4 = ps[4][:, 0:388].rearrange("p (r c) -> p r c", r=4)
                    P.op("dve", lambda e, c4=c4: e.tensor_scalar(out=zz[:, 0, :], in0=c4[:, :, 96], scalar1=1e-30, scalar2=None, op0=ALU.max),
                         [pb[4]], [zzB])
                    P.op("dve", lambda e: e.reciprocal(out=zz[:, 0, :], in_=zz[:, 0, :]), [zzB], [zzB])
                    P.op("dve", lambda e, c4=c4: e.tensor_tensor(out=sc4, in0=c4[:, :, 0:32], in1=zz[:, 0, :].unsqueeze(2).broadcast_to([128, 4, 32]),
                                                               op=ALU.mult), [pb[4], zzB], [sc4B])
                    P.op("dve", lambda e: e.tensor_reduce(out=scr, in_=sc4.rearrange("p r j -> p j r"), axis=AX.X, op=ALU.add), [sc4B], [scrB])
                    P.op("dve", lambda e, i=i: e.tensor_tensor(out=scr, in0=scr, in1=selA[:, i, :], op=ALU.mult), [scrB, cB], [scrB])
                    P.op("dve", lambda e, i=i: e.tensor_tensor(out=scr, in0=scr, in1=selB[:, i, :], op=ALU.add), [scrB, cB], [scrB])
                    P.op("dve", lambda e: e.max(out=top8, in_=scr), [scrB], [scrB])
                    P.op("dve", lambda e: e.tensor_scalar(out=seln, in0=scr, scalar1=top8[:, 7:8], scalar2=1.0, op0=ALU.is_ge, op1=ALU.subtract),
                         [scrB], [selnB])
                    P.op("pe", lambda e: e.transpose(out=p6b[0:32, 0:128], in_=seln, identity=ident), [selnB, kB], [pb[6]])
                    P.op("act", lambda e: e.activation(out=selT, in_=p6b[0:32, 0:128], func=AF.Copy, scale=-NEG), [pb[6]], [selTB])
                    gv = gts[:, i, g * 12:(g + 1) * 12].rearrange("p (r b) -> p b r", b=3)
                    P.op("dve", lambda e, gv=gv: e.tensor_tensor(out=zz[:, 0, :], in0=zz[:, 0, :], in1=gv[:, 0, :], op=ALU.mult), [zzB, sqB_], [zzB])
                    P.op("dve", lambda e, c4=c4: e.tensor_tensor(out=ocs, in0=c4[:, :, 32:96], in1=zz[:, 0, :].unsqueeze(2).broadcast_to([128, 4, 64]),
                                                               op=ALU.mult), [pb[4], zzB], [ocsB])
                    for kt in range(i + 1):
                        ks_ = slice(kt * 128, (kt + 1) * 128)
                        extra = [(E[:, kt, :], selT, selTB)]
                        if kt == i:
                            extra.append((ident, mneg, kB))
                        pi = scores(ksT[0:64, g, ks_], ksT[64:128, g, ks_], 128, extra)
                        for r in range(4):
                            P.op("pe", lambda e, r=r, pi=pi, g=g, kt=kt: e.matmul(
                                ps[5][:, r * 65:(r + 1) * 65], lhsT=pT[pi][:, r, :], rhs=vsa[:, kt, g, :], start=(kt == 0 and r == 0),
                                stop=(kt == i and r == 3), skip_group_check=True), [pTB[pi], sqB_], [pb[5]])
                    kts = [kt for kt in (i - 2, i - 1, i) if kt >= 0]
                    for kt in kts:
                        ks_ = slice(kt * 128, (kt + 1) * 128)
                        extra = []
                        if kt == i:
                            extra.append((ident, mneg, kB))
                        if kt == i - 2:
                            extra.append((ident, mneg2, kB))
                        pi = scores(kwT[0:64, g, ks_], kwT[64:128, g, ks_], 128, extra)
                        for r in range(4):
                            P.op("pe", lambda e, r=r, pi=pi, g=g, kt=kt, first=(kt == kts[0] and r == 0), last=(kt == i and r == 3): e.matmul(
                                ps[7][:, r * 65:(r + 1) * 65], lhsT=pT[pi][:, r, :], rhs=vwa[:, kt, g, :], start=first, stop=last,
                                skip_group_check=True), [pTB[pi], sqB_], [pb[7]])
                    c5 = ps[5][:, 0:260].rearrange("p (r c) -> p r c", r=4)
                    c7 = ps[7][:, 0:260].rearrange("p (r c) -> p r c", r=4)
                    og_ = otk[:, g * 256:(g + 1) * 256].rearrange("p (r d) -> p r d", r=4)
                    for br, cc, bk in ((1, c5, 5), (2, c7, 7)):
                        P.op("dve", lambda e, br=br, cc=cc: e.reciprocal(out=zz[:, br, :], in_=cc[:, :, 64]), [pb[bk], zzB], [zzB])
                        P.op("dve", lambda e, br=br, gv=gv: e.tensor_tensor(out=zz[:, br, :], in0=zz[:, br, :], in1=gv[:, br, :], op=ALU.mult),
                             [zzB, sqB_], [zzB])
                        P.op("dve", lambda e, br=br, cc=cc: e.tensor_tensor(out=tmp, in0=cc[:, :, 0:64],
                                                                          in1=zz[:, br, :].unsqueeze(2).broadcast_to([128, 4, 64]), op=ALU.mult),
                             [pb[bk], zzB], [tmpB])
                        if br == 1:
                            P.op("dve", lambda e: e.tensor_tensor(out=ocs, in0=ocs, in1=tmp, op=ALU.add), [ocsB, tmpB], [ocsB])
                        else:
                            P.op("dve", lambda e, og_=og_: e.tensor_tensor(out=og_, in0=ocs, in1=tmp, op=ALU.add), [ocsB, tmpB], [otkB])
                P.op("act", lambda e: e.activation(out=otb, in_=otk, func=AF.Copy), [otkB], [otbB])
                for yc in range(8):
                    P.op("pe", lambda e, yc=yc: e.transpose(out=p6b[:, yc * 128:(yc + 1) * 128], in_=otb[:, yc * 128:(yc + 1) * 128], identity=ident),
                         [otbB, kB], [pb[6]])
                P.op("act", lambda e: e.activation(out=oT, in_=p6b[:, 0:1024].rearrange("p (c q) -> p c q", c=8), func=AF.Copy), [pb[6]], [oTB])
                for dh in range(2):
                    b = 4 + dh
                    for dc in range(4):
                        d_ = dh * 4 + dc
                        for yc in range(8):
                            P.op("pe", lambda e, yc=yc, d_=d_, dc=dc, b=b: e.matmul(ps[b][:, dc * 128:(dc + 1) * 128],
                                                                                 lhsT=wout[:, yc, d_ * 128:(d_ + 1) * 128], rhs=oT[:, yc, :],
                                                                                 start=(yc == 0 and dc == 0), stop=(yc == 7 and dc == 3),
                                                                                 skip_group_check=True), [wB, oTB], [pb[b]])
                    P.op("dve", lambda e, dh=dh, b=b: e.tensor_tensor(out=xt[:, dh * 4:(dh + 1) * 4, :], in0=xt[:, dh * 4:(dh + 1) * 4, :],
                                                                     in1=ps[b].rearrange("p (c q) -> p c q", c=4), op=ALU.add), [xtB, pb[b]], [xtB])
                P.dma("sp", Xv[:, :, tok0:tok0 + 128], xt, C.XB, xtB, join=True)
        P.barrier()


def perm_gu(w):
    wg = w[:, :DFF].reshape(8, 128, NF, 128)
    wu = w[:, DFF:].reshape(8, 128, NF, 128)
    o = np.stack([wg, wu], axis=3)
    o = o.transpose(2, 1, 0, 3, 4)
    return np.ascontiguousarray(o.reshape(NF, 128, 8 * 256))


def perm_vec(v):
    return np.ascontiguousarray(v.reshape(8, 128).T)


def build(upto="all"):
    nc = bass.Bass("TRN2", target_bir_lowering=False)
    P = Prog(nc)
    C = Ctx()
    C.P, C.nc = P, nc
    dram_in = lambda n, shp: nc.dram_tensor(n, list(shp), F32, kind="ExternalInput").ap()
    xin = dram_in("xin", [8, 128, NTOK])
    nrm_d = dram_in("nrm", [128, 56])
    wgu_d = [dram_in("wgu%d" % i, [NF, 128, 2048]) for i in range(4)]
    wd_d = [dram_in("wd%d" % i, [NF, 128, D]) for i in range(4)]
    C.out = nc.dram_tensor("out", [8, 128, NTOK], F32, kind="ExternalOutput").ap()
    C.outB = P.buf("out", keep=True)
    C.X = nc.dram_tensor("X", [8, 128, NTOK], F32, kind="Internal").ap()
    C.XB = P.buf("X", keep=True)

    with ExitStack() as es0:
        C.ps = [es0.enter_context(nc.psum_tensor("ps%d" % i, [128, 512], F32)).ap() for i in range(8)]
        C.pb = [P.buf("ps%d" % i, keep=True) for i in range(8)]
        C.constB = P.buf("const", keep=True)
        C.ones_bf = sb(es0, nc, "ones_bf", [128, 128], BF16)
        C.eps_t = sb(es0, nc, "eps_t", [128, 1], F32)
        C.nrm = sb(es0, nc, "nrm_sb", [128, 56], F32)
        P.op("dve", lambda e: e.memset(C.ones_bf, 1.0), [], [C.constB])
        P.op("dve", lambda e: e.memset(C.eps_t, EPS), [C.constB], [C.constB])
        nrmB = P.buf("nrmld")
        P.dma("sp", C.nrm, nrm_d, nrmB, None)
        P.dma("sp", C.X, xin, C.XB, None)
        P.barrier()
        C.constB.w = None
        C.constB.rd = []
        g = lambda i: C.nrm[:, i * 8:(i + 1) * 8]
        D_ = {}
        for n, shp in [("ab_win", [128, 8, 3080]), ("ab_wout", [128, 8, D]), ("rgw", [128, 4, 2, 128]), ("rgc", [128, 4, 8]),
                       ("mlc", [128, 8, 5]), ("mlrow", [128, 520])]:
            D_[n] = dram_in(n, shp)
        for n, shp in [("nsa_win", [128, 8, NCOLN]), ("nsa_wout", [128, 8, D]), ("w1d", [128, 2, 32, 256]), ("w2kd", [128, 2, 128]),
                       ("w2vd", [128, 2, 64]), ("peTd", [128, 2, 32]), ("b1d", [128, 2, 2]), ("mseld", [128, 32]),
                       ("selA", [128, 16, 32]), ("selB", [128, 16, 32]), ("bgrow", [128, 48])]:
            D_[n] = dram_in(n, shp)
        for n, shp, dt in [("Qs", [NSEQ, 128, 8, S], BF16), ("KsT", [NSEQ, 128, 4, S], BF16), ("KwT", [NSEQ, 128, 4, S], BF16),
                           ("KcT", [NSEQ, 128, 2, S], BF16), ("VcT", [NSEQ, 128, 2, S], BF16), ("Vs", [NSEQ, S, 256], BF16),
                           ("Vw", [NSEQ, S, 256], BF16), ("G", [NSEQ, S, 48], F32)]:
            D_[n] = nc.dram_tensor("scr_" + n, shp, dt, kind="Internal").ap()
            D_[n + "B"] = P.buf("scr_" + n, keep=True)
        stages = ["ffn1", "mix0", "ffn2", "ffn1b", "mix1", "all"]
        ns = stages.index(upto) + 1
        ffn_phase(C, es0, g(0), wgu_d[0], wd_d[0], final=(g(6) if ns == 1 else None))
        if ns >= 2:
            ab_phase(C, g(1), D_)
        if ns >= 3:
            ffn_phase(C, es0, g(2), wgu_d[1], wd_d[1])
        if ns >= 4:
            ffn_phase(C, es0, g(3), wgu_d[2], wd_d[2])
        if ns >= 5:
            nsa_proj_phase(C, g(4), D_)
            nsa_core_phase(C, D_)
        if ns >= 6:
            ffn_phase(C, es0, g(5), wgu_d[3], wd_d[3], final=g(6))
        elif ns >= 2:
            final_phase(C, g(6))
        P.emit([C.outB])
    return nc, P


_CACHE = {}


def kernel(upto="all", **inp):
    x = np.asarray(inp["x"], np.float32)
    B = x.shape[0]
    ncores = B // NSEQ
    if upto not in _CACHE:
        _CACHE[upto] = build(upto)
    nc, P = _CACHE[upto]
    nrm = np.concatenate([perm_vec(inp["ffn1_norm"][0]), perm_vec(inp["mix_norm"][0]), perm_vec(inp["ffn2_norm"][0]),
                          perm_vec(inp["ffn1_norm"][1]), perm_vec(inp["mix_norm"][1]), perm_vec(inp["ffn2_norm"][1]),
                          perm_vec(inp["final_norm"])], axis=1).astype(np.float32)
    shared = {"nrm": np.ascontiguousarray(nrm)}
    ffns = [("ffn1", 0), ("ffn2", 0), ("ffn1", 1), ("ffn2", 1)]
    for i, (nm, l) in enumerate(ffns):
        shared["wgu%d" % i] = perm_gu(np.asarray(inp[nm + "_w_gu"][l], np.float32))
        shared["wd%d" % i] = np.ascontiguousarray(np.asarray(inp[nm + "_w_down"][l], np.float32).reshape(NF, 128, D))
    f32 = lambda a: np.ascontiguousarray(np.asarray(a, np.float32))
    shared["ab_win"] = f32(inp["ab_w_in"][0].reshape(8, 128, 3080).transpose(1, 0, 2))
    shared["ab_wout"] = f32(inp["ab_w_out"][0].reshape(8, 128, D).transpose(1, 0, 2))
    rgw = np.zeros((128, 4, 2, 128), np.float32)
    for c in range(4):
        for k, nm in enumerate(["rg_w_r", "rg_w_i"]):
            rgw[0:64, c, k, 0:64] = inp[nm][0][2 * c]
            rgw[64:128, c, k, 64:128] = inp[nm][0][2 * c + 1]
    shared["rgw"] = rgw
    pc = lambda v, n: np.asarray(v, np.float32).reshape(n, 128).T
    rgc = np.stack([pc(inp["rg_conv_w"][0][j], 4) for j in range(4)] + [pc(inp["rg_conv_b"][0], 4), pc(inp["rg_b_r"][0], 4),
                   pc(inp["rg_b_i"][0], 4), pc(inp["rg_lambda"][0], 4)], axis=2)
    shared["rgc"] = f32(rgc)
    mlc = np.stack([pc(inp["ml_conv_w"][0][j], 8) for j in range(4)] + [pc(inp["ml_conv_b"][0], 8)], axis=2)
    shared["mlc"] = f32(mlc)
    row = np.concatenate([inp["ml_b_i"][0], inp["ml_b_f"][0], inp["ml_norm"][0]]).astype(np.float32)
    shared["mlrow"] = f32(np.tile(row[None, :], (128, 1)))
    W = np.asarray(inp["nsa_w_in"][0], np.float32)
    qW = W[:, 0:1024].reshape(1024, 16, 64)
    cols = []
    for g_ in range(4):
        cols += [qW[:, g_ * 4 + 0], qW[:, g_ * 4 + 2], qW[:, g_ * 4 + 1], qW[:, g_ * 4 + 3]]
    kcW, vcW, ksW, vsW, kwW, vwW = [W[:, 1024 + 256 * i_:1280 + 256 * i_] for i_ in range(6)]
    gW = W[:, 2560:2608]
    for src_ in (ksW, kwW):
        for g_ in range(4):
            cols += [src_[:, g_ * 64:(g_ + 1) * 64], src_[:, g_ * 64:(g_ + 1) * 64]]
    cols += [kcW, vcW, vsW, vwW, gW]
    Wp = np.concatenate(cols, axis=1)
    assert Wp.shape == (1024, NCOLN)
    shared["nsa_win"] = f32(Wp.reshape(8, 128, NCOLN).transpose(1, 0, 2))
    shared["nsa_wout"] = f32(inp["nsa_w_out"][0].reshape(8, 128, D).transpose(1, 0, 2))
    w1d = np.zeros((128, 2, 32, 256), np.float32)
    peTd = np.zeros((128, 2, 32), np.float32)
    b1d = np.zeros((128, 2, 2), np.float32)
    for kv_, (w1n, pen, b1n) in enumerate([("nsa_k_w1", "nsa_pe_k", "nsa_k_b1"), ("nsa_v_w1", "nsa_pe_v", "nsa_v_b1")]):
        w1_ = np.asarray(inp[w1n][0], np.float32).reshape(32, 64, 256).transpose(1, 0, 2)
        w1d[0:64, kv_] = w1_
        w1d[64:128, kv_] = w1_
        pe_ = np.asarray(inp[pen][0], np.float32).T
        peTd[0:64, kv_] = pe_
        peTd[64:128, kv_] = pe_
        b1d[:, kv_, :] = np.asarray(inp[b1n][0], np.float32).reshape(2, 128).T
    shared["w1d"], shared["peTd"], shared["b1d"] = w1d, peTd, b1d
    w2k_ = np.asarray(inp["nsa_k_w2"][0], np.float32).reshape(2, 128, 64).transpose(1, 0, 2)
    shared["w2kd"] = f32(np.concatenate([w2k_, w2k_], axis=2))
    shared["w2vd"] = f32(np.asarray(inp["nsa_v_w2"][0], np.float32).reshape(2, 128, 64).transpose(1, 0, 2))
    ncmp = (S - 32) // 16 + 1
    cs_ = np.arange(ncmp)[:, None] * 16
    ss_ = np.arange(32)[None, :] * 64
    ov = np.clip(np.minimum(cs_ + 32, ss_ + 64) - np.maximum(cs_, ss_), 0, None) / 32.0
    msel = np.zeros((128, 32), np.float32)
    msel[:ncmp] = ov
    shared["mseld"] = msel
    tpos = (np.arange(16)[None, :] * 128 + np.arange(128)[:, None])[:, :, None]
    jj = np.arange(32)[None, None, :]
    cur = tpos // 64
    valid = (jj * 64 <= tpos)
    forced = ((jj == 0) | (jj == cur) | (jj == cur - 1)) & valid
    shared["selA"] = f32((valid & ~forced).astype(np.float32))
    shared["selB"] = f32(1e6 * forced.astype(np.float32) - (1.0 - valid.astype(np.float32)))
    shared["bgrow"] = f32(np.tile(np.asarray(inp["nsa_b_gate"][0], np.float32)[None, :], (128, 1)))
    in_maps = []
    for c in range(ncores):
        xs = x[c * NSEQ:(c + 1) * NSEQ].reshape(NTOK, 8, 128).transpose(1, 2, 0)
        m = dict(shared)
        m["xin"] = np.ascontiguousarray(xs)
        in_maps.append(m)
    res = run_bass_kernel_spmd(nc, in_maps, core_ids=list(range(ncores)))
    outs = []
    for c in range(ncores):
        o = res.results[c]["out"]
        outs.append(o.transpose(2, 0, 1).reshape(NSEQ, S, D))
    return np.ascontiguousarray(np.concatenate(outs, axis=0))
```

```python
import os
from contextlib import ExitStack
import numpy as np
import concourse.bass as bass
import concourse.mybir as mybir
from concourse.bass_utils import run_bass_kernel_spmd

F32 = mybir.dt.float32
BF16 = mybir.dt.bfloat16
ALU = mybir.AluOpType
AF = mybir.ActivationFunctionType
AX = mybir.AxisListType

D = 1024
S = 2048
NSEQ = 2
NTOK = NSEQ * S
DFF = 2816
NF = DFF // 128
NCORES = 8
EPS = 1e-6


class Buf:
    __slots__ = ("name", "w", "rd", "sem", "cnt", "keep", "q")

    def __init__(self, name, keep=False):
        self.name = name
        self.keep = keep
        self.w = None
        self.rd = []
        self.sem = None
        self.cnt = 0


class Op:
    __slots__ = ("eng", "fn", "waits", "marked", "semval", "dma")

    def __init__(self, eng, fn):
        self.eng = eng
        self.fn = fn
        self.waits = []
        self.marked = False
        self.semval = None
        self.dma = None


class Prog:
    ENGS = ("pe", "act", "dve", "pool", "sp")
    CENGS = ("pe", "act", "dve", "pool")

    def __init__(self, nc):
        self.nc = nc
        self.lists = {e: [] for e in self.ENGS}
        self.esem = {e: nc.alloc_semaphore("es_" + e) for e in self.CENGS}
        self.lastc = {e: None for e in self.CENGS}
        self.bufs = []
        self.nsem = 0
        self.sempool = {}

    def buf(self, name="b", keep=False):
        b = Buf(name, keep)
        self.bufs.append(b)
        return b

    def bufs_n(self, n, name="b"):
        return [self.buf("%s%d" % (name, i)) for i in range(n)]

    def _deps(self, o, reads, writes):
        toks = []
        for b in reads:
            if b.w is not None:
                toks.append(b.w)
        for b in writes:
            if b.w is not None:
                toks.append(b.w)
            toks.extend(b.rd)
        seen = set()
        for t in toks:
            if id(t) in seen:
                continue
            seen.add(id(t))
            if t[0] == "e":
                p = t[1]
                if p is o:
                    continue
                if p.eng == "pe" and o.eng == "pe":
                    continue
                p.marked = True
            o.waits.append(t)

    def op(self, eng, fn, reads=(), writes=()):
        o = Op(eng, fn)
        self._deps(o, reads, writes)
        tok = ("e", o)
        for b in reads:
            b.rd.append(tok)
        for b in writes:
            b.w = tok
            b.rd = []
        self.lists[eng].append(o)
        self.lastc[eng] = o
        return o

    def dma(self, q, out, in_, dst, src, join=False, **kw):
        o = Op(q, None)
        if dst.sem is None:
            pool_ = self.sempool.setdefault(q, [])
            if pool_ and not os.environ.get("NOPOOL"):
                dst.sem, dst.cnt = pool_.pop()
                dst.q = q
            else:
                dst.sem = self.nc.alloc_semaphore("ds_%d" % self.nsem)
                self.nsem += 1
                dst.q = q
        assert dst.q == q, "one DMA queue per buffer"
        srcs = [src] if src is not None else []
        if join and dst.w is not None and dst.w[0] == "d" and dst.w[1] is dst:
            saved = dst.w
            dst.w = None
            self._deps(o, srcs, [dst])
            dst.w = saved
        else:
            self._deps(o, srcs, [dst])
        dst.cnt += 16
        tok = ("d", dst, dst.cnt)
        dst.w = tok
        dst.rd = []
        if src is not None:
            src.rd.append(tok)
        o.dma = (out, in_, dst, kw)
        self.lists[q].append(o)
        return o

    def barrier(self):
        toks = []
        for e in self.CENGS:
            if self.lastc[e] is not None:
                self.lastc[e].marked = True
                toks.append(("e", self.lastc[e]))
        for b in self.bufs:
            if b.sem is not None and b.cnt > 0:
                toks.append(("d", b, b.cnt))
        for e in self.ENGS:
            o = Op(e, None)
            o.waits = [t for t in toks if not (t[0] == "e" and t[1].eng == e)]
            self.lists[e].append(o)
        kept = []
        for b in self.bufs:
            if b.keep:
                kept.append(b)
            elif b.sem is not None:
                self.sempool.setdefault(b.q, []).append((b.sem, b.cnt))
        self.bufs = kept

    def emit(self, final_bufs=()):
        nc = self.nc
        for e in self.CENGS:
            c = 0
            for o in self.lists[e]:
                if o.marked:
                    c += 1
                    o.semval = c
        fin = Op("sp", None)
        for b in final_bufs:
            if b.w is not None:
                fin.waits.append(b.w)
        self.lists["sp"].append(fin)

        def run(engname, eng):
            seen = {}
            for o in self.lists[engname]:
                for t in o.waits:
                    if t[0] == "e":
                        sem, val = self.esem[t[1].eng], t[1].semval
                    else:
                        sem, val = t[1].sem, t[2]
                    if seen.get(sem.num, 0) < val:
                        eng.wait_ge(sem, val)
                        seen[sem.num] = val
                if o.dma is not None:
                    out, in_, dst, kw = o.dma
                    eng.dma_start(out=out, in_=in_, **kw).then_inc(dst.sem, 16)
                elif o.fn is not None:
                    ins = o.fn(eng)
                    if o.marked:
                        ins.then_inc(self.esem[engname], 1)

        with nc.Block() as block:
            @block.tensor
            def _(eng):
                run("pe", eng)

            @block.scalar
            def _(eng):
                run("act", eng)

            @block.vector
            def _(eng):
                run("dve", eng)

            @block.gpsimd
            def _(eng):
                run("pool", eng)

            @block.sync
            def _(eng):
                run("sp", eng)

    def stats(self):
        return {e: len(l) for e, l in self.lists.items()}


class Ctx:
    pass


_SBN = [0]


def sb(es, nc, name, shape, dt):
    _SBN[0] += 1
    return es.enter_context(nc.sbuf_tensor("s%d_%s" % (_SBN[0], name), list(shape), dt)).ap()


def rmsnorm_fm(C, x3, xB, g2, out3, outB, sq3, sqB, T):
    P, ps, pb = C.P, C.ps, C.pb
    rs, rstd, rsB, rstdB = C.rs, C.rstd, C.rsB, C.rstdB
    nh = T // 512
    for kc in range(8):
        P.op("act", lambda e, kc=kc: e.activation(out=sq3[:, kc, 0:T], in_=x3[:, kc, 0:T], func=AF.Square),
             [xB], [sqB[kc]])
    for hf in range(nh):
        bank = 6 + (hf % 2)
        for kc in range(8):
            P.op("pe", lambda e, kc=kc, hf=hf, bank=bank: e.matmul(
                ps[bank], lhsT=C.ones_bf, rhs=sq3[:, kc, hf * 512:(hf + 1) * 512], start=(kc == 0), stop=(kc == 7)),
                [sqB[kc], C.constB], [pb[bank]])
        P.op("act", lambda e, hf=hf, bank=bank: e.activation(
            out=rs[:, hf * 512:(hf + 1) * 512], in_=ps[bank], func=AF.Sqrt, scale=1.0 / D, bias=C.eps_t[:, 0:1]),
            [pb[bank], C.constB], [rsB])
    P.op("dve", lambda e: e.reciprocal(out=rstd[:, 0:T], in_=rs[:, 0:T]), [rsB], [rstdB])
    for kc in range(8):
        P.op("dve", lambda e, kc=kc: e.scalar_tensor_tensor(
            out=out3[:, kc, 0:T], in0=x3[:, kc, 0:T], scalar=g2[:, kc:kc + 1], in1=rstd[:, 0:T],
            op0=ALU.mult, op1=ALU.mult), [xB, rstdB, C.constB], [outB[kc] if isinstance(outB, list) else outB])


def rmsnorm_fm_h(C, x3, xB, g2, out3, outB, sq3, sqB, T):
    for _ in rmsnorm_fm_g(C, x3, xB, g2, out3, outB, sq3, sqB, T):
        pass


def rmsnorm_fm_g(C, x3, xB, g2, out3, outB, sq3, sqB, T):
    P, ps, pb = C.P, C.ps, C.pb
    rs, rstd, rsB, rstdB = C.rs, C.rstd, C.rsB, C.rstdB
    for hf in range(T // 512):
        cs = slice(hf * 512, (hf + 1) * 512)
        bank = 6 + (hf % 2)
        for kc in range(8):
            P.op("act", lambda e, kc=kc, cs=cs: e.activation(out=sq3[:, kc, :], in_=x3[:, kc, cs], func=AF.Square), [xB], [sqB[kc]])
            if kc % 4 == 3:
                yield
        for kc in range(8):
            P.op("pe", lambda e, kc=kc, bank=bank: e.matmul(ps[bank], lhsT=C.ones_bf, rhs=sq3[:, kc, :], start=(kc == 0), stop=(kc == 7)),
                 [sqB[kc], C.constB], [pb[bank]])
        P.op("act", lambda e, cs=cs, bank=bank: e.activation(out=rs[:, cs], in_=ps[bank], func=AF.Sqrt, scale=1.0 / D, bias=C.eps_t[:, 0:1]),
             [pb[bank], C.constB], [rsB])
        yield
        P.op("dve", lambda e, cs=cs: e.reciprocal(out=rstd[:, cs], in_=rs[:, cs]), [rsB], [rstdB])
        yield
        for kc in range(8):
            P.op("dve", lambda e, kc=kc, cs=cs: e.scalar_tensor_tensor(
                out=out3[:, kc, cs], in0=x3[:, kc, cs], scalar=g2[:, kc:kc + 1], in1=rstd[:, cs],
                op0=ALU.mult, op1=ALU.mult), [xB, rstdB, C.constB], [outB[kc] if isinstance(outB, list) else outB])
            if kc % 2 == 1:
                yield


def ffn_phase(C, es0, gain, wgu_d, wd_d, final=None):
    P, nc, ps, pb = C.P, C.nc, C.ps, C.pb
    T = 1024
    NT = NTOK // T
    with ExitStack() as es:
        xt = [sb(es, nc, "xt%d" % i, [128, 8, T], F32) for i in range(2)]
        xtB = P.bufs_n(2, "xt")
        xn = sb(es, nc, "xn", [128, 8, T], BF16)
        xnB = P.bufs_n(8, "xn")
        h = sb(es, nc, "h", [128, NF, T], BF16)
        hB = P.bufs_n(NF, "h")
        wd = sb(es, nc, "wd", [128, NF, D], BF16)
        wdB = P.bufs_n(NF, "wd")
        NR = 4
        wg = [sb(es, nc, "wg%d" % i, [128, 2048], BF16) for i in range(NR)]
        wgB = P.bufs_n(NR, "wg")
        sl = [sb(es, nc, "sl%d" % i, [128, 512], F32) for i in range(2)]
        slB = P.bufs_n(2, "sl")
        nsq = sb(es, nc, "nsq", [128, 8, 512], BF16)
        nsqB = P.bufs_n(8, "nsq")
        C.rs = sb(es, nc, "rs", [128, T], F32)
        C.rstd = sb(es, nc, "rstd", [128, T], F32)
        C.rsB, C.rstdB = P.buf("rs"), P.buf("rstd")

        Xv = C.X.rearrange("c p n -> p c n")
        Ov = C.out.rearrange("c p n -> p c n")
        def load(t):
            P.dma("sp", xt[t % 2], Xv[:, :, t * T:(t + 1) * T], xtB[t % 2], C.XB)

        load(0)
        load(1)
        gi = 0
        for t in range(NT):
            x3, xB = xt[t % 2], xtB[t % 2]
            if t == 0:
                rmsnorm_fm_h(C, x3, xB, gain, xn, xnB, nsq, nsqB, T)
            for fi in range(NF):
                slot = gi % NR
                gi += 1
                P.dma("pool", wg[slot], wgu_d[fi], wgB[slot], None)
                if t == 0 and fi == NR - 1:
                    for fj in range(NF):
                        P.dma("pool", wd[:, fj, :], wd_d[fj], wdB[fj], None)
                for hf in range(2):
                    pg, pu = 2 * hf, 2 * hf + 1
                    cs = slice(hf * 512, (hf + 1) * 512)
                    for kc in range(8):
                        P.op("pe", lambda e, kc=kc, slot=slot, cs=cs, pg=pg: e.matmul(
                            ps[pg], lhsT=wg[slot][:, kc * 256:kc * 256 + 128], rhs=xn[:, kc, cs],
                            start=(kc == 0), stop=(kc == 7)), [wgB[slot], xnB[kc]], [pb[pg]])
                    for kc in range(8):
                        P.op("pe", lambda e, kc=kc, slot=slot, cs=cs, pu=pu: e.matmul(
                            ps[pu], lhsT=wg[slot][:, kc * 256 + 128:kc * 256 + 256], rhs=xn[:, kc, cs],
                            start=(kc == 0), stop=(kc == 7)), [wgB[slot], xnB[kc]], [pb[pu]])
                    P.op("act", lambda e, hf=hf, pg=pg: e.activation(out=sl[hf], in_=ps[pg], func=AF.Silu),
                         [pb[pg]], [slB[hf]])
                    P.op("dve", lambda e, hf=hf, pu=pu, fi=fi, cs=cs: e.tensor_tensor(
                        out=h[:, fi, cs], in0=sl[hf], in1=ps[pu], op=ALU.mult), [slB[hf], pb[pu]], [hB[fi]])
            def down_gen(x3=x3, xB=xB):
                k = 0
                for dc in range(8):
                    for hf in range(2):
                        po = 4 + (k % 2)
                        k += 1
                        cs = slice(hf * 512, (hf + 1) * 512)
                        for fi in range(NF):
                            P.op("pe", lambda e, fi=fi, dc=dc, cs=cs, po=po: e.matmul(
                                ps[po], lhsT=wd[:, fi, dc * 128:(dc + 1) * 128], rhs=h[:, fi, cs],
                                start=(fi == 0), stop=(fi == NF - 1)), [wdB[fi], hB[fi]], [pb[po]])
                        P.op("dve", lambda e, dc=dc, cs=cs, po=po, x3=x3: e.scalar_tensor_tensor(
                            out=x3[:, dc, cs], in0=ps[po], scalar=0.5, in1=x3[:, dc, cs], op0=ALU.mult, op1=ALU.add),
                            [pb[po], xB], [xB])
                        yield

            ngen = None
            if t + 1 < NT:
                ngen = rmsnorm_fm_g(C, xt[(t + 1) % 2], xtB[(t + 1) % 2], gain, xn, xnB, nsq, nsqB, T)
            interleave([down_gen(), ngen])
            if final is None:
                P.dma("sp", Xv[:, :, t * T:(t + 1) * T], x3, C.XB, xB, join=True)
            else:
                rmsnorm_fm_h(C, x3, xB, final, x3, xB, nsq, nsqB, T)
                P.dma("sp", Ov[:, :, t * T:(t + 1) * T], x3, C.outB, xB, join=True)
            if t + 2 < NT:
                load(t + 2)
        P.barrier()


def ab_phase(C, gain, D_):
    P, nc, ps, pb = C.P, C.nc, C.ps, C.pb
    T = 512
    NT = NTOK // T
    TPS = S // T
    QS = 128.0 ** -0.5
    with ExitStack() as es:
        A = lambda name, shape, dt: sb(es, nc, name, shape, dt)
        win = A("win", [128, 8, 3080], BF16)
        wout = A("wout", [128, 8, D], BF16)
        rgw = A("rgw", [128, 4, 2, 128], BF16)
        rgc = A("rgc", [128, 4, 8], F32)
        mlc = A("mlc", [128, 8, 5], F32)
        mlrow = A("mlrow", [128, 8 + 512], F32)
        cneg = A("cneg", [128, 4], F32)
        U = A("U", [128, 128], BF16)
        mneg = A("mneg", [128, 128], BF16)
        ident = A("ident", [128, 128], BF16)
        onesf = A("onesf", [128, 128], F32)
        zerof = A("zerof", [128, 128], F32)
        wB = P.buf("abw")
        for kc in range(8):
            P.dma("pool", win[:, kc, :], D_["ab_win"][:, kc, :], wB, None, join=True)
        P.dma("pool", wout, D_["ab_wout"], wB, None, join=True)
        P.dma("pool", rgw, D_["rgw"], wB, None, join=True)
        cB = P.buf("abc")
        P.dma("sp", rgc, D_["rgc"], cB, None, join=True)
        P.dma("sp", mlc, D_["mlc"], cB, None, join=True)
        P.dma("sp", mlrow, D_["mlrow"], cB, None, join=True)
        kB = P.buf("abk")
        P.op("pool", lambda e: e.memset(onesf, 1.0), [], [kB])
        P.op("pool", lambda e: e.memset(zerof, 0.0), [kB], [kB])
        P.op("pool", lambda e: e.affine_select(out=U, in_=onesf, pattern=[[1, 128]], compare_op=ALU.is_ge, fill=0.0,
                                              base=0, channel_multiplier=-1), [kB], [kB])
        P.op("pool", lambda e: e.affine_select(out=mneg, in_=zerof, pattern=[[1, 128]], compare_op=ALU.is_ge,
                                              fill=-30000.0, base=0, channel_multiplier=-1), [kB], [kB])
        P.op("pool", lambda e: e.affine_select(out=ident, in_=onesf, pattern=[[1, 128]], compare_op=ALU.is_equal,
                                              fill=0.0, base=0, channel_multiplier=-1), [kB], [kB])
        P.op("act", lambda e: e.activation(out=cneg, in_=rgc[:, :, 7], func=AF.Exp, scale=-1.0), [cB], [kB])
        P.op("act", lambda e: e.activation(out=cneg, in_=cneg, func=AF.Ln, bias=1.0), [kB], [kB])
        P.op("dve", lambda e: e.tensor_scalar(out=cneg, in0=cneg, scalar1=-8.0, scalar2=None, op0=ALU.mult), [kB], [kB])

        xt = A("xt", [128, 8, T], F32); xtB = P.buf("xt")
        xn = A("xn", [128, 8, T], BF16); xnB = P.bufs_n(8, "xn")
        yT = A("yT", [128, 8, T], BF16); yTB = P.bufs_n(8, "yT")
        C.rs = A("rs", [128, T], F32); C.rstd = A("rstd", [128, T], F32)
        C.rsB, C.rstdB = P.buf("rs"), P.buf("rstd")
        xa = A("xa", [128, 4, T + 3], F32); xaB = P.bufs_n(4, "xa")
        qk = A("qk", [128, 8, T + 3], F32); qkB = P.bufs_n(8, "qk")
        xc = A("xc", [128, T], F32); xcB = P.buf("xc")
        xcb = A("xcb", [128, T], BF16); xcbB = P.buf("xcb")
        rr = A("rr", [128, T], F32); rrB = P.buf("rr")
        ig = A("ig", [128, T], F32); igB = P.buf("ig")
        aa = A("aa", [128, T], F32); aaB = P.buf("aa")
        uu = A("uu", [128, T], F32); uuB = P.buf("uu")
        hs = A("hs", [128, T], F32); hsB = P.buf("hs")
        gg = A("gg", [128, T], F32); ggB = P.buf("gg")
        hst = A("hst", [128, 4], F32); hstB = P.bufs_n(4, "hst")
        qc = A("qc", [128, T], F32); qcB = P.buf("qc")
        qT = A("qT", [128, 4, T], BF16); qTB = P.bufs_n(4, "qT")
        kT = A("kT", [128, 4, T], BF16); kTB = P.bufs_n(4, "kT")
        va = A("va", [128, 4, 4, 129], BF16); vaB = P.bufs_n(4, "va")
        og = A("og", [128, 4, 512], F32); ogB = P.bufs_n(4, "og")
        gt = A("gt", [128, 4, 8], F32); gtB = P.bufs_n(4, "gt")
        lhi = A("lhi", [128, 4], BF16); llo = A("llo", [128, 4], BF16); lhf = A("lhf", [128, 4], F32)
        lB = P.buf("lhl")
        Lh = A("Lh", [128, 4, 128], BF16); Ll = A("Ll", [128, 4, 128], BF16); LB = P.buf("L")
        sm = A("sm", [128, 24], F32); smB = P.buf("sm")
        DT = A("DT", [128, 4, 128], F32); DTB = P.buf("DT")
        AT = A("AT", [128, 4, 128], BF16); ATB = P.buf("AT")
        kw = A("kw", [128, 4, 128], BF16); kwB = P.buf("kw")
        itr = A("itr", [128, 4, 129], F32); itrB = P.buf("itr")
        tot = A("tot", [128, 4, 129], F32); totB = P.buf("tot")
        hh = A("hh", [128, 4, 128], F32); hhB = P.buf("hh")
        h2 = A("h2", [128, 4, 128], F32); h2B = P.buf("h2")
        ytk = A("ytk", [128, 512], BF16); ytkB = P.buf("ytk")
        Cst = A("Cst", [128, 4, 129], F32); CstB = P.buf("Cst")
        Cbf = A("Cbf", [128, 4, 129], BF16); CbfB = P.buf("Cbf")
        P.op("pool", lambda e: e.memset(va, 1.0), [], vaB)
        Xv = C.X.rearrange("c p n -> p c n")
        bank_rr = [0]

        def nb():
            bank_rr[0] = (bank_rr[0] + 1) % 6
            return bank_rr[0]

        def proj_fm(col0, t, bank=None):
            b = nb() if bank is None else bank
            for kc in range(8):
                P.op("pe", lambda e, kc=kc, b=b: e.matmul(ps[b], lhsT=win[:, kc, col0:col0 + 128], rhs=xn[:, kc, :],
                                                          start=(kc == 0), stop=(kc == 7)), [wB, xnB[kc]], [pb[b]])
            return b

        for t in range(NT):
            first = (t % TPS == 0)
            P.dma("sp", xt, Xv[:, :, t * T:(t + 1) * T], xtB, C.XB)
            rmsnorm_fm(C, xt, xtB, gain, xn, xnB, yT, yTB, T)
            if first:
                P.op("pool", lambda e: e.memset(Cst, 0.0), [], [CstB])
                P.op("pool", lambda e: e.memset(Cbf, 0.0), [], [CbfB])
            for c in range(8):
                if first:
                    P.op("pool", lambda e, c=c: e.memset(qk[:, c, 0:3], 0.0), [], [qkB[c]])
                else:
                    P.op("pool", lambda e, c=c: e.tensor_copy(out=qk[:, c, 0:3], in_=qk[:, c, T:T + 3]), [qkB[c]], [qkB[c]])
                b = proj_fm(1024 + c * 128, t)
                P.op("act", lambda e, c=c, b=b: e.activation(out=qk[:, c, 3:T + 3], in_=ps[b], func=AF.Copy), [pb[b]], [qkB[c]])
                P.op("dve", lambda e, c=c: e.tensor_scalar(out=qc, in0=qk[:, c, 0:T], scalar1=mlc[:, c, 0:1],
                                                          scalar2=mlc[:, c, 4:5], op0=ALU.mult, op1=ALU.add), [qkB[c], cB], [qcB])
                for j in range(1, 4):
                    P.op("dve", lambda e, c=c, j=j: e.scalar_tensor_tensor(
                        out=qc, in0=qk[:, c, j:j + T], scalar=mlc[:, c, j:j + 1], in1=qc, op0=ALU.mult, op1=ALU.add),
                        [qkB[c], cB, qcB], [qcB])
                if c < 4:
                    P.op("act", lambda e: e.activation(out=qc, in_=qc, func=AF.Silu), [qcB], [qcB])
                    P.op("dve", lambda e, c=c: e.tensor_scalar(out=qT[:, c, :], in0=qc, scalar1=QS, scalar2=None, op0=ALU.mult),
                         [qcB], [qTB[c]])
                else:
                    P.op("act", lambda e, c=c: e.activation(out=kT[:, c - 4, :], in_=qc, func=AF.Silu), [qcB], [kTB[c - 4]])
            for j in range(4):
                ts_ = slice(j * 128, (j + 1) * 128)
                b = nb()
                for kc in range(8):
                    P.op("pe", lambda e, kc=kc, b=b, ts_=ts_: e.matmul(ps[b], lhsT=xn[:, kc, ts_], rhs=win[:, kc, 2048:2560],
                                                                     start=(kc == 0), stop=(kc == 7)), [wB, xnB[kc]], [pb[b]])
                P.op("act", lambda e, j=j, b=b: e.activation(out=va[:, j, :, 0:128], in_=ps[b].rearrange("p (h v) -> p h v", h=4),
                                                             func=AF.Copy), [pb[b]], [vaB[j]])
                b = nb()
                for kc in range(8):
                    P.op("pe", lambda e, kc=kc, b=b, ts_=ts_: e.matmul(ps[b], lhsT=xn[:, kc, ts_], rhs=win[:, kc, 2560:3072],
                                                                     start=(kc == 0), stop=(kc == 7)), [wB, xnB[kc]], [pb[b]])
                P.op("act", lambda e, j=j, b=b: e.activation(out=og[:, j, :], in_=ps[b], func=AF.Sigmoid), [pb[b]], [ogB[j]])
                b = nb()
                for kc in range(8):
                    P.op("pe", lambda e, kc=kc, b=b, ts_=ts_: e.matmul(ps[b][:, 0:8], lhsT=xn[:, kc, ts_], rhs=win[:, kc, 3072:3080],
                                                                     start=(kc == 0), stop=(kc == 7)), [wB, xnB[kc]], [pb[b]])
                P.op("dve", lambda e, j=j, b=b: e.tensor_tensor(out=gt[:, j, :], in0=ps[b][:, 0:8], in1=mlrow[:, 0:8], op=ALU.add),
                     [pb[b], cB], [gtB[j]])
                P.op("act", lambda e, j=j: e.activation(out=gt[:, j, 4:8], in_=gt[:, j, 4:8], func=AF.Exp, scale=-1.0), [gtB[j]], [gtB[j]])
                P.op("act", lambda e, j=j: e.activation(out=gt[:, j, 4:8], in_=gt[:, j, 4:8], func=AF.Ln, bias=1.0), [gtB[j]], [gtB[j]])
                P.op("dve", lambda e, j=j: e.tensor_scalar(out=gt[:, j, 4:8], in0=gt[:, j, 4:8], scalar1=-1.0, scalar2=None, op0=ALU.mult),
                     [gtB[j]], [gtB[j]])
            def rg_stream():
                for c in range(4):
                    if first:
                        P.op("pool", lambda e, c=c: e.memset(xa[:, c, 0:3], 0.0), [], [xaB[c]])
                    else:
                        P.op("pool", lambda e, c=c: e.tensor_copy(out=xa[:, c, 0:3], in_=xa[:, c, T:T + 3]), [xaB[c]], [xaB[c]])
                    b = proj_fm(c * 128, t, 5)
                    P.op("act", lambda e, c=c, b=b: e.activation(out=xa[:, c, 3:T + 3], in_=ps[b], func=AF.Copy), [pb[b]], [xaB[c]])
                    P.op("dve", lambda e, c=c: e.tensor_scalar(out=xc, in0=xa[:, c, 0:T], scalar1=rgc[:, c, 0:1],
                                                              scalar2=rgc[:, c, 4:5], op0=ALU.mult, op1=ALU.add), [xaB[c], cB], [xcB])
                    for j in range(1, 4):
                        P.op("dve", lambda e, c=c, j=j: e.scalar_tensor_tensor(
                            out=xc, in0=xa[:, c, j:j + T], scalar=rgc[:, c, j:j + 1], in1=xc, op0=ALU.mult, op1=ALU.add),
                            [xaB[c], cB, xcB], [xcB])
                    P.op("act", lambda e: e.activation(out=xcb, in_=xc, func=AF.Copy), [xcB], [xcbB])
                    yield
                    br = 5
                    P.op("pe", lambda e, c=c, br=br: e.matmul(ps[br], lhsT=rgw[:, c, 0, :], rhs=xcb, start=True, stop=True),
                         [wB, xcbB], [pb[br]])
                    P.op("act", lambda e, c=c, br=br: e.activation(out=rr, in_=ps[br], func=AF.Sigmoid, bias=rgc[:, c, 5:6]),
                         [pb[br], cB], [rrB])
                    yield
                    bi = 5
                    P.op("pe", lambda e, c=c, bi=bi: e.matmul(ps[bi], lhsT=rgw[:, c, 1, :], rhs=xcb, start=True, stop=True),
                         [wB, xcbB], [pb[bi]])
                    P.op("act", lambda e, c=c, bi=bi: e.activation(out=ig, in_=ps[bi], func=AF.Sigmoid, bias=rgc[:, c, 6:7]),
                         [pb[bi], cB], [igB])
                    P.op("act", lambda e, c=c: e.activation(out=aa, in_=rr, func=AF.Exp, scale=cneg[:, c:c + 1]), [rrB, kB], [aaB])
                    yield
                    P.op("act", lambda e: e.activation(out=rr, in_=aa, func=AF.Square), [aaB], [rrB])
                    P.op("act", lambda e: e.activation(out=rr, in_=rr, func=AF.Sqrt, scale=-1.0, bias=onesf[:, 0:1]), [rrB, kB], [rrB])
                    P.op("dve", lambda e: e.tensor_tensor(out=uu, in0=ig, in1=xc, op=ALU.mult), [igB, xcB], [uuB])
                    P.op("dve", lambda e: e.tensor_tensor(out=uu, in0=uu, in1=rr, op=ALU.mult), [uuB, rrB], [uuB])
                    yield
                    if first:
                        P.op("dve", lambda e: e.tensor_tensor_scan(out=hs, data0=aa, data1=uu, initial=0.0, op0=ALU.mult, op1=ALU.add),
                             [aaB, uuB], [hsB])
                    else:
                        P.op("dve", lambda e, c=c: e.tensor_tensor_scan(out=hs, data0=aa, data1=uu, initial=hst[:, c:c + 1],
                                                                       op0=ALU.mult, op1=ALU.add), [aaB, uuB, hstB[c]], [hsB])
                    P.op("dve", lambda e, c=c: e.tensor_copy(out=hst[:, c:c + 1], in_=hs[:, T - 1:T]), [hsB], [hstB[c]])
                    yield
                    bg = proj_fm(512 + c * 128, t, 5)
                    P.op("act", lambda e, bg=bg: e.activation(out=gg, in_=ps[bg], func=AF.Gelu_apprx_tanh), [pb[bg]], [ggB])
                    P.op("dve", lambda e, c=c: e.tensor_tensor(out=yT[:, c, :], in0=gg, in1=hs, op=ALU.mult), [ggB, hsB], [yTB[c]])
                    yield
            def ml_stream():
                for j in range(4):
                    ts_ = slice(j * 128, (j + 1) * 128)
                    logi = gt[:, j, 0:4]
                    logf = gt[:, j, 4:8]
                    P.op("dve", lambda e, logf=logf: e.tensor_copy(out=lhi, in_=logf), [gtB[j]], [lB])
                    P.op("dve", lambda e: e.tensor_copy(out=lhf, in_=lhi), [lB], [lB])
                    P.op("dve", lambda e, logf=logf: e.tensor_tensor(out=lhf, in0=logf, in1=lhf, op=ALU.subtract), [gtB[j], lB], [lB])
                    P.op("dve", lambda e: e.tensor_copy(out=llo, in_=lhf), [lB], [lB])
                    P.op("dve", lambda e: e.tensor_copy(out=Lh, in_=lhi.unsqueeze(2).broadcast_to([128, 4, 128])), [lB], [LB])
                    P.op("dve", lambda e: e.tensor_copy(out=Ll, in_=llo.unsqueeze(2).broadcast_to([128, 4, 128])), [lB, LB], [LB])
                    P.op("pe", lambda e: e.matmul(ps[7][:, 264:268], lhsT=U, rhs=lhi, start=True, stop=False), [kB, lB], [pb[7]])
                    P.op("pe", lambda e: e.matmul(ps[7][:, 264:268], lhsT=U, rhs=llo, start=False, stop=True), [kB, lB], [pb[7]])
                    P.op("pe", lambda e: e.matmul(ps[7][:, 268:272], lhsT=C.ones_bf, rhs=lhi, start=True, stop=False), [kB, lB], [pb[7]])
                    P.op("pe", lambda e: e.matmul(ps[7][:, 268:272], lhsT=C.ones_bf, rhs=llo, start=False, stop=True), [kB, lB], [pb[7]])
                    for hd in range(4):
                        o_ = ps[0][:, hd * 128:(hd + 1) * 128]
                        P.op("pe", lambda e, hd=hd, o_=o_: e.matmul(o_, lhsT=Lh[:, hd, :], rhs=U, start=True, stop=False), [LB, kB], [pb[0]])
                        P.op("pe", lambda e, hd=hd, o_=o_: e.matmul(o_, lhsT=Ll[:, hd, :], rhs=U, start=False, stop=False), [LB, kB], [pb[0]])
                        P.op("pe", lambda e, hd=hd, o_=o_: e.matmul(o_, lhsT=ident, rhs=mneg, start=False, stop=True), [kB], [pb[0]])
                        P.op("pe", lambda e, hd=hd, ts_=ts_: e.matmul(ps[1][:, hd * 128:(hd + 1) * 128], lhsT=kT[:, hd, ts_], rhs=qT[:, hd, ts_],
                                                                     start=True, stop=True), [kTB[hd], qTB[hd]], [pb[1]])
                    yield
                    bcol = ps[7][:, 264:268]
                    gcol = ps[7][:, 268:272]
                    P.op("dve", lambda e, logi=logi, bcol=bcol: e.tensor_tensor(out=sm[:, 0:4], in0=logi, in1=bcol, op=ALU.subtract),
                         [gtB[j], pb[7]], [smB])
                    P.op("dve", lambda e, gcol=gcol: e.tensor_tensor(out=sm[:, 8:12], in0=sm[:, 0:4], in1=gcol, op=ALU.add),
                         [smB, pb[7]], [smB])
                    P.op("act", lambda e, bcol=bcol: e.activation(out=sm[:, 4:8], in_=bcol, func=AF.Exp), [pb[7], smB], [smB])
                    P.op("act", lambda e: e.activation(out=sm[:, 8:12], in_=sm[:, 8:12], func=AF.Exp), [smB], [smB])
                    P.op("act", lambda e, gcol=gcol: e.activation(out=sm[:, 12:16], in_=gcol, func=AF.Exp), [pb[7], smB], [smB])
                    yield
                    for hd in range(4):
                        P.op("act", lambda e, hd=hd: e.activation(out=DT[:, hd, :], in_=ps[0][:, hd * 128:(hd + 1) * 128], func=AF.Exp,
                                                                  bias=sm[:, hd:hd + 1]), [pb[0], smB], [DTB])
                    P.op("dve", lambda e: e.tensor_tensor(out=AT, in0=DT, in1=ps[1].rearrange("p (h k) -> p h k", h=4), op=ALU.mult),
                         [DTB, pb[1]], [ATB])
                    yield
                    for hd in range(4):
                        bi_ = 2 + hd // 2
                        cs = slice((hd % 2) * 129, (hd % 2) * 129 + 129)
                        P.op("pe", lambda e, hd=hd, bi_=bi_, cs=cs, ts_=ts_: e.matmul(ps[bi_][:, cs], lhsT=qT[:, hd, ts_], rhs=Cbf[:, hd, :],
                                                                                    start=True, stop=True), [qTB[hd], CbfB], [pb[bi_]])
                    for hp in range(2):
                        for hd in (2 * hp, 2 * hp + 1):
                            cs = slice((hd % 2) * 129, (hd % 2) * 129 + 129)
                            P.op("pe", lambda e, hd=hd, cs=cs, j=j: e.matmul(ps[4][:, cs], lhsT=AT[:, hd, :], rhs=va[:, j, hd, :],
                                                                           start=True, stop=True), [ATB, vaB[j]], [pb[4]])
                        P.op("act", lambda e, hp=hp: e.activation(out=itr[:, 2 * hp:2 * hp + 2, :],
                                                                  in_=ps[4][:, 0:258].rearrange("p (h v) -> p h v", h=2), func=AF.Copy),
                             [pb[4]], [itrB])
                    yield
                    for hd in range(4):
                        bi_ = 2 + hd // 2
                        cs = slice((hd % 2) * 129, (hd % 2) * 129 + 129)
                        P.op("dve", lambda e, hd=hd, bi_=bi_, cs=cs: e.scalar_tensor_tensor(
                            out=tot[:, hd, :], in0=ps[bi_][:, cs], scalar=sm[:, 4 + hd:5 + hd], in1=itr[:, hd, :], op0=ALU.mult, op1=ALU.add),
                            [pb[bi_], smB, itrB], [totB])
                    yield
                    P.op("dve", lambda e: e.tensor_scalar(out=sm[:, 16:20], in0=tot[:, :, 128], scalar1=-1.0, scalar2=1.0,
                                                          op0=ALU.mult, op1=ALU.max), [totB, smB], [smB])
                    P.op("dve", lambda e: e.tensor_tensor(out=sm[:, 16:20], in0=sm[:, 16:20], in1=tot[:, :, 128], op=ALU.max),
                         [totB, smB], [smB])
                    P.op("dve", lambda e: e.reciprocal(out=sm[:, 16:20], in_=sm[:, 16:20]), [smB], [smB])
                    P.op("dve", lambda e: e.tensor_tensor(out=hh, in0=tot[:, :, 0:128],
                                                          in1=sm[:, 16:20].unsqueeze(2).broadcast_to([128, 4, 128]), op=ALU.mult),
                         [totB, smB], [hhB])
                    P.op("dve", lambda e: e.tensor_tensor(out=h2, in0=hh, in1=hh, op=ALU.mult), [hhB], [h2B])
                    P.op("dve", lambda e: e.tensor_reduce(out=sm[:, 20:24], in_=h2, axis=AX.X, op=ALU.add), [h2B, smB], [smB])
                    P.op("act", lambda e: e.activation(out=sm[:, 20:24], in_=sm[:, 20:24], func=AF.Sqrt, scale=1.0 / 128, bias=C.eps_t[:, 0:1]),
                         [smB], [smB])
                    P.op("dve", lambda e: e.reciprocal(out=sm[:, 20:24], in_=sm[:, 20:24]), [smB], [smB])
                    P.op("dve", lambda e: e.tensor_tensor(out=hh, in0=hh, in1=sm[:, 20:24].unsqueeze(2).broadcast_to([128, 4, 128]),
                                                          op=ALU.mult), [hhB, smB], [hhB])
                    P.op("dve", lambda e: e.tensor_tensor(out=hh, in0=hh, in1=mlrow[:, 8:520].rearrange("p (h v) -> p h v", h=4),
                                                          op=ALU.mult), [hhB, cB], [hhB])
                    P.op("dve", lambda e, j=j: e.tensor_tensor(out=ytk.rearrange("p (h v) -> p h v", h=4), in0=hh,
                                                               in1=og[:, j, :].rearrange("p (h v) -> p h v", h=4), op=ALU.mult),
                         [hhB, ogB[j]], [ytkB])
                    yield
                    p6b = ps[6].bitcast(BF16)
                    for hd in range(4):
                        P.op("pe", lambda e, hd=hd, p6b=p6b: e.transpose(out=p6b[:, hd * 128:(hd + 1) * 128], in_=ytk[:, hd * 128:(hd + 1) * 128],
                                                                       identity=ident), [ytkB, kB], [pb[6]])
                    for hd in range(4):
                        P.op("act", lambda e, hd=hd, p6b=p6b, ts_=ts_: e.activation(out=yT[:, 4 + hd, ts_], in_=p6b[:, hd * 128:(hd + 1) * 128],
                                                                                  func=AF.Copy), [pb[6]], [yTB[4 + hd]])
                    yield
                    p7b = ps[7].bitcast(BF16)
                    for hd in range(4):
                        P.op("pe", lambda e, hd=hd, p7b=p7b, ts_=ts_: e.transpose(out=p7b[:, 512 + hd * 128:512 + (hd + 1) * 128],
                                                                                in_=kT[:, hd, ts_], identity=ident), [kTB[hd], kB], [pb[7]])
                    for hd in range(4):
                        P.op("dve", lambda e, hd=hd, p7b=p7b: e.tensor_scalar(out=kw[:, hd, :], in0=p7b[:, 512 + hd * 128:512 + (hd + 1) * 128],
                                                                            scalar1=sm[:, 8 + hd:9 + hd], scalar2=None, op0=ALU.mult),
                             [pb[7], smB], [kwB])
                    for hd in range(4):
                        bs_ = 2 + hd // 2
                        cs = slice((hd % 2) * 129, (hd % 2) * 129 + 129)
                        P.op("pe", lambda e, hd=hd, bs_=bs_, cs=cs, j=j: e.matmul(ps[bs_][:, cs], lhsT=kw[:, hd, :], rhs=va[:, j, hd, :],
                                                                                start=True, stop=True), [kwB, vaB[j]], [pb[bs_]])
                    for hd in range(4):
                        bs_ = 2 + hd // 2
                        cs = slice((hd % 2) * 129, (hd % 2) * 129 + 129)
                        P.op("dve", lambda e, hd=hd, bs_=bs_, cs=cs: e.scalar_tensor_tensor(
                            out=Cst[:, hd, :], in0=Cst[:, hd, :], scalar=sm[:, 12 + hd:13 + hd], in1=ps[bs_][:, cs], op0=ALU.mult, op1=ALU.add),
                            [CstB, smB, pb[bs_]], [CstB])
                    P.op("act", lambda e: e.activation(out=Cbf, in_=Cst, func=AF.Copy), [CstB], [CbfB])
                    yield
            interleave([rg_stream(), ml_stream()])
            for dc in range(8):
                b = nb()
                for yc in range(8):
                    P.op("pe", lambda e, yc=yc, dc=dc, b=b: e.matmul(ps[b], lhsT=wout[:, yc, dc * 128:(dc + 1) * 128], rhs=yT[:, yc, :],
                                                                   start=(yc == 0), stop=(yc == 7)), [wB, yTB[yc]], [pb[b]])
                P.op("dve", lambda e, dc=dc, b=b: e.tensor_tensor(out=xt[:, dc, :], in0=xt[:, dc, :], in1=ps[b], op=ALU.add),
                     [xtB, pb[b]], [xtB])
            P.dma("sp", Xv[:, :, t * T:(t + 1) * T], xt, C.XB, xtB, join=True)
        P.barrier()


def final_phase(C, gain):
    P, nc = C.P, C.nc
    T = 512
    with ExitStack() as es:
        xt = sb(es, nc, "fxt", [128, 8, T], F32); xtB = P.buf("fxt")
        sq = sb(es, nc, "fsq", [128, 8, T], BF16); sqB = P.bufs_n(8, "fsq")
        C.rs = sb(es, nc, "rs", [128, T], F32); C.rstd = sb(es, nc, "rstd", [128, T], F32)
        C.rsB, C.rstdB = P.buf("rs"), P.buf("rstd")
        Xv = C.X.rearrange("c p n -> p c n")
        Ov = C.out.rearrange("c p n -> p c n")
        for t in range(NTOK // T):
            P.dma("sp", xt, Xv[:, :, t * T:(t + 1) * T], xtB, C.XB)
            rmsnorm_fm(C, xt, xtB, gain, xt, xtB, sq, sqB, T)
            P.dma("sp", Ov[:, :, t * T:(t + 1) * T], xt, C.outB, xtB, join=True)
        P.barrier()


NCOLN = 2608


def interleave(gens):
    gens = [g for g in gens if g is not None]
    while gens:
        for g in list(gens):
            try:
                next(g)
            except StopIteration:
                gens.remove(g)


def nsa_proj_phase(C, gain, D_):
    P, nc, ps, pb = C.P, C.nc, C.ps, C.pb
    T = 512
    NT = NTOK // T
    TPS = S // T
    with ExitStack() as es:
        A = lambda name, shape, dt: sb(es, nc, name, shape, dt)
        win = A("nwin", [128, 8, NCOLN], BF16)
        wB = P.buf("nw")
        for kc in range(8):
            P.dma("pool", win[:, kc, :], D_["nsa_win"][:, kc, :], wB, None, join=True)
        bg = A("bg", [128, 48], F32)
        cB = P.buf("nc")
        P.dma("sp", bg, D_["bgrow"], cB, None)
        xt = [A("xt%d" % i, [128, 8, T], F32) for i in range(2)]; xtB = P.bufs_n(2, "xt")
        xn2 = [A("xn%d" % i, [128, 8, T], BF16) for i in range(2)]; xn2B = [P.bufs_n(8, "xn%d_" % i) for i in range(2)]
        sq2 = [A("sq%d" % i, [128, 8, T], BF16) for i in range(2)]; sq2B = [P.bufs_n(8, "sq%d_" % i) for i in range(2)]
        C.rs = A("rs", [128, T], F32); C.rstd = A("rstd", [128, T], F32)
        C.rsB, C.rstdB = P.buf("rs"), P.buf("rstd")
        stg = [A("stg%d" % i, [128, 16, T], BF16) for i in range(2)]; stgB = [P.bufs_n(5, "stg%d_" % i) for i in range(2)]
        vtk = [A("vtk%d" % i, [128, 4, 512], BF16) for i in range(2)]; vtkB = P.bufs_n(2, "vtk")
        gtk = [A("gtk%d" % i, [128, 4, 48], F32) for i in range(2)]; gtkB = P.bufs_n(2, "gtk")
        Xv = C.X.rearrange("c p n -> p c n")
        k = 0
        groups = [("Qs", 0, 8, 0.125, 0), ("KsT", 8, 2, 1.0, 1024), ("KwT", 10, 2, 1.0, 1280), ("KcT", 12, 2, 1.0, 1536),
                  ("VcT", 14, 2, 1.0, 1792)]
        P.dma("sp", xt[0], Xv[:, :, 0:T], xtB[0], C.XB)
        for t in range(NT):
            sq_, t0 = t // TPS, (t % TPS) * T
            pr = t % 2
            if t + 1 < NT:
                P.dma("sp", xt[(t + 1) % 2], Xv[:, :, (t + 1) * T:(t + 2) * T], xtB[(t + 1) % 2], C.XB)
            xn, xnB, sq, sqB = xn2[pr], xn2B[pr], sq2[pr], sq2B[pr]
            if t == 0:
                rmsnorm_fm(C, xt[pr], xtB[pr], gain, xn, xnB, sq, sqB, T)
            for gi, (nm, c0, ncnk, scl, cb) in enumerate(groups):
                if gi == 1 and t + 1 < NT:
                    pn = (t + 1) % 2
                    rmsnorm_fm(C, xt[pn], xtB[pn], gain, xn2[pn], xn2B[pn], sq2[pn], sq2B[pn], T)
                for cc in range(ncnk):
                    b = k % 6
                    k += 1
                    col0 = cb + cc * 128
                    for kc in range(8):
                        P.op("pe", lambda e, kc=kc, b=b, col0=col0, xn=xn: e.matmul(ps[b], lhsT=win[:, kc, col0:col0 + 128], rhs=xn[:, kc, :],
                                                                          start=(kc == 0), stop=(kc == 7)), [wB, xnB[kc]], [pb[b]])
                    if (c0 + cc) % 2 == 0:
                        P.op("act", lambda e, b=b, c=c0 + cc, scl=scl, pr=pr: e.activation(out=stg[pr][:, c, :], in_=ps[b], func=AF.Copy, scale=scl),
                             [pb[b]], [stgB[pr][gi]])
                    else:
                        P.op("dve", lambda e, b=b, c=c0 + cc, scl=scl, pr=pr: e.tensor_scalar(out=stg[pr][:, c, :], in0=ps[b], scalar1=scl,
                                                                                          scalar2=None, op0=ALU.mult), [pb[b]], [stgB[pr][gi]])
                if nm in ("Qs", "KsT", "KwT"):
                    nh = 2 * ncnk
                    P.dma("sp", D_[nm][sq_, :, 0:nh:2, t0:t0 + T], stg[pr][0:64, c0:c0 + ncnk, :], D_[nm + "B"], stgB[pr][gi], join=True)
                    P.dma("sp", D_[nm][sq_, :, 1:nh:2, t0:t0 + T], stg[pr][64:128, c0:c0 + ncnk, :], D_[nm + "B"], stgB[pr][gi], join=True)
                else:
                    P.dma("sp", D_[nm][sq_, :, :, t0:t0 + T], stg[pr][:, c0:c0 + ncnk, :], D_[nm + "B"], stgB[pr][gi], join=True)
            for j in range(4):
                ts_ = slice(j * 128, (j + 1) * 128)
                b = k % 6
                k += 1
                for kc in range(8):
                    P.op("pe", lambda e, kc=kc, b=b, ts_=ts_, xn=xn: e.matmul(ps[b], lhsT=xn[:, kc, ts_], rhs=win[:, kc, 2048:2560],
                                                                     start=(kc == 0), stop=(kc == 7)), [wB, xnB[kc]], [pb[b]])
                P.op("act", lambda e, b=b, j=j, pr=pr: e.activation(out=vtk[pr][:, j, :], in_=ps[b], func=AF.Copy), [pb[b]], [vtkB[pr]])
                b = k % 6
                k += 1
                for kc in range(8):
                    P.op("pe", lambda e, kc=kc, b=b, ts_=ts_, xn=xn: e.matmul(ps[b][:, 0:48], lhsT=xn[:, kc, ts_], rhs=win[:, kc, 2560:2608],
                                                                     start=(kc == 0), stop=(kc == 7)), [wB, xnB[kc]], [pb[b]])
                P.op("dve", lambda e, b=b, j=j, pr=pr: e.tensor_tensor(out=gtk[pr][:, j, :], in0=ps[b][:, 0:48], in1=bg, op=ALU.add),
                     [pb[b], cB], [gtkB[pr]])
            P.op("act", lambda e, pr=pr: e.activation(out=gtk[pr], in_=gtk[pr], func=AF.Sigmoid), [gtkB[pr]], [gtkB[pr]])
            P.dma("sp", D_["Vs"][sq_, t0:t0 + T, :].rearrange("(j p) c -> p j c", p=128), vtk[pr][:, :, 0:256], D_["VsB"], vtkB[pr], join=True)
            P.dma("sp", D_["Vw"][sq_, t0:t0 + T, :].rearrange("(j p) c -> p j c", p=128), vtk[pr][:, :, 256:512], D_["VwB"], vtkB[pr], join=True)
            P.dma("sp", D_["G"][sq_, t0:t0 + T, :].rearrange("(j p) c -> p j c", p=128), gtk[pr], D_["GB"], gtkB[pr], join=True)
        P.barrier()


def nsa_core_phase(C, D_):
    P, nc, ps, pb = C.P, C.nc, C.ps, C.pb
    NQ = S // 128
    NEG = -30000.0
    with ExitStack() as es:
        A = lambda name, shape, dt: sb(es, nc, name, shape, dt)
        wout = A("nwout", [128, 8, D], BF16)
        w1 = A("w1", [128, 2, 32, 256], BF16)
        w2k = A("w2k", [128, 2, 64], BF16)
        w2v = A("w2v", [128, 2, 64], BF16)
        peT = A("peT", [128, 2, 32], BF16)
        b1 = A("b1", [128, 2, 2], F32)
        selA = A("selA", [128, 16, 32], F32)
        selB = A("selB", [128, 16, 32], F32)
        wB = P.buf("n2w")
        P.dma("pool", wout, D_["nsa_wout"], wB, None, join=True)
        P.dma("pool", w1, D_["w1d"], wB, None, join=True)
        P.dma("pool", w2k, D_["w2kd"], wB, None, join=True)
        P.dma("pool", w2v, D_["w2vd"], wB, None, join=True)
        P.dma("pool", peT, D_["peTd"], wB, None, join=True)
        cB = P.buf("n2c")
        P.dma("sp", b1, D_["b1d"], cB, None, join=True)
        P.dma("sp", selA, D_["selA"], cB, None, join=True)
        P.dma("sp", selB, D_["selB"], cB, None, join=True)
        kB = P.buf("n2k")
        onesf = A("onesf", [128, 2048], F32)
        zerof = A("zerof", [128, 2048], F32)
        ident = A("ident", [128, 128], BF16)
        mneg = A("mneg", [128, 128], BF16)
        mneg2 = A("mneg2", [128, 128], BF16)
        cmask = A("cmask", [128, 16, 128], BF16)
        E = A("E", [32, 16, 128], BF16)
        E0 = A("E0", [32, 16, 128], F32)
        P.op("pool", lambda e: e.memset(onesf, 1.0), [], [kB])
        P.op("pool", lambda e: e.memset(zerof, 0.0), [kB], [kB])
        P.op("pool", lambda e: e.affine_select(out=ident, in_=onesf[:, 0:128], pattern=[[1, 128]], compare_op=ALU.is_equal, fill=0.0,
                                              base=0, channel_multiplier=-1), [kB], [kB])
        P.op("pool", lambda e: e.affine_select(out=mneg, in_=zerof[:, 0:128], pattern=[[1, 128]], compare_op=ALU.is_ge, fill=NEG,
                                              base=0, channel_multiplier=-1), [kB], [kB])
        P.op("pool", lambda e: e.affine_select(out=mneg2, in_=zerof[:, 0:128], pattern=[[-1, 128]], compare_op=ALU.is_ge, fill=NEG,
                                              base=-1, channel_multiplier=1), [kB], [kB])
        for i in range(16):
            P.op("pool", lambda e, i=i: e.affine_select(out=cmask[:, i, :], in_=zerof[:, 0:128], pattern=[[1, 128]], compare_op=ALU.is_ge,
                                                       fill=NEG, base=128 * i - 31, channel_multiplier=-16), [kB], [kB])
        P.op("pool", lambda e: e.affine_select(out=E0, in_=onesf[0:32, :].rearrange("p (a b) -> p a b", a=16), pattern=[[128, 16], [1, 128]],
                                              compare_op=ALU.is_ge, fill=0.0, base=0, channel_multiplier=-64), [kB], [kB])
        P.op("pool", lambda e: e.affine_select(out=E, in_=E0, pattern=[[-128, 16], [-1, 128]], compare_op=ALU.is_ge, fill=0.0,
                                              base=63, channel_multiplier=64), [kB], [kB])

        sqB_ = P.buf("seqdata")
        ksT = A("ksT", [128, 4, S], BF16)
        kwT = A("kwT", [128, 4, S], BF16)
        kcT = A("kcT", [128, 2, S], BF16)
        vcT = A("vcT", [128, 2, S], BF16)
        vsa = A("vsa", [128, 16, 4, 65], BF16)
        vwa = A("vwa", [128, 16, 4, 65], BF16)
        gts = A("gts", [128, 16, 48], F32)
        hT = A("hT", [128, 2, 128], BF16); hTB = P.buf("hT")
        btot = A("btot", [128, 2, 2], F32); btB = P.buf("btot")
        kcmp = A("kcmp", [128, 4, 128], BF16); kcmpB = P.buf("kcmp")
        P.op("dve", lambda e: e.memset(kcmp, 0.0), [], [kcmpB])
        vcmp = A("vcmp", [128, 4, 97], BF16); vcmpB = P.buf("vcmp")
        qt = [A("qt%d" % i, [128, 16, 128], BF16) for i in range(2)]; qtB = P.bufs_n(2, "qt")
        selT = [A("selT%d" % i, [128, 4, 128], BF16) for i in range(2)]; selTB = P.bufs_n(2, "selT")
        E128 = A("E128", [128, 16, 128], BF16)
        P.op("dve", lambda e: e.memset(E128, 0.0), [], [kB])
        for i_ in range(2):
            P.op("dve", lambda e, i_=i_: e.memset(qt[i_], 0.0), [], [qtB[i_]])
            P.op("dve", lambda e, i_=i_: e.memset(selT[i_], 0.0), [], [selTB[i_]])
        P.op("dve", lambda e: e.tensor_copy(out=E128[0:32], in_=E), [kB], [kB])
        P.op("dve", lambda e: e.memset(ksT, 0.0), [], [sqB_])
        P.op("dve", lambda e: e.memset(kwT, 0.0), [sqB_], [sqB_])
        ocs = [A("ocs%d" % i, [128, 4, 4, 64], F32) for i in range(2)]; ocsB = P.bufs_n(2, "ocs")
        xt2 = [A("xt2_%d" % i, [128, 8, 128], F32) for i in range(2)]; xt2B = P.bufs_n(2, "xt2")
        pT = [A("pT%d" % i, [128, 4, 128], BF16) for i in range(2)]; pTB = P.bufs_n(2, "pT")
        pTa = A("pTa", [128, 4, 128], BF16); pTaB = P.buf("pTa")
        sc4 = A("sc4", [128, 4, 32], F32); sc4B = P.buf("sc4")
        scr = A("scr", [128, 32], F32); scrB = P.buf("scr")
        top8 = A("top8", [128, 8], F32)
        seln = A("seln", [128, 32], BF16); selnB = P.buf("seln")
        zza = A("zza", [128, 4], F32); zzaB = P.buf("zza")
        zz = A("zz", [128, 2, 4], F32); zzB = P.buf("zz")
        otk = A("otk", [128, 1024], F32); otkB = P.buf("otk")
        tmp = A("tmpo", [128, 4, 64], F32); tmpB = P.buf("tmpo")
        tmp2 = A("tmpo2", [128, 4, 64], F32); tmp2B = P.buf("tmpo2")
        otb = A("otb", [128, 1024], BF16); otbB = P.buf("otb")
        oT = A("oT", [128, 8, 128], BF16); oTB = P.buf("oT")
        P.op("dve", lambda e: e.memset(vsa, 1.0), [], [sqB_])
        P.op("dve", lambda e: e.memset(vwa, 1.0), [sqB_], [sqB_])
        P.op("dve", lambda e: e.memset(vcmp, 1.0), [], [vcmpB])
        Xv = C.X.rearrange("c p n -> p c n")
        p6b = ps[6].bitcast(BF16)
        p7b = ps[7].bitcast(BF16)

        def stage_a(sq_, i):
            p = i % 2
            P.dma("sp", qt[p][0:64], D_["Qs"][sq_, :, :, i * 128:(i + 1) * 128], qtB[p], D_["QsB"])
            for g in range(4):
                P.op("pe", lambda e, g=g, p=p: e.matmul(ps[4][0:127, :], lhsT=kcmp[:, g, 0:127], rhs=qt[p][:, 4 * g:4 * g + 4, :],
                                                       start=True, stop=False), [kcmpB, qtB[p]], [pb[4]])
                P.op("pe", lambda e, i=i: e.matmul(ps[4][0:127, :], lhsT=ident[0:127, 0:127],
                                                   rhs=cmask[0:127, i, :].unsqueeze(1).broadcast_to([127, 4, 128]), start=False, stop=True),
                     [kB], [pb[4]])
                P.op("act", lambda e: e.activation(out=pTa[0:127], in_=ps[4][0:127, :].rearrange("p (r q) -> p r q", r=4), func=AF.Exp),
                     [pb[4]], [pTaB])
                yield
                for r in range(4):
                    P.op("pe", lambda e, r=r, g=g: e.matmul(ps[5][:, r * 97:(r + 1) * 97], lhsT=pTa[0:127, r, :], rhs=vcmp[0:127, g, :],
                                                           start=(r == 0), stop=(r == 3), skip_group_check=True), [pTaB, vcmpB], [pb[5]])
                yield
                c4 = ps[5][:, 0:388].rearrange("p (r c) -> p r c", r=4)
                P.op("dve", lambda e, c4=c4: e.tensor_scalar(out=zza, in0=c4[:, :, 96], scalar1=1e-30, scalar2=None, op0=ALU.max), [pb[5]], [zzaB])
                P.op("dve", lambda e: e.reciprocal(out=zza, in_=zza), [zzaB], [zzaB])
                P.op("dve", lambda e, c4=c4: e.tensor_tensor(out=sc4, in0=c4[:, :, 0:32], in1=zza.unsqueeze(2).broadcast_to([128, 4, 32]),
                                                           op=ALU.mult), [pb[5], zzaB], [sc4B])
                P.op("dve", lambda e: e.tensor_reduce(out=scr, in_=sc4.rearrange("p r j -> p j r"), axis=AX.X, op=ALU.add), [sc4B], [scrB])
                yield
                P.op("dve", lambda e, i=i: e.tensor_tensor(out=scr, in0=scr, in1=selA[:, i, :], op=ALU.mult), [scrB, cB], [scrB])
                P.op("dve", lambda e, i=i: e.tensor_tensor(out=scr, in0=scr, in1=selB[:, i, :], op=ALU.add), [scrB, cB], [scrB])
                P.op("dve", lambda e: e.max(out=top8, in_=scr), [scrB], [scrB])
                P.op("dve", lambda e: e.tensor_scalar(out=seln, in0=scr, scalar1=top8[:, 7:8], scalar2=1.0, op0=ALU.is_ge, op1=ALU.subtract),
                     [scrB], [selnB])
                yield
                P.op("pe", lambda e: e.transpose(out=p7b[0:32, 0:128], in_=seln, identity=ident), [selnB, kB], [pb[7]])
                gv = gts[:, i, g * 12:(g + 1) * 12].rearrange("p (r b) -> p b r", b=3)
                P.op("dve", lambda e, gv=gv: e.tensor_tensor(out=zza, in0=zza, in1=gv[:, 0, :], op=ALU.mult), [zzaB, sqB_], [zzaB])
                P.op("dve", lambda e, c4=c4, g=g, p=p: e.tensor_tensor(out=ocs[p][:, g], in0=c4[:, :, 32:96],
                                                                     in1=zza.unsqueeze(2).broadcast_to([128, 4, 64]), op=ALU.mult),
                     [pb[5], zzaB], [ocsB[p]])
                P.op("act", lambda e, g=g, p=p: e.activation(out=selT[p][0:32, g, :], in_=p7b[0:32, 0:128], func=AF.Copy, scale=-NEG),
                     [pb[7]], [selTB[p]])
                yield

        def stage_b(sq_, i):
            p = i % 2
            vcount = [0]
            for g in range(4):
                q4 = qt[p][:, 4 * g:4 * g + 4, :]
                visits = [("sel", kt) for kt in range(i + 1)] + [("win", kt) for kt in (i - 2, i - 1, i) if kt >= 0]
                firstwin = min(kt for br, kt in visits if br == "win")

                def issue_scores(br, kt):
                    pi = vcount[0] % 2
                    vcount[0] += 1
                    ks_ = slice(kt * 128, (kt + 1) * 128)
                    kk = ksT if br == "sel" else kwT
                    extra = []
                    if br == "sel":
                        extra.append((E128[:, kt, :], selT[p][:, g, :], selTB[p]))
                    if kt == i:
                        extra.append((ident, mneg, kB))
                    if br == "win" and kt == i - 2:
                        extra.append((ident, mneg2, kB))
                    P.op("pe", lambda e, pi=pi, kk=kk, ks_=ks_, g=g, q4=q4, ne=len(extra): e.matmul(
                        ps[pi], lhsT=kk[:, g, ks_], rhs=q4, start=True, stop=(ne == 0)), [sqB_, qtB[p]], [pb[pi]])
                    for ei, (ml, mr, mB) in enumerate(extra):
                        P.op("pe", lambda e, pi=pi, ml=ml, mr=mr, last=(ei == len(extra) - 1): e.matmul(
                            ps[pi], lhsT=ml, rhs=mr.unsqueeze(1).broadcast_to([mr.shape[0], 4, 128]), start=False, stop=last), [kB, mB], [pb[pi]])
                    P.op("act", lambda e, pi=pi: e.activation(out=pT[pi], in_=ps[pi].rearrange("p (r q) -> p r q", r=4), func=AF.Exp),
                         [pb[pi]], [pTB[pi]])
                    return pi

                def issue_pv(br, kt, pi):
                    ob = 2 if br == "sel" else 3
                    va_ = vsa if br == "sel" else vwa
                    for r in range(4):
                        first = (r == 0 and ((br == "sel" and kt == 0) or (br == "win" and kt == firstwin)))
                        last = (r == 3 and kt == i)
                        P.op("pe", lambda e, r=r, pi=pi, ob=ob, va_=va_, kt=kt, first=first, last=last, g=g: e.matmul(
                            ps[ob][:, r * 65:(r + 1) * 65], lhsT=pT[pi][:, r, :], rhs=va_[:, kt, g, :], start=first, stop=last,
                            skip_group_check=True), [pTB[pi], sqB_], [pb[ob]])

                prev = None
                for (br, kt) in visits:
                    pi = issue_scores(br, kt)
                    if prev is not None:
                        issue_pv(*prev)
                    prev = (br, kt, pi)
                    yield
                issue_pv(*prev)
                yield
                gv = gts[:, i, g * 12:(g + 1) * 12].rearrange("p (r b) -> p b r", b=3)
                c2 = ps[2][:, 0:260].rearrange("p (r c) -> p r c", r=4)
                c3 = ps[3][:, 0:260].rearrange("p (r c) -> p r c", r=4)
                og_ = otk[:, g * 256:(g + 1) * 256].rearrange("p (r d) -> p r d", r=4)
                for bi_, cc, bk in ((0, c2, 2), (1, c3, 3)):
                    P.op("dve", lambda e, bi_=bi_, cc=cc: e.reciprocal(out=zz[:, bi_, :], in_=cc[:, :, 64]), [pb[bk], zzB], [zzB])
                    P.op("dve", lambda e, bi_=bi_, gv=gv: e.tensor_tensor(out=zz[:, bi_, :], in0=zz[:, bi_, :], in1=gv[:, bi_ + 1, :], op=ALU.mult),
                         [zzB, sqB_], [zzB])
                P.op("dve", lambda e, c2=c2: e.tensor_tensor(out=tmp, in0=c2[:, :, 0:64], in1=zz[:, 0, :].unsqueeze(2).broadcast_to([128, 4, 64]),
                                                           op=ALU.mult), [pb[2], zzB], [tmpB])
                P.op("dve", lambda e, c3=c3: e.tensor_tensor(out=tmp2, in0=c3[:, :, 0:64], in1=zz[:, 1, :].unsqueeze(2).broadcast_to([128, 4, 64]),
                                                            op=ALU.mult), [pb[3], zzB], [tmp2B])
                me_ = "dve"
                P.op(me_, lambda e, g=g, p=p: e.tensor_tensor(out=tmp, in0=tmp, in1=ocs[p][:, g], op=ALU.add), [tmpB, ocsB[p]], [tmpB])
                P.op(me_, lambda e, og_=og_: e.tensor_tensor(out=og_, in0=tmp, in1=tmp2, op=ALU.add), [tmpB, tmp2B], [otkB])
                yield

        def stage_c(sq_, i):
            p = i % 2
            xt, xtB = xt2[p], xt2B[p]
            tok0 = sq_ * S + i * 128
            P.dma("sp", xt, Xv[:, :, tok0:tok0 + 128], xtB, C.XB)
            P.op("act", lambda e: e.activation(out=otb, in_=otk, func=AF.Copy), [otkB], [otbB])
            yield
            for yc in range(8):
                P.op("pe", lambda e, yc=yc: e.transpose(out=p6b[:, yc * 128:(yc + 1) * 128], in_=otb[:, yc * 128:(yc + 1) * 128], identity=ident),
                     [otbB, kB], [pb[6]])
            P.op("act", lambda e: e.activation(out=oT, in_=p6b[:, 0:1024].rearrange("p (c q) -> p c q", c=8), func=AF.Copy), [pb[6]], [oTB])
            yield
            for dh in range(2):
                b = 6
                for dc in range(4):
                    d_ = dh * 4 + dc
                    for yc in range(8):
                        P.op("pe", lambda e, yc=yc, d_=d_, dc=dc, b=b: e.matmul(ps[b][:, dc * 128:(dc + 1) * 128],
                                                                             lhsT=wout[:, yc, d_ * 128:(d_ + 1) * 128], rhs=oT[:, yc, :],
                                                                             start=(yc == 0 and dc == 0), stop=(yc == 7 and dc == 3),
                                                                             skip_group_check=True), [wB, oTB], [pb[b]])
                    yield
                P.op("dve", lambda e, dh=dh, b=b, xt=xt: e.tensor_tensor(out=xt[:, dh * 4:(dh + 1) * 4, :], in0=xt[:, dh * 4:(dh + 1) * 4, :],
                                                                        in1=ps[b].rearrange("p (c q) -> p c q", c=4), op=ALU.add), [xtB, pb[b]], [xtB])
                yield
            P.dma("sp", Xv[:, :, tok0:tok0 + 128], xt, C.XB, xtB, join=True)
            yield

        for sq_ in range(NSEQ):
            P.dma("sp", ksT[0:64], D_["KsT"][sq_], sqB_, D_["KsTB"], join=True)
            P.dma("sp", kwT[0:64], D_["KwT"][sq_], sqB_, D_["KwTB"], join=True)
            P.dma("sp", kcT, D_["KcT"][sq_], sqB_, D_["KcTB"], join=True)
            P.dma("sp", vcT, D_["VcT"][sq_], sqB_, D_["VcTB"], join=True)
            for g in range(4):
                P.dma("sp", vsa[:, :, g, 0:64], D_["Vs"][sq_, :, g * 64:(g + 1) * 64].rearrange("(k p) d -> p k d", p=128), sqB_, D_["VsB"], join=True)
                P.dma("sp", vwa[:, :, g, 0:64], D_["Vw"][sq_, :, g * 64:(g + 1) * 64].rearrange("(k p) d -> p k d", p=128), sqB_, D_["VwB"], join=True)
            P.dma("sp", gts, D_["G"][sq_].rearrange("(k p) c -> p k c", p=128), sqB_, D_["GB"], join=True)
            if sq_ == 0:
                for kv in range(2):
                    for hc in range(2):
                        for l in range(32):
                            P.op("pe", lambda e, kv=kv, hc=hc, l=l: e.matmul(ps[7][:, 2 * kv + hc:2 * kv + hc + 1],
                                                                           lhsT=w1[0:64, kv, l, hc * 128:(hc + 1) * 128], rhs=peT[0:64, kv, l:l + 1],
                                                                           start=(l == 0), stop=(l == 31)), [wB], [pb[7]])
                P.op("dve", lambda e: e.tensor_tensor(out=btot, in0=b1, in1=ps[7][:, 0:4].rearrange("p (a b) -> p a b", a=2), op=ALU.add),
                     [pb[7], cB], [btB])
                for g in range(4):
                    P.dma("pool", vcmp[:, g, 0:32], D_["mseld"], vcmpB, None, join=True)
            kk_ = 0
            for kv, src in enumerate([kcT, vcT]):
                for g in range(4):
                    r0 = (g % 2) * 64
                    ch = g // 2
                    for hc in range(2):
                        b = kk_ % 4
                        kk_ += 1
                        for l in range(32):
                            P.op("pe", lambda e, kv=kv, hc=hc, l=l, r0=r0, ch=ch, src=src, b=b: e.matmul(
                                ps[b][:, 0:127], lhsT=w1[r0:r0 + 64, kv, l, hc * 128:(hc + 1) * 128],
                                rhs=src[r0:r0 + 64, ch, l:l + 16 * 126 + 1:16], start=(l == 0), stop=(l == 31)), [wB, sqB_], [pb[b]])
                        P.op("act", lambda e, kv=kv, hc=hc, b=b: e.activation(out=hT[:, hc, 0:127], in_=ps[b][:, 0:127], func=AF.Gelu_apprx_tanh,
                                                                           bias=btot[:, kv, hc:hc + 1]), [pb[b], btB], [hTB])
                    if kv == 0:
                        for hc in range(2):
                            P.op("pe", lambda e, hc=hc: e.matmul(ps[7][0:64, 0:127], lhsT=w2k[:, hc, :], rhs=hT[:, hc, 0:127],
                                                                 start=(hc == 0), stop=(hc == 1)), [wB, hTB], [pb[7]])
                        P.op("act", lambda e, g=g: e.activation(out=kcmp[0:64, g, 0:127], in_=ps[7][0:64, 0:127], func=AF.Copy), [pb[7]], [kcmpB])
                    else:
                        for hc in range(2):
                            P.op("pe", lambda e, hc=hc: e.matmul(ps[7][0:127, 0:64], lhsT=hT[:, hc, 0:127], rhs=w2v[:, hc, :],
                                                                 start=(hc == 0), stop=(hc == 1)), [wB, hTB], [pb[7]])
                        P.op("act", lambda e, g=g: e.activation(out=vcmp[0:127, g, 32:96], in_=ps[7][0:127, 0:64], func=AF.Copy), [pb[7]], [vcmpB])
            dbg = int(os.environ.get("NSADBG", "9"))
            if dbg >= 1:
                interleave([stage_a(sq_, 0)])
            for i in range(NQ):
                if dbg == 1:
                    interleave([stage_a(sq_, i + 1) if i + 1 < NQ else None])
                elif dbg == 2:
                    interleave([stage_b(sq_, i)])
                    interleave([stage_a(sq_, i + 1) if i + 1 < NQ else None])
                elif dbg >= 3:
                    interleave([stage_c(sq_, i - 1) if i >= 1 else None, stage_b(sq_, i), stage_a(sq_, i + 1) if i + 1 < NQ else None])
            if dbg >= 3:
                interleave([stage_c(sq_, NQ - 1)])
        P.barrier()


def perm_gu(w):
    wg = w[:, :DFF].reshape(8, 128, NF, 128)
    wu = w[:, DFF:].reshape(8, 128, NF, 128)
    o = np.stack([wg, wu], axis=3)
    o = o.transpose(2, 1, 0, 3, 4)
    return np.ascontiguousarray(o.reshape(NF, 128, 8 * 256))


def perm_vec(v):
    return np.ascontiguousarray(v.reshape(8, 128).T)


def build(upto="all"):
    nc = bass.Bass("TRN2", target_bir_lowering=False)
    P = Prog(nc)
    C = Ctx()
    C.P, C.nc = P, nc
    dram_in = lambda n, shp: nc.dram_tensor(n, list(shp), F32, kind="ExternalInput").ap()
    xin = dram_in("xin", [8, 128, NTOK])
    nrm_d = dram_in("nrm", [128, 56])
    wgu_d = [dram_in("wgu%d" % i, [NF, 128, 2048]) for i in range(4)]
    wd_d = [dram_in("wd%d" % i, [NF, 128, D]) for i in range(4)]
    C.out = nc.dram_tensor("out", [8, 128, NTOK], F32, kind="ExternalOutput").ap()
    C.outB = P.buf("out", keep=True)
    C.X = nc.dram_tensor("X", [8, 128, NTOK], F32, kind="Internal").ap()
    C.XB = P.buf("X", keep=True)

    with ExitStack() as es0:
        C.ps = [es0.enter_context(nc.psum_tensor("ps%d" % i, [128, 512], F32)).ap() for i in range(8)]
        C.pb = [P.buf("ps%d" % i, keep=True) for i in range(8)]
        C.constB = P.buf("const", keep=True)
        C.ones_bf = sb(es0, nc, "ones_bf", [128, 128], BF16)
        C.eps_t = sb(es0, nc, "eps_t", [128, 1], F32)
        C.nrm = sb(es0, nc, "nrm_sb", [128, 56], F32)
        P.op("dve", lambda e: e.memset(C.ones_bf, 1.0), [], [C.constB])
        P.op("dve", lambda e: e.memset(C.eps_t, EPS), [C.constB], [C.constB])
        nrmB = P.buf("nrmld")
        P.dma("sp", C.nrm, nrm_d, nrmB, None)
        P.dma("sp", C.X, xin, C.XB, None)
        P.barrier()
        C.constB.w = None
        C.constB.rd = []
        g = lambda i: C.nrm[:, i * 8:(i + 1) * 8]
        D_ = {}
        for n, shp in [("ab_win", [128, 8, 3080]), ("ab_wout", [128, 8, D]), ("rgw", [128, 4, 2, 128]), ("rgc", [128, 4, 8]),
                       ("mlc", [128, 8, 5]), ("mlrow", [128, 520])]:
            D_[n] = dram_in(n, shp)
        for n, shp in [("nsa_win", [128, 8, NCOLN]), ("nsa_wout", [128, 8, D]), ("w1d", [128, 2, 32, 256]), ("w2kd", [128, 2, 64]),
                       ("w2vd", [128, 2, 64]), ("peTd", [128, 2, 32]), ("b1d", [128, 2, 2]), ("mseld", [128, 32]),
                       ("selA", [128, 16, 32]), ("selB", [128, 16, 32]), ("bgrow", [128, 48])]:
            D_[n] = dram_in(n, shp)
        for n, shp, dt in [("Qs", [NSEQ, 64, 16, S], BF16), ("KsT", [NSEQ, 64, 4, S], BF16), ("KwT", [NSEQ, 64, 4, S], BF16),
                           ("KcT", [NSEQ, 128, 2, S], BF16), ("VcT", [NSEQ, 128, 2, S], BF16), ("Vs", [NSEQ, S, 256], BF16),
                           ("Vw", [NSEQ, S, 256], BF16), ("G", [NSEQ, S, 48], F32)]:
            D_[n] = nc.dram_tensor("scr_" + n, shp, dt, kind="Internal").ap()
            D_[n + "B"] = P.buf("scr_" + n, keep=True)
        stages = ["ffn1", "mix0", "ffn2", "ffn1b", "mix1", "all"]
        if upto in ("n1only", "n2only", "abonly"):
            if upto == "abonly":
                ab_phase(C, g(1), D_)
            else:
                nsa_proj_phase(C, g(4), D_)
                if upto == "n2only":
                    nsa_core_phase(C, D_)
            final_phase(C, g(6))
            P.emit([C.outB])
            return nc, P
        ns = stages.index(upto) + 1
        ffn_phase(C, es0, g(0), wgu_d[0], wd_d[0], final=(g(6) if ns == 1 else None))
        if ns >= 2:
            ab_phase(C, g(1), D_)
        if ns >= 3:
            ffn_phase(C, es0, g(2), wgu_d[1], wd_d[1])
        if ns >= 4:
            ffn_phase(C, es0, g(3), wgu_d[2], wd_d[2])
        if ns >= 5:
            nsa_proj_phase(C, g(4), D_)
            nsa_core_phase(C, D_)
        if ns >= 6:
            ffn_phase(C, es0, g(5), wgu_d[3], wd_d[3], final=g(6))
        elif ns >= 2:
            final_phase(C, g(6))
        P.emit([C.outB])
    return nc, P


_CACHE = {}


def kernel(upto="all", **inp):
    x = np.asarray(inp["x"], np.float32)
    B = x.shape[0]
    ncores = B // NSEQ
    if upto not in _CACHE:
        _CACHE[upto] = build(upto)
    nc, P = _CACHE[upto]
    nrm = np.concatenate([perm_vec(inp["ffn1_norm"][0]), perm_vec(inp["mix_norm"][0]), perm_vec(inp["ffn2_norm"][0]),
                          perm_vec(inp["ffn1_norm"][1]), perm_vec(inp["mix_norm"][1]), perm_vec(inp["ffn2_norm"][1]),
                          perm_vec(inp["final_norm"])], axis=1).astype(np.float32)
    shared = {"nrm": np.ascontiguousarray(nrm)}
    ffns = [("ffn1", 0), ("ffn2", 0), ("ffn1", 1), ("ffn2", 1)]
    for i, (nm, l) in enumerate(ffns):
        shared["wgu%d" % i] = perm_gu(np.asarray(inp[nm + "_w_gu"][l], np.float32))
        shared["wd%d" % i] = np.ascontiguousarray(np.asarray(inp[nm + "_w_down"][l], np.float32).reshape(NF, 128, D))
    f32 = lambda a: np.ascontiguousarray(np.asarray(a, np.float32))
    shared["ab_win"] = f32(inp["ab_w_in"][0].reshape(8, 128, 3080).transpose(1, 0, 2))
    shared["ab_wout"] = f32(inp["ab_w_out"][0].reshape(8, 128, D).transpose(1, 0, 2))
    rgw = np.zeros((128, 4, 2, 128), np.float32)
    for c in range(4):
        for k, nm in enumerate(["rg_w_r", "rg_w_i"]):
            rgw[0:64, c, k, 0:64] = inp[nm][0][2 * c]
            rgw[64:128, c, k, 64:128] = inp[nm][0][2 * c + 1]
    shared["rgw"] = rgw
    pc = lambda v, n: np.asarray(v, np.float32).reshape(n, 128).T
    rgc = np.stack([pc(inp["rg_conv_w"][0][j], 4) for j in range(4)] + [pc(inp["rg_conv_b"][0], 4), pc(inp["rg_b_r"][0], 4),
                   pc(inp["rg_b_i"][0], 4), pc(inp["rg_lambda"][0], 4)], axis=2)
    shared["rgc"] = f32(rgc)
    mlc = np.stack([pc(inp["ml_conv_w"][0][j], 8) for j in range(4)] + [pc(inp["ml_conv_b"][0], 8)], axis=2)
    shared["mlc"] = f32(mlc)
    row = np.concatenate([inp["ml_b_i"][0], inp["ml_b_f"][0], inp["ml_norm"][0]]).astype(np.float32)
    shared["mlrow"] = f32(np.tile(row[None, :], (128, 1)))
    W = np.asarray(inp["nsa_w_in"][0], np.float32)
    kcW, vcW, ksW, vsW, kwW, vwW = [W[:, 1024 + 256 * i_:1280 + 256 * i_] for i_ in range(6)]
    Wp = np.concatenate([W[:, 0:1024], ksW, kwW, kcW, vcW, vsW, vwW, W[:, 2560:2608]], axis=1)
    assert Wp.shape == (1024, NCOLN)
    shared["nsa_win"] = f32(Wp.reshape(8, 128, NCOLN).transpose(1, 0, 2))
    shared["nsa_wout"] = f32(inp["nsa_w_out"][0].reshape(8, 128, D).transpose(1, 0, 2))
    w1d = np.zeros((128, 2, 32, 256), np.float32)
    peTd = np.zeros((128, 2, 32), np.float32)
    b1d = np.zeros((128, 2, 2), np.float32)
    for kv_, (w1n, pen, b1n) in enumerate([("nsa_k_w1", "nsa_pe_k", "nsa_k_b1"), ("nsa_v_w1", "nsa_pe_v", "nsa_v_b1")]):
        w1_ = np.asarray(inp[w1n][0], np.float32).reshape(32, 64, 256).transpose(1, 0, 2)
        w1d[0:64, kv_] = w1_
        w1d[64:128, kv_] = w1_
        pe_ = np.asarray(inp[pen][0], np.float32).T
        peTd[0:64, kv_] = pe_
        peTd[64:128, kv_] = pe_
        b1d[:, kv_, :] = np.asarray(inp[b1n][0], np.float32).reshape(2, 128).T
    shared["w1d"], shared["peTd"], shared["b1d"] = w1d, peTd, b1d
    w2k_ = np.asarray(inp["nsa_k_w2"][0], np.float32).reshape(2, 128, 64).transpose(1, 0, 2)
    shared["w2kd"] = f32(w2k_)
    shared["w2vd"] = f32(np.asarray(inp["nsa_v_w2"][0], np.float32).reshape(2, 128, 64).transpose(1, 0, 2))
    ncmp = (S - 32) // 16 + 1
    cs_ = np.arange(ncmp)[:, None] * 16
    ss_ = np.arange(32)[None, :] * 64
    ov = np.clip(np.minimum(cs_ + 32, ss_ + 64) - np.maximum(cs_, ss_), 0, None) / 32.0
    msel = np.zeros((128, 32), np.float32)
    msel[:ncmp] = ov
    shared["mseld"] = msel
    tpos = (np.arange(16)[None, :] * 128 + np.arange(128)[:, None])[:, :, None]
    jj = np.arange(32)[None, None, :]
    cur = tpos // 64
    valid = (jj * 64 <= tpos)
    forced = ((jj == 0) | (jj == cur) | (jj == cur - 1)) & valid
    shared["selA"] = f32((valid & ~forced).astype(np.float32))
    shared["selB"] = f32(1e6 * forced.astype(np.float32) - (1.0 - valid.astype(np.float32)))
    shared["bgrow"] = f32(np.tile(np.asarray(inp["nsa_b_gate"][0], np.float32)[None, :], (128, 1)))
    in_maps = []
    for c in range(ncores):
        xs = x[c * NSEQ:(c + 1) * NSEQ].reshape(NTOK, 8, 128).transpose(1, 2, 0)
        m = dict(shared)
        m["xin"] = np.ascontiguousarray(xs)
        in_maps.append(m)
    res = run_bass_kernel_spmd(nc, in_maps, core_ids=list(range(ncores)))
    outs = []
    for c in range(ncores):
        o = res.results[c]["out"]
        outs.append(o.transpose(2, 0, 1).reshape(NSEQ, S, D))
    return np.ascontiguousarray(np.concatenate(outs, axis=0))
```

```python
import os
from contextlib import ExitStack
import numpy as np
import concourse.bass as bass
import concourse.mybir as mybir
from concourse.bass_utils import run_bass_kernel_spmd

F32 = mybir.dt.float32
BF16 = mybir.dt.bfloat16
ALU = mybir.AluOpType
AF = mybir.ActivationFunctionType
AX = mybir.AxisListType

D = 1024
S = 2048
NSEQ = 2
NTOK = NSEQ * S
DFF = 2816
NF = DFF // 128
NCORES = 8
EPS = 1e-6


class Buf:
    __slots__ = ("name", "w", "rd", "sem", "cnt", "keep", "q")

    def __init__(self, name, keep=False):
        self.name = name
        self.keep = keep
        self.w = None
        self.rd = []
        self.sem = None
        self.cnt = 0


class Op:
    __slots__ = ("eng", "fn", "waits", "marked", "semval", "dma")

    def __init__(self, eng, fn):
        self.eng = eng
        self.fn = fn
        self.waits = []
        self.marked = False
        self.semval = None
        self.dma = None


class Prog:
    ENGS = ("pe", "act", "dve", "pool", "sp")
    CENGS = ("pe", "act", "dve", "pool")

    def __init__(self, nc):
        self.nc = nc
        self.lists = {e: [] for e in self.ENGS}
        self.esem = {e: nc.alloc_semaphore("es_" + e) for e in self.CENGS}
        self.lastc = {e: None for e in self.CENGS}
        self.bufs = []
        self.nsem = 0
        self.sempool = {}

    def buf(self, name="b", keep=False):
        b = Buf(name, keep)
        self.bufs.append(b)
        return b

    def bufs_n(self, n, name="b"):
        return [self.buf("%s%d" % (name, i)) for i in range(n)]

    def _deps(self, o, reads, writes):
        toks = []
        for b in reads:
            if b.w is not None:
                toks.append(b.w)
        for b in writes:
            if b.w is not None:
                toks.append(b.w)
            toks.extend(b.rd)
        seen = set()
        for t in toks:
            if id(t) in seen:
                continue
            seen.add(id(t))
            if t[0] == "e":
                p = t[1]
                if p is o:
                    continue
                if p.eng == "pe" and o.eng == "pe":
                    continue
                p.marked = True
            o.waits.append(t)

    def op(self, eng, fn, reads=(), writes=()):
        o = Op(eng, fn)
        self._deps(o, reads, writes)
        tok = ("e", o)
        for b in reads:
            b.rd.append(tok)
        for b in writes:
            b.w = tok
            b.rd = []
        self.lists[eng].append(o)
        self.lastc[eng] = o
        return o

    def dma(self, q, out, in_, dst, src, join=False, **kw):
        o = Op(q, None)
        if dst.sem is None:
            pool_ = self.sempool.setdefault(q, [])
            if pool_ and not os.environ.get("NOPOOL"):
                dst.sem, dst.cnt = pool_.pop()
                dst.q = q
            else:
                dst.sem = self.nc.alloc_semaphore("ds_%d" % self.nsem)
                self.nsem += 1
                dst.q = q
        assert dst.q == q, "one DMA queue per buffer"
        srcs = [src] if src is not None else []
        if join and dst.w is not None and dst.w[0] == "d" and dst.w[1] is dst:
            saved = dst.w
            dst.w = None
            self._deps(o, srcs, [dst])
            dst.w = saved
        else:
            self._deps(o, srcs, [dst])
        dst.cnt += 16
        tok = ("d", dst, dst.cnt)
        dst.w = tok
        dst.rd = []
        if src is not None:
            src.rd.append(tok)
        o.dma = (out, in_, dst, kw)
        self.lists[q].append(o)
        return o

    def barrier(self):
        toks = []
        for e in self.CENGS:
            if self.lastc[e] is not None:
                self.lastc[e].marked = True
                toks.append(("e", self.lastc[e]))
        for b in self.bufs:
            if b.sem is not None and b.cnt > 0:
                toks.append(("d", b, b.cnt))
        for e in self.ENGS:
            o = Op(e, None)
            o.waits = [t for t in toks if not (t[0] == "e" and t[1].eng == e)]
            self.lists[e].append(o)
        kept = []
        for b in self.bufs:
            if b.keep:
                kept.append(b)
            elif b.sem is not None:
                self.sempool.setdefault(b.q, []).append((b.sem, b.cnt))
        self.bufs = kept

    def emit(self, final_bufs=()):
        nc = self.nc
        for e in self.CENGS:
            c = 0
            for o in self.lists[e]:
                if o.marked:
                    c += 1
                    o.semval = c
        fin = Op("sp", None)
        for b in final_bufs:
            if b.w is not None:
                fin.waits.append(b.w)
        self.lists["sp"].append(fin)

        def run(engname, eng):
            seen = {}
            for o in self.lists[engname]:
                for t in o.waits:
                    if t[0] == "e":
                        sem, val = self.esem[t[1].eng], t[1].semval
                    else:
                        sem, val = t[1].sem, t[2]
                    if seen.get(sem.num, 0) < val:
                        eng.wait_ge(sem, val)
                        seen[sem.num] = val
                if o.dma is not None:
                    out, in_, dst, kw = o.dma
                    eng.dma_start(out=out, in_=in_, **kw).then_inc(dst.sem, 16)
                elif o.fn is not None:
                    ins = o.fn(eng)
                    if o.marked:
                        ins.then_inc(self.esem[engname], 1)

        with nc.Block() as block:
            @block.tensor
            def _(eng):
                run("pe", eng)

            @block.scalar
            def _(eng):
                run("act", eng)

            @block.vector
            def _(eng):
                run("dve", eng)

            @block.gpsimd
            def _(eng):
                run("pool", eng)

            @block.sync
            def _(eng):
                run("sp", eng)

    def stats(self):
        return {e: len(l) for e, l in self.lists.items()}


class Ctx:
    pass


_SBN = [0]


def sb(es, nc, name, shape, dt):
    _SBN[0] += 1
    return es.enter_context(nc.sbuf_tensor("s%d_%s" % (_SBN[0], name), list(shape), dt)).ap()


def rmsnorm_fm(C, x3, xB, g2, out3, outB, sq3, sqB, T):
    P, ps, pb = C.P, C.ps, C.pb
    rs, rstd, rsB, rstdB = C.rs, C.rstd, C.rsB, C.rstdB
    nh = T // 512
    for kc in range(8):
        P.op("act", lambda e, kc=kc: e.activation(out=sq3[:, kc, 0:T], in_=x3[:, kc, 0:T], func=AF.Square),
             [xB], [sqB[kc]])
    for hf in range(nh):
        bank = 6 + (hf % 2)
        for kc in range(8):
            P.op("pe", lambda e, kc=kc, hf=hf, bank=bank: e.matmul(
                ps[bank], lhsT=C.ones_bf, rhs=sq3[:, kc, hf * 512:(hf + 1) * 512], start=(kc == 0), stop=(kc == 7)),
                [sqB[kc], C.constB], [pb[bank]])
        P.op("act", lambda e, hf=hf, bank=bank: e.activation(
            out=rs[:, hf * 512:(hf + 1) * 512], in_=ps[bank], func=AF.Sqrt, scale=1.0 / D, bias=C.eps_t[:, 0:1]),
            [pb[bank], C.constB], [rsB])
    P.op("dve", lambda e: e.reciprocal(out=rstd[:, 0:T], in_=rs[:, 0:T]), [rsB], [rstdB])
    for kc in range(8):
        P.op("dve", lambda e, kc=kc: e.scalar_tensor_tensor(
            out=out3[:, kc, 0:T], in0=x3[:, kc, 0:T], scalar=g2[:, kc:kc + 1], in1=rstd[:, 0:T],
            op0=ALU.mult, op1=ALU.mult), [xB, rstdB, C.constB], [outB[kc] if isinstance(outB, list) else outB])


def rmsnorm_fm_h(C, x3, xB, g2, out3, outB, sq3, sqB, T):
    for _ in rmsnorm_fm_g(C, x3, xB, g2, out3, outB, sq3, sqB, T):
        pass


def rmsnorm_fm_g(C, x3, xB, g2, out3, outB, sq3, sqB, T):
    P, ps, pb = C.P, C.ps, C.pb
    rs, rstd, rsB, rstdB = C.rs, C.rstd, C.rsB, C.rstdB
    for hf in range(T // 512):
        cs = slice(hf * 512, (hf + 1) * 512)
        bank = 6 + (hf % 2)
        for kc in range(8):
            P.op("act", lambda e, kc=kc, cs=cs: e.activation(out=sq3[:, kc, :], in_=x3[:, kc, cs], func=AF.Square), [xB], [sqB[kc]])
            if kc % 4 == 3:
                yield
        for kc in range(8):
            P.op("pe", lambda e, kc=kc, bank=bank: e.matmul(ps[bank], lhsT=C.ones_bf, rhs=sq3[:, kc, :], start=(kc == 0), stop=(kc == 7)),
                 [sqB[kc], C.constB], [pb[bank]])
        P.op("act", lambda e, cs=cs, bank=bank: e.activation(out=rs[:, cs], in_=ps[bank], func=AF.Sqrt, scale=1.0 / D, bias=C.eps_t[:, 0:1]),
             [pb[bank], C.constB], [rsB])
        yield
        P.op("dve", lambda e, cs=cs: e.reciprocal(out=rstd[:, cs], in_=rs[:, cs]), [rsB], [rstdB])
        yield
        for kc in range(8):
            P.op("dve", lambda e, kc=kc, cs=cs: e.scalar_tensor_tensor(
                out=out3[:, kc, cs], in0=x3[:, kc, cs], scalar=g2[:, kc:kc + 1], in1=rstd[:, cs],
                op0=ALU.mult, op1=ALU.mult), [xB, rstdB, C.constB], [outB[kc] if isinstance(outB, list) else outB])
            if kc % 2 == 1:
                yield


def ffn_phase(C, es0, cfgs):
    P, nc, ps, pb = C.P, C.nc, C.ps, C.pb
    T = 1024
    NT = NTOK // T
    with ExitStack() as es:
        xt = [sb(es, nc, "xt%d" % i, [128, 8, T], F32) for i in range(2)]
        xtB = P.bufs_n(2, "xt")
        xn = sb(es, nc, "xn", [128, 8, T], BF16)
        xnB = P.bufs_n(8, "xn")
        h = sb(es, nc, "h", [128, NF, T], BF16)
        hB = P.bufs_n(NF, "h")
        wd = sb(es, nc, "wd", [128, NF, D], BF16)
        wdB = P.bufs_n(NF, "wd")
        NR = 4
        wg = [sb(es, nc, "wg%d" % i, [128, 2048], BF16) for i in range(NR)]
        wgB = P.bufs_n(NR, "wg")
        sl = [sb(es, nc, "sl%d" % i, [128, 512], F32) for i in range(2)]
        slB = P.bufs_n(2, "sl")
        nsq = sb(es, nc, "nsq", [128, 8, 512], BF16)
        nsqB = P.bufs_n(8, "nsq")
        C.rs = sb(es, nc, "rs", [128, T], F32)
        C.rstd = sb(es, nc, "rstd", [128, T], F32)
        C.rsB, C.rstdB = P.buf("rs"), P.buf("rstd")

        def run_one(gain, wgu_d, wd_d, final, xsrc):
            Xv = C.X.rearrange("c p n -> p c n")
            Sv = xsrc.rearrange("c p n -> p c n")
            sB = C.XB if xsrc is C.X else None
            Ov = C.out.rearrange("c p n -> p c n")
            def load(t):
                P.dma("sp", xt[t % 2], Sv[:, :, t * T:(t + 1) * T], xtB[t % 2], sB)

            load(0)
            load(1)
            gi = 0
            for t in range(NT):
                x3, xB = xt[t % 2], xtB[t % 2]
                if t == 0:
                    rmsnorm_fm_h(C, x3, xB, gain, xn, xnB, nsq, nsqB, T)
                for fi in range(NF):
                    slot = gi % NR
                    gi += 1
                    P.dma("pool", wg[slot], wgu_d[fi], wgB[slot], None)
                    if t == 0 and fi == NR - 1:
                        for fj in range(NF):
                            P.dma("pool", wd[:, fj, :], wd_d[fj], wdB[fj], None)
                    for hf in range(2):
                        pg, pu = 2 * hf, 2 * hf + 1
                        cs = slice(hf * 512, (hf + 1) * 512)
                        for kc in range(8):
                            P.op("pe", lambda e, kc=kc, slot=slot, cs=cs, pg=pg: e.matmul(
                                ps[pg], lhsT=wg[slot][:, kc * 256:kc * 256 + 128], rhs=xn[:, kc, cs],
                                start=(kc == 0), stop=(kc == 7)), [wgB[slot], xnB[kc]], [pb[pg]])
                        for kc in range(8):
                            P.op("pe", lambda e, kc=kc, slot=slot, cs=cs, pu=pu: e.matmul(
                                ps[pu], lhsT=wg[slot][:, kc * 256 + 128:kc * 256 + 256], rhs=xn[:, kc, cs],
                                start=(kc == 0), stop=(kc == 7)), [wgB[slot], xnB[kc]], [pb[pu]])
                        P.op("act", lambda e, hf=hf, pg=pg: e.activation(out=sl[hf], in_=ps[pg], func=AF.Silu),
                             [pb[pg]], [slB[hf]])
                        P.op("dve", lambda e, hf=hf, pu=pu, fi=fi, cs=cs: e.tensor_tensor(
                            out=h[:, fi, cs], in0=sl[hf], in1=ps[pu], op=ALU.mult), [slB[hf], pb[pu]], [hB[fi]])
                def down_gen(x3=x3, xB=xB):
                    k = 0
                    for dc in range(8):
                        for hf in range(2):
                            po = 4 + (k % 2)
                            k += 1
                            cs = slice(hf * 512, (hf + 1) * 512)
                            for fi in range(NF):
                                P.op("pe", lambda e, fi=fi, dc=dc, cs=cs, po=po: e.matmul(
                                    ps[po], lhsT=wd[:, fi, dc * 128:(dc + 1) * 128], rhs=h[:, fi, cs],
                                    start=(fi == 0), stop=(fi == NF - 1)), [wdB[fi], hB[fi]], [pb[po]])
                            P.op("dve", lambda e, dc=dc, cs=cs, po=po, x3=x3: e.scalar_tensor_tensor(
                                out=x3[:, dc, cs], in0=ps[po], scalar=0.5, in1=x3[:, dc, cs], op0=ALU.mult, op1=ALU.add),
                                [pb[po], xB], [xB])
                            yield

                ngen = None
                if t + 1 < NT:
                    ngen = rmsnorm_fm_g(C, xt[(t + 1) % 2], xtB[(t + 1) % 2], gain, xn, xnB, nsq, nsqB, T)
                interleave([down_gen(), ngen])
                if final is None:
                    P.dma("sp", Xv[:, :, t * T:(t + 1) * T], x3, C.XB, xB, join=True)
                else:
                    rmsnorm_fm_h(C, x3, xB, final, x3, xB, nsq, nsqB, T)
                    P.dma("sp", Ov[:, :, t * T:(t + 1) * T], x3, C.outB, xB, join=True)
                if t + 2 < NT:
                    load(t + 2)

        for cfg in cfgs:
            run_one(*cfg)
        P.barrier()


def ab_phase(C, gain, D_):
    P, nc, ps, pb = C.P, C.nc, C.ps, C.pb
    T = 512
    NT = NTOK // T
    TPS = S // T
    QS = 128.0 ** -0.5
    with ExitStack() as es:
        A = lambda name, shape, dt: sb(es, nc, name, shape, dt)
        win = A("win", [128, 8, 3080], BF16)
        wout = A("wout", [128, 8, D], BF16)
        rgw = A("rgw", [128, 4, 2, 128], BF16)
        rgc = A("rgc", [128, 4, 8], F32)
        mlc = A("mlc", [128, 8, 5], F32)
        mlrow = A("mlrow", [128, 8 + 512], F32)
        cneg = A("cneg", [128, 4], F32)
        U = A("U", [128, 128], BF16)
        mneg = A("mneg", [128, 128], BF16)
        ident = A("ident", [128, 128], BF16)
        onesf = A("onesf", [128, 128], F32)
        zerof = A("zerof", [128, 128], F32)
        wB = P.buf("abw")
        for kc in range(8):
            P.dma("pool", win[:, kc, :], D_["ab_win"][:, kc, :], wB, None, join=True)
        P.dma("pool", wout, D_["ab_wout"], wB, None, join=True)
        P.dma("pool", rgw, D_["rgw"], wB, None, join=True)
        cB = P.buf("abc")
        P.dma("sp", rgc, D_["rgc"], cB, None, join=True)
        P.dma("sp", mlc, D_["mlc"], cB, None, join=True)
        P.dma("sp", mlrow, D_["mlrow"], cB, None, join=True)
        kB = P.buf("abk")
        P.op("pool", lambda e: e.memset(onesf, 1.0), [], [kB])
        P.op("pool", lambda e: e.memset(zerof, 0.0), [kB], [kB])
        P.op("pool", lambda e: e.affine_select(out=U, in_=onesf, pattern=[[1, 128]], compare_op=ALU.is_ge, fill=0.0,
                                              base=0, channel_multiplier=-1), [kB], [kB])
        P.op("pool", lambda e: e.affine_select(out=mneg, in_=zerof, pattern=[[1, 128]], compare_op=ALU.is_ge,
                                              fill=-30000.0, base=0, channel_multiplier=-1), [kB], [kB])
        P.op("pool", lambda e: e.affine_select(out=ident, in_=onesf, pattern=[[1, 128]], compare_op=ALU.is_equal,
                                              fill=0.0, base=0, channel_multiplier=-1), [kB], [kB])
        P.op("act", lambda e: e.activation(out=cneg, in_=rgc[:, :, 7], func=AF.Exp, scale=-1.0), [cB], [kB])
        P.op("act", lambda e: e.activation(out=cneg, in_=cneg, func=AF.Ln, bias=1.0), [kB], [kB])
        P.op("dve", lambda e: e.tensor_scalar(out=cneg, in0=cneg, scalar1=-8.0, scalar2=None, op0=ALU.mult), [kB], [kB])

        xt = A("xt", [128, 8, T], F32); xtB = P.buf("xt")
        xn = A("xn", [128, 8, T], BF16); xnB = P.bufs_n(8, "xn")
        yT = A("yT", [128, 8, T], BF16); yTB = P.bufs_n(8, "yT")
        C.rs = A("rs", [128, T], F32); C.rstd = A("rstd", [128, T], F32)
        C.rsB, C.rstdB = P.buf("rs"), P.buf("rstd")
        xa = A("xa", [128, 4, T + 3], F32); xaB = P.bufs_n(4, "xa")
        qk = A("qk", [128, 8, T + 3], F32); qkB = P.bufs_n(8, "qk")
        xc = A("xc", [128, T], F32); xcB = P.buf("xc")
        xcb = A("xcb", [128, T], BF16); xcbB = P.buf("xcb")
        rr = A("rr", [128, T], F32); rrB = P.buf("rr")
        ig = A("ig", [128, T], F32); igB = P.buf("ig")
        aa = A("aa", [128, T], F32); aaB = P.buf("aa")
        uu = A("uu", [128, T], F32); uuB = P.buf("uu")
        hs = A("hs", [128, T], F32); hsB = P.buf("hs")
        gg = A("gg", [128, T], F32); ggB = P.buf("gg")
        hst = A("hst", [128, 4], F32); hstB = P.bufs_n(4, "hst")
        qc = A("qc", [128, T], F32); qcB = P.buf("qc")
        qT = A("qT", [128, 4, T], BF16); qTB = P.bufs_n(4, "qT")
        kT = A("kT", [128, 4, T], BF16); kTB = P.bufs_n(4, "kT")
        va = A("va", [128, 4, 4, 129], BF16); vaB = P.bufs_n(4, "va")
        og = A("og", [128, 4, 512], F32); ogB = P.bufs_n(4, "og")
        gt = A("gt", [128, 4, 8], F32); gtB = P.bufs_n(4, "gt")
        lhi = A("lhi", [128, 4], BF16); llo = A("llo", [128, 4], BF16); lhf = A("lhf", [128, 4], F32)
        lB = P.buf("lhl")
        Lh = A("Lh", [128, 4, 128], BF16); Ll = A("Ll", [128, 4, 128], BF16); LB = P.buf("L")
        sm = A("sm", [128, 24], F32); smB = P.buf("sm")
        DT = A("DT", [128, 4, 128], F32); DTB = P.buf("DT")
        AT = A("AT", [128, 4, 128], BF16); ATB = P.buf("AT")
        kw = A("kw", [128, 4, 128], BF16); kwB = P.buf("kw")
        itr = A("itr", [128, 4, 129], F32); itrB = P.buf("itr")
        tot = A("tot", [128, 4, 129], F32); totB = P.buf("tot")
        hh = A("hh", [128, 4, 128], F32); hhB = P.buf("hh")
        h2 = A("h2", [128, 4, 128], F32); h2B = P.buf("h2")
        ytk = A("ytk", [128, 512], BF16); ytkB = P.buf("ytk")
        Cst = A("Cst", [128, 4, 129], F32); CstB = P.buf("Cst")
        Cbf = A("Cbf", [128, 4, 129], BF16); CbfB = P.buf("Cbf")
        P.op("pool", lambda e: e.memset(va, 1.0), [], vaB)
        Xv = C.X.rearrange("c p n -> p c n")
        bank_rr = [0]

        def nb():
            bank_rr[0] = (bank_rr[0] + 1) % 6
            return bank_rr[0]

        def proj_fm(col0, t, bank=None):
            b = nb() if bank is None else bank
            for kc in range(8):
                P.op("pe", lambda e, kc=kc, b=b: e.matmul(ps[b], lhsT=win[:, kc, col0:col0 + 128], rhs=xn[:, kc, :],
                                                          start=(kc == 0), stop=(kc == 7)), [wB, xnB[kc]], [pb[b]])
            return b

        for t in range(NT):
            first = (t % TPS == 0)
            P.dma("sp", xt, Xv[:, :, t * T:(t + 1) * T], xtB, C.XB)
            rmsnorm_fm(C, xt, xtB, gain, xn, xnB, yT, yTB, T)
            if first:
                P.op("pool", lambda e: e.memset(Cst, 0.0), [], [CstB])
                P.op("pool", lambda e: e.memset(Cbf, 0.0), [], [CbfB])
            for c in range(8):
                if first:
                    P.op("pool", lambda e, c=c: e.memset(qk[:, c, 0:3], 0.0), [], [qkB[c]])
                else:
                    P.op("pool", lambda e, c=c: e.tensor_copy(out=qk[:, c, 0:3], in_=qk[:, c, T:T + 3]), [qkB[c]], [qkB[c]])
                b = proj_fm(1024 + c * 128, t)
                P.op("act", lambda e, c=c, b=b: e.activation(out=qk[:, c, 3:T + 3], in_=ps[b], func=AF.Copy), [pb[b]], [qkB[c]])
                P.op("dve", lambda e, c=c: e.tensor_scalar(out=qc, in0=qk[:, c, 0:T], scalar1=mlc[:, c, 0:1],
                                                          scalar2=mlc[:, c, 4:5], op0=ALU.mult, op1=ALU.add), [qkB[c], cB], [qcB])
                for j in range(1, 4):
                    P.op("dve", lambda e, c=c, j=j: e.scalar_tensor_tensor(
                        out=qc, in0=qk[:, c, j:j + T], scalar=mlc[:, c, j:j + 1], in1=qc, op0=ALU.mult, op1=ALU.add),
                        [qkB[c], cB, qcB], [qcB])
                if c < 4:
                    P.op("act", lambda e: e.activation(out=qc, in_=qc, func=AF.Silu), [qcB], [qcB])
                    P.op("dve", lambda e, c=c: e.tensor_scalar(out=qT[:, c, :], in0=qc, scalar1=QS, scalar2=None, op0=ALU.mult),
                         [qcB], [qTB[c]])
                else:
                    P.op("act", lambda e, c=c: e.activation(out=kT[:, c - 4, :], in_=qc, func=AF.Silu), [qcB], [kTB[c - 4]])
            for j in range(4):
                ts_ = slice(j * 128, (j + 1) * 128)
                b = nb()
                for kc in range(8):
                    P.op("pe", lambda e, kc=kc, b=b, ts_=ts_: e.matmul(ps[b], lhsT=xn[:, kc, ts_], rhs=win[:, kc, 2048:2560],
                                                                     start=(kc == 0), stop=(kc == 7)), [wB, xnB[kc]], [pb[b]])
                P.op("act", lambda e, j=j, b=b: e.activation(out=va[:, j, :, 0:128], in_=ps[b].rearrange("p (h v) -> p h v", h=4),
                                                             func=AF.Copy), [pb[b]], [vaB[j]])
                b = nb()
                for kc in range(8):
                    P.op("pe", lambda e, kc=kc, b=b, ts_=ts_: e.matmul(ps[b], lhsT=xn[:, kc, ts_], rhs=win[:, kc, 2560:3072],
                                                                     start=(kc == 0), stop=(kc == 7)), [wB, xnB[kc]], [pb[b]])
                P.op("act", lambda e, j=j, b=b: e.activation(out=og[:, j, :], in_=ps[b], func=AF.Sigmoid), [pb[b]], [ogB[j]])
                b = nb()
                for kc in range(8):
                    P.op("pe", lambda e, kc=kc, b=b, ts_=ts_: e.matmul(ps[b][:, 0:8], lhsT=xn[:, kc, ts_], rhs=win[:, kc, 3072:3080],
                                                                     start=(kc == 0), stop=(kc == 7)), [wB, xnB[kc]], [pb[b]])
                P.op("dve", lambda e, j=j, b=b: e.tensor_tensor(out=gt[:, j, :], in0=ps[b][:, 0:8], in1=mlrow[:, 0:8], op=ALU.add),
                     [pb[b], cB], [gtB[j]])
                P.op("act", lambda e, j=j: e.activation(out=gt[:, j, 4:8], in_=gt[:, j, 4:8], func=AF.Exp, scale=-1.0), [gtB[j]], [gtB[j]])
                P.op("act", lambda e, j=j: e.activation(out=gt[:, j, 4:8], in_=gt[:, j, 4:8], func=AF.Ln, bias=1.0), [gtB[j]], [gtB[j]])
                P.op("dve", lambda e, j=j: e.tensor_scalar(out=gt[:, j, 4:8], in0=gt[:, j, 4:8], scalar1=-1.0, scalar2=None, op0=ALU.mult),
                     [gtB[j]], [gtB[j]])
            def rg_stream():
                for c in range(4):
                    if first:
                        P.op("pool", lambda e, c=c: e.memset(xa[:, c, 0:3], 0.0), [], [xaB[c]])
                    else:
                        P.op("pool", lambda e, c=c: e.tensor_copy(out=xa[:, c, 0:3], in_=xa[:, c, T:T + 3]), [xaB[c]], [xaB[c]])
                    b = proj_fm(c * 128, t, 5)
                    P.op("act", lambda e, c=c, b=b: e.activation(out=xa[:, c, 3:T + 3], in_=ps[b], func=AF.Copy), [pb[b]], [xaB[c]])
                    P.op("dve", lambda e, c=c: e.tensor_scalar(out=xc, in0=xa[:, c, 0:T], scalar1=rgc[:, c, 0:1],
                                                              scalar2=rgc[:, c, 4:5], op0=ALU.mult, op1=ALU.add), [xaB[c], cB], [xcB])
                    for j in range(1, 4):
                        P.op("dve", lambda e, c=c, j=j: e.scalar_tensor_tensor(
                            out=xc, in0=xa[:, c, j:j + T], scalar=rgc[:, c, j:j + 1], in1=xc, op0=ALU.mult, op1=ALU.add),
                            [xaB[c], cB, xcB], [xcB])
                    P.op("act", lambda e: e.activation(out=xcb, in_=xc, func=AF.Copy), [xcB], [xcbB])
                    yield
                    br = 5
                    P.op("pe", lambda e, c=c, br=br: e.matmul(ps[br], lhsT=rgw[:, c, 0, :], rhs=xcb, start=True, stop=True),
                         [wB, xcbB], [pb[br]])
                    P.op("act", lambda e, c=c, br=br: e.activation(out=rr, in_=ps[br], func=AF.Sigmoid, bias=rgc[:, c, 5:6]),
                         [pb[br], cB], [rrB])
                    yield
                    bi = 5
                    P.op("pe", lambda e, c=c, bi=bi: e.matmul(ps[bi], lhsT=rgw[:, c, 1, :], rhs=xcb, start=True, stop=True),
                         [wB, xcbB], [pb[bi]])
                    P.op("act", lambda e, c=c, bi=bi: e.activation(out=ig, in_=ps[bi], func=AF.Sigmoid, bias=rgc[:, c, 6:7]),
                         [pb[bi], cB], [igB])
                    P.op("act", lambda e, c=c: e.activation(out=aa, in_=rr, func=AF.Exp, scale=cneg[:, c:c + 1]), [rrB, kB], [aaB])
                    yield
                    P.op("act", lambda e: e.activation(out=rr, in_=aa, func=AF.Square), [aaB], [rrB])
                    P.op("act", lambda e: e.activation(out=rr, in_=rr, func=AF.Sqrt, scale=-1.0, bias=onesf[:, 0:1]), [rrB, kB], [rrB])
                    P.op("dve", lambda e: e.tensor_tensor(out=uu, in0=ig, in1=xc, op=ALU.mult), [igB, xcB], [uuB])
                    P.op("dve", lambda e: e.tensor_tensor(out=uu, in0=uu, in1=rr, op=ALU.mult), [uuB, rrB], [uuB])
                    yield
                    if first:
                        P.op("dve", lambda e: e.tensor_tensor_scan(out=hs, data0=aa, data1=uu, initial=0.0, op0=ALU.mult, op1=ALU.add),
                             [aaB, uuB], [hsB])
                    else:
                        P.op("dve", lambda e, c=c: e.tensor_tensor_scan(out=hs, data0=aa, data1=uu, initial=hst[:, c:c + 1],
                                                                       op0=ALU.mult, op1=ALU.add), [aaB, uuB, hstB[c]], [hsB])
                    P.op("dve", lambda e, c=c: e.tensor_copy(out=hst[:, c:c + 1], in_=hs[:, T - 1:T]), [hsB], [hstB[c]])
                    yield
                    bg = proj_fm(512 + c * 128, t, 5)
                    P.op("act", lambda e, bg=bg: e.activation(out=gg, in_=ps[bg], func=AF.Gelu_apprx_tanh), [pb[bg]], [ggB])
                    P.op("dve", lambda e, c=c: e.tensor_tensor(out=yT[:, c, :], in0=gg, in1=hs, op=ALU.mult), [ggB, hsB], [yTB[c]])
                    yield
            def ml_stream():
                for j in range(4):
                    ts_ = slice(j * 128, (j + 1) * 128)
                    logi = gt[:, j, 0:4]
                    logf = gt[:, j, 4:8]
                    P.op("dve", lambda e, logf=logf: e.tensor_copy(out=lhi, in_=logf), [gtB[j]], [lB])
                    P.op("dve", lambda e: e.tensor_copy(out=lhf, in_=lhi), [lB], [lB])
                    P.op("dve", lambda e, logf=logf: e.tensor_tensor(out=lhf, in0=logf, in1=lhf, op=ALU.subtract), [gtB[j], lB], [lB])
                    P.op("dve", lambda e: e.tensor_copy(out=llo, in_=lhf), [lB], [lB])
                    P.op("dve", lambda e: e.tensor_copy(out=Lh, in_=lhi.unsqueeze(2).broadcast_to([128, 4, 128])), [lB], [LB])
                    P.op("dve", lambda e: e.tensor_copy(out=Ll, in_=llo.unsqueeze(2).broadcast_to([128, 4, 128])), [lB, LB], [LB])
                    P.op("pe", lambda e: e.matmul(ps[7][:, 264:268], lhsT=U, rhs=lhi, start=True, stop=False), [kB, lB], [pb[7]])
                    P.op("pe", lambda e: e.matmul(ps[7][:, 264:268], lhsT=U, rhs=llo, start=False, stop=True), [kB, lB], [pb[7]])
                    P.op("pe", lambda e: e.matmul(ps[7][:, 268:272], lhsT=C.ones_bf, rhs=lhi, start=True, stop=False), [kB, lB], [pb[7]])
                    P.op("pe", lambda e: e.matmul(ps[7][:, 268:272], lhsT=C.ones_bf, rhs=llo, start=False, stop=True), [kB, lB], [pb[7]])
                    for hd in range(4):
                        o_ = ps[0][:, hd * 128:(hd + 1) * 128]
                        P.op("pe", lambda e, hd=hd, o_=o_: e.matmul(o_, lhsT=Lh[:, hd, :], rhs=U, start=True, stop=False), [LB, kB], [pb[0]])
                        P.op("pe", lambda e, hd=hd, o_=o_: e.matmul(o_, lhsT=Ll[:, hd, :], rhs=U, start=False, stop=False), [LB, kB], [pb[0]])
                        P.op("pe", lambda e, hd=hd, o_=o_: e.matmul(o_, lhsT=ident, rhs=mneg, start=False, stop=True), [kB], [pb[0]])
                        P.op("pe", lambda e, hd=hd, ts_=ts_: e.matmul(ps[1][:, hd * 128:(hd + 1) * 128], lhsT=kT[:, hd, ts_], rhs=qT[:, hd, ts_],
                                                                     start=True, stop=True), [kTB[hd], qTB[hd]], [pb[1]])
                    yield
                    bcol = ps[7][:, 264:268]
                    gcol = ps[7][:, 268:272]
                    P.op("dve", lambda e, logi=logi, bcol=bcol: e.tensor_tensor(out=sm[:, 0:4], in0=logi, in1=bcol, op=ALU.subtract),
                         [gtB[j], pb[7]], [smB])
                    P.op("dve", lambda e, gcol=gcol: e.tensor_tensor(out=sm[:, 8:12], in0=sm[:, 0:4], in1=gcol, op=ALU.add),
                         [smB, pb[7]], [smB])
                    P.op("act", lambda e, bcol=bcol: e.activation(out=sm[:, 4:8], in_=bcol, func=AF.Exp), [pb[7], smB], [smB])
                    P.op("act", lambda e: e.activation(out=sm[:, 8:12], in_=sm[:, 8:12], func=AF.Exp), [smB], [smB])
                    P.op("act", lambda e, gcol=gcol: e.activation(out=sm[:, 12:16], in_=gcol, func=AF.Exp), [pb[7], smB], [smB])
                    yield
                    for hd in range(4):
                        P.op("act", lambda e, hd=hd: e.activation(out=DT[:, hd, :], in_=ps[0][:, hd * 128:(hd + 1) * 128], func=AF.Exp,
                                                                  bias=sm[:, hd:hd + 1]), [pb[0], smB], [DTB])
                    P.op("dve", lambda e: e.tensor_tensor(out=AT, in0=DT, in1=ps[1].rearrange("p (h k) -> p h k", h=4), op=ALU.mult),
                         [DTB, pb[1]], [ATB])
                    yield
                    for hd in range(4):
                        bi_ = 2 + hd // 2
                        cs = slice((hd % 2) * 129, (hd % 2) * 129 + 129)
                        P.op("pe", lambda e, hd=hd, bi_=bi_, cs=cs, ts_=ts_: e.matmul(ps[bi_][:, cs], lhsT=qT[:, hd, ts_], rhs=Cbf[:, hd, :],
                                                                                    start=True, stop=True), [qTB[hd], CbfB], [pb[bi_]])
                    for hp in range(2):
                        for hd in (2 * hp, 2 * hp + 1):
                            cs = slice((hd % 2) * 129, (hd % 2) * 129 + 129)
                            P.op("pe", lambda e, hd=hd, cs=cs, j=j: e.matmul(ps[4][:, cs], lhsT=AT[:, hd, :], rhs=va[:, j, hd, :],
                                                                           start=True, stop=True), [ATB, vaB[j]], [pb[4]])
                        P.op("act", lambda e, hp=hp: e.activation(out=itr[:, 2 * hp:2 * hp + 2, :],
                                                                  in_=ps[4][:, 0:258].rearrange("p (h v) -> p h v", h=2), func=AF.Copy),
                             [pb[4]], [itrB])
                    yield
                    for hd in range(4):
                        bi_ = 2 + hd // 2
                        cs = slice((hd % 2) * 129, (hd % 2) * 129 + 129)
                        P.op("dve", lambda e, hd=hd, bi_=bi_, cs=cs: e.scalar_tensor_tensor(
                            out=tot[:, hd, :], in0=ps[bi_][:, cs], scalar=sm[:, 4 + hd:5 + hd], in1=itr[:, hd, :], op0=ALU.mult, op1=ALU.add),
                            [pb[bi_], smB, itrB], [totB])
                    yield
                    P.op("dve", lambda e: e.tensor_scalar(out=sm[:, 16:20], in0=tot[:, :, 128], scalar1=-1.0, scalar2=1.0,
                                                          op0=ALU.mult, op1=ALU.max), [totB, smB], [smB])
                    P.op("dve", lambda e: e.tensor_tensor(out=sm[:, 16:20], in0=sm[:, 16:20], in1=tot[:, :, 128], op=ALU.max),
                         [totB, smB], [smB])
                    P.op("dve", lambda e: e.reciprocal(out=sm[:, 16:20], in_=sm[:, 16:20]), [smB], [smB])
                    P.op("dve", lambda e: e.tensor_tensor(out=hh, in0=tot[:, :, 0:128],
                                                          in1=sm[:, 16:20].unsqueeze(2).broadcast_to([128, 4, 128]), op=ALU.mult),
                         [totB, smB], [hhB])
                    P.op("dve", lambda e: e.tensor_tensor(out=h2, in0=hh, in1=hh, op=ALU.mult), [hhB], [h2B])
                    P.op("dve", lambda e: e.tensor_reduce(out=sm[:, 20:24], in_=h2, axis=AX.X, op=ALU.add), [h2B, smB], [smB])
                    P.op("act", lambda e: e.activation(out=sm[:, 20:24], in_=sm[:, 20:24], func=AF.Sqrt, scale=1.0 / 128, bias=C.eps_t[:, 0:1]),
                         [smB], [smB])
                    P.op("dve", lambda e: e.reciprocal(out=sm[:, 20:24], in_=sm[:, 20:24]), [smB], [smB])
                    P.op("dve", lambda e: e.tensor_tensor(out=hh, in0=hh, in1=sm[:, 20:24].unsqueeze(2).broadcast_to([128, 4, 128]),
                                                          op=ALU.mult), [hhB, smB], [hhB])
                    P.op("dve", lambda e: e.tensor_tensor(out=hh, in0=hh, in1=mlrow[:, 8:520].rearrange("p (h v) -> p h v", h=4),
                                                          op=ALU.mult), [hhB, cB], [hhB])
                    P.op("dve", lambda e, j=j: e.tensor_tensor(out=ytk.rearrange("p (h v) -> p h v", h=4), in0=hh,
                                                               in1=og[:, j, :].rearrange("p (h v) -> p h v", h=4), op=ALU.mult),
                         [hhB, ogB[j]], [ytkB])
                    yield
                    p6b = ps[6].bitcast(BF16)
                    for hd in range(4):
                        P.op("pe", lambda e, hd=hd, p6b=p6b: e.transpose(out=p6b[:, hd * 128:(hd + 1) * 128], in_=ytk[:, hd * 128:(hd + 1) * 128],
                                                                       identity=ident), [ytkB, kB], [pb[6]])
                    for hd in range(4):
                        P.op("act", lambda e, hd=hd, p6b=p6b, ts_=ts_: e.activation(out=yT[:, 4 + hd, ts_], in_=p6b[:, hd * 128:(hd + 1) * 128],
                                                                                  func=AF.Copy), [pb[6]], [yTB[4 + hd]])
                    yield
                    p7b = ps[7].bitcast(BF16)
                    for hd in range(4):
                        P.op("pe", lambda e, hd=hd, p7b=p7b, ts_=ts_: e.transpose(out=p7b[:, 512 + hd * 128:512 + (hd + 1) * 128],
                                                                                in_=kT[:, hd, ts_], identity=ident), [kTB[hd], kB], [pb[7]])
                    for hd in range(4):
                        P.op("dve", lambda e, hd=hd, p7b=p7b: e.tensor_scalar(out=kw[:, hd, :], in0=p7b[:, 512 + hd * 128:512 + (hd + 1) * 128],
                                                                            scalar1=sm[:, 8 + hd:9 + hd], scalar2=None, op0=ALU.mult),
                             [pb[7], smB], [kwB])
                    for hd in range(4):
                        bs_ = 2 + hd // 2
                        cs = slice((hd % 2) * 129, (hd % 2) * 129 + 129)
                        P.op("pe", lambda e, hd=hd, bs_=bs_, cs=cs, j=j: e.matmul(ps[bs_][:, cs], lhsT=kw[:, hd, :], rhs=va[:, j, hd, :],
                                                                                start=True, stop=True), [kwB, vaB[j]], [pb[bs_]])
                    for hd in range(4):
                        bs_ = 2 + hd // 2
                        cs = slice((hd % 2) * 129, (hd % 2) * 129 + 129)
                        P.op("dve", lambda e, hd=hd, bs_=bs_, cs=cs: e.scalar_tensor_tensor(
                            out=Cst[:, hd, :], in0=Cst[:, hd, :], scalar=sm[:, 12 + hd:13 + hd], in1=ps[bs_][:, cs], op0=ALU.mult, op1=ALU.add),
                            [CstB, smB, pb[bs_]], [CstB])
                    P.op("act", lambda e: e.activation(out=Cbf, in_=Cst, func=AF.Copy), [CstB], [CbfB])
                    yield
            interleave([rg_stream(), ml_stream()])
            for dc in range(8):
                b = nb()
                for yc in range(8):
                    P.op("pe", lambda e, yc=yc, dc=dc, b=b: e.matmul(ps[b], lhsT=wout[:, yc, dc * 128:(dc + 1) * 128], rhs=yT[:, yc, :],
                                                                   start=(yc == 0), stop=(yc == 7)), [wB, yTB[yc]], [pb[b]])
                P.op("dve", lambda e, dc=dc, b=b: e.tensor_tensor(out=xt[:, dc, :], in0=xt[:, dc, :], in1=ps[b], op=ALU.add),
                     [xtB, pb[b]], [xtB])
            P.dma("sp", Xv[:, :, t * T:(t + 1) * T], xt, C.XB, xtB, join=True)
        P.barrier()


def final_phase(C, gain):
    P, nc = C.P, C.nc
    T = 512
    with ExitStack() as es:
        xt = sb(es, nc, "fxt", [128, 8, T], F32); xtB = P.buf("fxt")
        sq = sb(es, nc, "fsq", [128, 8, T], BF16); sqB = P.bufs_n(8, "fsq")
        C.rs = sb(es, nc, "rs", [128, T], F32); C.rstd = sb(es, nc, "rstd", [128, T], F32)
        C.rsB, C.rstdB = P.buf("rs"), P.buf("rstd")
        Xv = C.X.rearrange("c p n -> p c n")
        Ov = C.out.rearrange("c p n -> p c n")
        for t in range(NTOK // T):
            P.dma("sp", xt, Xv[:, :, t * T:(t + 1) * T], xtB, C.XB)
            rmsnorm_fm(C, xt, xtB, gain, xt, xtB, sq, sqB, T)
            P.dma("sp", Ov[:, :, t * T:(t + 1) * T], xt, C.outB, xtB, join=True)
        P.barrier()


NCOLN = 2608


def interleave(gens):
    gens = [g for g in gens if g is not None]
    while gens:
        for g in list(gens):
            try:
                next(g)
            except StopIteration:
                gens.remove(g)


def nsa_proj_phase(C, gain, D_):
    P, nc, ps, pb = C.P, C.nc, C.ps, C.pb
    T = 512
    NT = NTOK // T
    TPS = S // T
    with ExitStack() as es:
        A = lambda name, shape, dt: sb(es, nc, name, shape, dt)
        win = A("nwin", [128, 8, NCOLN], BF16)
        wB = P.buf("nw")
        for kc in range(8):
            P.dma("pool", win[:, kc, :], D_["nsa_win"][:, kc, :], wB, None, join=True)
        bg = A("bg", [128, 48], F32)
        cB = P.buf("nc")
        P.dma("sp", bg, D_["bgrow"], cB, None)
        xt = [A("xt%d" % i, [128, 8, T], F32) for i in range(2)]; xtB = P.bufs_n(2, "xt")
        xn2 = [A("xn%d" % i, [128, 8, T], BF16) for i in range(2)]; xn2B = [P.bufs_n(8, "xn%d_" % i) for i in range(2)]
        sq2 = [A("sq%d" % i, [128, 8, T], BF16) for i in range(2)]; sq2B = [P.bufs_n(8, "sq%d_" % i) for i in range(2)]
        C.rs = A("rs", [128, T], F32); C.rstd = A("rstd", [128, T], F32)
        C.rsB, C.rstdB = P.buf("rs"), P.buf("rstd")
        stg = [A("stg%d" % i, [128, 16, T], BF16) for i in range(2)]; stgB = [P.bufs_n(5, "stg%d_" % i) for i in range(2)]
        vtk = [A("vtk%d" % i, [128, 4, 512], BF16) for i in range(2)]; vtkB = P.bufs_n(2, "vtk")
        gtk = [A("gtk%d" % i, [128, 4, 48], F32) for i in range(2)]; gtkB = P.bufs_n(2, "gtk")
        Xv = C.X.rearrange("c p n -> p c n")
        k = 0
        groups = [("Qs", 0, 8, 0.125, 0), ("KsT", 8, 2, 1.0, 1024), ("KwT", 10, 2, 1.0, 1280), ("KcT", 12, 2, 1.0, 1536),
                  ("VcT", 14, 2, 1.0, 1792)]
        P.dma("sp", xt[0], Xv[:, :, 0:T], xtB[0], C.XB)
        for t in range(NT):
            sq_, t0 = t // TPS, (t % TPS) * T
            pr = t % 2
            if t + 1 < NT:
                P.dma("sp", xt[(t + 1) % 2], Xv[:, :, (t + 1) * T:(t + 2) * T], xtB[(t + 1) % 2], C.XB)
            xn, xnB, sq, sqB = xn2[pr], xn2B[pr], sq2[pr], sq2B[pr]
            if t == 0:
                rmsnorm_fm(C, xt[pr], xtB[pr], gain, xn, xnB, sq, sqB, T)
            for gi, (nm, c0, ncnk, scl, cb) in enumerate(groups):
                if gi == 1 and t + 1 < NT:
                    pn = (t + 1) % 2
                    rmsnorm_fm(C, xt[pn], xtB[pn], gain, xn2[pn], xn2B[pn], sq2[pn], sq2B[pn], T)
                for cc in range(ncnk):
                    b = k % 6
                    k += 1
                    col0 = cb + cc * 128
                    for kc in range(8):
                        P.op("pe", lambda e, kc=kc, b=b, col0=col0, xn=xn: e.matmul(ps[b], lhsT=win[:, kc, col0:col0 + 128], rhs=xn[:, kc, :],
                                                                          start=(kc == 0), stop=(kc == 7)), [wB, xnB[kc]], [pb[b]])
                    if (c0 + cc) % 2 == 0:
                        P.op("act", lambda e, b=b, c=c0 + cc, scl=scl, pr=pr: e.activation(out=stg[pr][:, c, :], in_=ps[b], func=AF.Copy, scale=scl),
                             [pb[b]], [stgB[pr][gi]])
                    else:
                        P.op("dve", lambda e, b=b, c=c0 + cc, scl=scl, pr=pr: e.tensor_scalar(out=stg[pr][:, c, :], in0=ps[b], scalar1=scl,
                                                                                          scalar2=None, op0=ALU.mult), [pb[b]], [stgB[pr][gi]])
                if nm in ("Qs", "KsT", "KwT"):
                    nh = 2 * ncnk
                    P.dma("sp", D_[nm][sq_, :, 0:nh:2, t0:t0 + T], stg[pr][0:64, c0:c0 + ncnk, :], D_[nm + "B"], stgB[pr][gi], join=True)
                    P.dma("sp", D_[nm][sq_, :, 1:nh:2, t0:t0 + T], stg[pr][64:128, c0:c0 + ncnk, :], D_[nm + "B"], stgB[pr][gi], join=True)
                else:
                    P.dma("sp", D_[nm][sq_, :, :, t0:t0 + T], stg[pr][:, c0:c0 + ncnk, :], D_[nm + "B"], stgB[pr][gi], join=True)
            for j in range(4):
                ts_ = slice(j * 128, (j + 1) * 128)
                b = k % 6
                k += 1
                for kc in range(8):
                    P.op("pe", lambda e, kc=kc, b=b, ts_=ts_, xn=xn: e.matmul(ps[b], lhsT=xn[:, kc, ts_], rhs=win[:, kc, 2048:2560],
                                                                     start=(kc == 0), stop=(kc == 7)), [wB, xnB[kc]], [pb[b]])
                P.op("act", lambda e, b=b, j=j, pr=pr: e.activation(out=vtk[pr][:, j, :], in_=ps[b], func=AF.Copy), [pb[b]], [vtkB[pr]])
                b = k % 6
                k += 1
                for kc in range(8):
                    P.op("pe", lambda e, kc=kc, b=b, ts_=ts_, xn=xn: e.matmul(ps[b][:, 0:48], lhsT=xn[:, kc, ts_], rhs=win[:, kc, 2560:2608],
                                                                     start=(kc == 0), stop=(kc == 7)), [wB, xnB[kc]], [pb[b]])
                P.op("dve", lambda e, b=b, j=j, pr=pr: e.tensor_tensor(out=gtk[pr][:, j, :], in0=ps[b][:, 0:48], in1=bg, op=ALU.add),
                     [pb[b], cB], [gtkB[pr]])
            P.op("act", lambda e, pr=pr: e.activation(out=gtk[pr], in_=gtk[pr], func=AF.Sigmoid), [gtkB[pr]], [gtkB[pr]])
            P.dma("sp", D_["Vs"][sq_, t0:t0 + T, :].rearrange("(j p) c -> p j c", p=128), vtk[pr][:, :, 0:256], D_["VsB"], vtkB[pr], join=True)
            P.dma("sp", D_["Vw"][sq_, t0:t0 + T, :].rearrange("(j p) c -> p j c", p=128), vtk[pr][:, :, 256:512], D_["VwB"], vtkB[pr], join=True)
            P.dma("sp", D_["G"][sq_, t0:t0 + T, :].rearrange("(j p) c -> p j c", p=128), gtk[pr], D_["GB"], gtkB[pr], join=True)
        P.barrier()


def nsa_core_phase(C, D_):
    P, nc, ps, pb = C.P, C.nc, C.ps, C.pb
    NQ = S // 128
    NEG = -30000.0
    with ExitStack() as es:
        A = lambda name, shape, dt: sb(es, nc, name, shape, dt)
        wout = A("nwout", [128, 8, D], BF16)
        w1 = A("w1", [128, 2, 32, 256], BF16)
        w2k = A("w2k", [128, 2, 64], BF16)
        w2v = A("w2v", [128, 2, 64], BF16)
        peT = A("peT", [128, 2, 32], BF16)
        b1 = A("b1", [128, 2, 2], F32)
        selA = A("selA", [128, 16, 32], F32)
        selB = A("selB", [128, 16, 32], F32)
        wB = P.buf("n2w")
        P.dma("pool", wout, D_["nsa_wout"], wB, None, join=True)
        P.dma("pool", w1, D_["w1d"], wB, None, join=True)
        P.dma("pool", w2k, D_["w2kd"], wB, None, join=True)
        P.dma("pool", w2v, D_["w2vd"], wB, None, join=True)
        P.dma("pool", peT, D_["peTd"], wB, None, join=True)
        cB = P.buf("n2c")
        P.dma("sp", b1, D_["b1d"], cB, None, join=True)
        P.dma("sp", selA, D_["selA"], cB, None, join=True)
        P.dma("sp", selB, D_["selB"], cB, None, join=True)
        kB = P.buf("n2k")
        onesf = A("onesf", [128, 2048], F32)
        zerof = A("zerof", [128, 2048], F32)
        ident = A("ident", [128, 128], BF16)
        mneg = A("mneg", [128, 128], BF16)
        mneg2 = A("mneg2", [128, 128], BF16)
        cmask = A("cmask", [128, 16, 128], BF16)
        E = A("E", [32, 16, 128], BF16)
        E0 = A("E0", [32, 16, 128], F32)
        P.op("pool", lambda e: e.memset(onesf, 1.0), [], [kB])
        P.op("pool", lambda e: e.memset(zerof, 0.0), [kB], [kB])
        P.op("pool", lambda e: e.affine_select(out=ident, in_=onesf[:, 0:128], pattern=[[1, 128]], compare_op=ALU.is_equal, fill=0.0,
                                              base=0, channel_multiplier=-1), [kB], [kB])
        P.op("pool", lambda e: e.affine_select(out=mneg, in_=zerof[:, 0:128], pattern=[[1, 128]], compare_op=ALU.is_ge, fill=NEG,
                                              base=0, channel_multiplier=-1), [kB], [kB])
        P.op("pool", lambda e: e.affine_select(out=mneg2, in_=zerof[:, 0:128], pattern=[[-1, 128]], compare_op=ALU.is_ge, fill=NEG,
                                              base=-1, channel_multiplier=1), [kB], [kB])
        for i in range(16):
            P.op("pool", lambda e, i=i: e.affine_select(out=cmask[:, i, :], in_=zerof[:, 0:128], pattern=[[1, 128]], compare_op=ALU.is_ge,
                                                       fill=NEG, base=128 * i - 31, channel_multiplier=-16), [kB], [kB])
        P.op("pool", lambda e: e.affine_select(out=E0, in_=onesf[0:32, :].rearrange("p (a b) -> p a b", a=16), pattern=[[128, 16], [1, 128]],
                                              compare_op=ALU.is_ge, fill=0.0, base=0, channel_multiplier=-64), [kB], [kB])
        P.op("pool", lambda e: e.affine_select(out=E, in_=E0, pattern=[[-128, 16], [-1, 128]], compare_op=ALU.is_ge, fill=0.0,
                                              base=63, channel_multiplier=64), [kB], [kB])

        sqB_ = P.buf("seqdata")
        ksT = A("ksT", [128, 4, S], BF16)
        kwT = A("kwT", [128, 4, S], BF16)
        kcT = A("kcT", [128, 2, S], BF16)
        vcT = A("vcT", [128, 2, S], BF16)
        vsa = A("vsa", [128, 16, 4, 65], BF16)
        vwa = A("vwa", [128, 16, 4, 65], BF16)
        gts = A("gts", [128, 16, 48], F32)
        hT = A("hT", [128, 2, 128], BF16); hTB = P.buf("hT")
        btot = A("btot", [128, 2, 2], F32); btB = P.buf("btot")
        kcmp = A("kcmp", [128, 4, 128], BF16); kcmpB = P.buf("kcmp")
        P.op("dve", lambda e: e.memset(kcmp, 0.0), [], [kcmpB])
        vcmp = A("vcmp", [128, 4, 97], BF16); vcmpB = P.buf("vcmp")
        qt = [A("qt%d" % i, [128, 16, 128], BF16) for i in range(2)]; qtB = P.bufs_n(2, "qt")
        selT = [A("selT%d" % i, [128, 4, 128], BF16) for i in range(2)]; selTB = P.bufs_n(2, "selT")
        E128 = A("E128", [128, 16, 128], BF16)
        P.op("dve", lambda e: e.memset(E128, 0.0), [], [kB])
        for i_ in range(2):
            P.op("dve", lambda e, i_=i_: e.memset(qt[i_], 0.0), [], [qtB[i_]])
            P.op("dve", lambda e, i_=i_: e.memset(selT[i_], 0.0), [], [selTB[i_]])
        P.op("dve", lambda e: e.tensor_copy(out=E128[0:32], in_=E), [kB], [kB])
        P.op("dve", lambda e: e.memset(ksT, 0.0), [], [sqB_])
        P.op("dve", lambda e: e.memset(kwT, 0.0), [sqB_], [sqB_])
        ocs = [A("ocs%d" % i, [128, 4, 4, 64], F32) for i in range(2)]; ocsB = P.bufs_n(2, "ocs")
        xt2 = [A("xt2_%d" % i, [128, 8, 128], F32) for i in range(2)]; xt2B = P.bufs_n(2, "xt2")
        pT = [A("pT%d" % i, [128, 4, 128], BF16) for i in range(2)]; pTB = P.bufs_n(2, "pT")
        pTa = A("pTa", [128, 4, 128], BF16); pTaB = P.buf("pTa")
        sc4 = A("sc4", [128, 4, 32], F32); sc4B = P.buf("sc4")
        scr = A("scr", [128, 32], F32); scrB = P.buf("scr")
        top8 = A("top8", [128, 8], F32)
        seln = A("seln", [128, 32], BF16); selnB = P.buf("seln")
        zza = A("zza", [128, 4], F32); zzaB = P.buf("zza")
        zz = A("zz", [128, 2, 4], F32); zzB = P.buf("zz")
        otk = A("otk", [128, 1024], F32); otkB = P.buf("otk")
        tmp = A("tmpo", [128, 4, 64], F32); tmpB = P.buf("tmpo")
        tmp2 = A("tmpo2", [128, 4, 64], F32); tmp2B = P.buf("tmpo2")
        otb = A("otb", [128, 1024], BF16); otbB = P.buf("otb")
        oT = A("oT", [128, 8, 128], BF16); oTB = P.buf("oT")
        P.op("dve", lambda e: e.memset(vsa, 1.0), [], [sqB_])
        P.op("dve", lambda e: e.memset(vwa, 1.0), [sqB_], [sqB_])
        P.op("dve", lambda e: e.memset(vcmp, 1.0), [], [vcmpB])
        Xv = C.X.rearrange("c p n -> p c n")
        p6b = ps[6].bitcast(BF16)
        p7b = ps[7].bitcast(BF16)

        def stage_a(sq_, i):
            p = i % 2
            P.dma("sp", qt[p][0:64], D_["Qs"][sq_, :, :, i * 128:(i + 1) * 128], qtB[p], D_["QsB"])
            for g in range(4):
                P.op("pe", lambda e, g=g, p=p: e.matmul(ps[4][0:127, :], lhsT=kcmp[:, g, 0:127], rhs=qt[p][:, 4 * g:4 * g + 4, :],
                                                       start=True, stop=False), [kcmpB, qtB[p]], [pb[4]])
                P.op("pe", lambda e, i=i: e.matmul(ps[4][0:127, :], lhsT=ident[0:127, 0:127],
                                                   rhs=cmask[0:127, i, :].unsqueeze(1).broadcast_to([127, 4, 128]), start=False, stop=True),
                     [kB], [pb[4]])
                P.op("act", lambda e: e.activation(out=pTa[0:127], in_=ps[4][0:127, :].rearrange("p (r q) -> p r q", r=4), func=AF.Exp),
                     [pb[4]], [pTaB])
                yield
                for r in range(4):
                    P.op("pe", lambda e, r=r, g=g: e.matmul(ps[5][:, r * 97:(r + 1) * 97], lhsT=pTa[0:127, r, :], rhs=vcmp[0:127, g, :],
                                                           start=(r == 0), stop=(r == 3), skip_group_check=True), [pTaB, vcmpB], [pb[5]])
                yield
                c4 = ps[5][:, 0:388].rearrange("p (r c) -> p r c", r=4)
                P.op("dve", lambda e, c4=c4: e.tensor_scalar(out=zza, in0=c4[:, :, 96], scalar1=1e-30, scalar2=None, op0=ALU.max), [pb[5]], [zzaB])
                P.op("dve", lambda e: e.reciprocal(out=zza, in_=zza), [zzaB], [zzaB])
                P.op("dve", lambda e, c4=c4: e.tensor_tensor(out=sc4, in0=c4[:, :, 0:32], in1=zza.unsqueeze(2).broadcast_to([128, 4, 32]),
                                                           op=ALU.mult), [pb[5], zzaB], [sc4B])
                P.op("dve", lambda e: e.tensor_reduce(out=scr, in_=sc4.rearrange("p r j -> p j r"), axis=AX.X, op=ALU.add), [sc4B], [scrB])
                yield
                P.op("dve", lambda e, i=i: e.tensor_tensor(out=scr, in0=scr, in1=selA[:, i, :], op=ALU.mult), [scrB, cB], [scrB])
                P.op("dve", lambda e, i=i: e.tensor_tensor(out=scr, in0=scr, in1=selB[:, i, :], op=ALU.add), [scrB, cB], [scrB])
                P.op("dve", lambda e: e.max(out=top8, in_=scr), [scrB], [scrB])
                P.op("dve", lambda e: e.tensor_scalar(out=seln, in0=scr, scalar1=top8[:, 7:8], scalar2=1.0, op0=ALU.is_ge, op1=ALU.subtract),
                     [scrB], [selnB])
                yield
                P.op("pe", lambda e: e.transpose(out=p7b[0:32, 0:128], in_=seln, identity=ident), [selnB, kB], [pb[7]])
                gv = gts[:, i, g * 12:(g + 1) * 12].rearrange("p (r b) -> p b r", b=3)
                P.op("dve", lambda e, gv=gv: e.tensor_tensor(out=zza, in0=zza, in1=gv[:, 0, :], op=ALU.mult), [zzaB, sqB_], [zzaB])
                P.op("dve", lambda e, c4=c4, g=g, p=p: e.tensor_tensor(out=ocs[p][:, g], in0=c4[:, :, 32:96],
                                                                     in1=zza.unsqueeze(2).broadcast_to([128, 4, 64]), op=ALU.mult),
                     [pb[5], zzaB], [ocsB[p]])
                P.op("act", lambda e, g=g, p=p: e.activation(out=selT[p][0:32, g, :], in_=p7b[0:32, 0:128], func=AF.Copy, scale=-NEG),
                     [pb[7]], [selTB[p]])
                yield

        def stage_b(sq_, i):
            p = i % 2
            vcount = [0]
            for g in range(4):
                q4 = qt[p][:, 4 * g:4 * g + 4, :]
                visits = [("sel", kt) for kt in range(i + 1)] + [("win", kt) for kt in (i - 2, i - 1, i) if kt >= 0]
                firstwin = min(kt for br, kt in visits if br == "win")

                def issue_scores(br, kt):
                    pi = vcount[0] % 2
                    vcount[0] += 1
                    ks_ = slice(kt * 128, (kt + 1) * 128)
                    kk = ksT if br == "sel" else kwT
                    extra = []
                    if br == "sel":
                        extra.append((E128[:, kt, :], selT[p][:, g, :], selTB[p]))
                    if kt == i:
                        extra.append((ident, mneg, kB))
                    if br == "win" and kt == i - 2:
                        extra.append((ident, mneg2, kB))
                    P.op("pe", lambda e, pi=pi, kk=kk, ks_=ks_, g=g, q4=q4, ne=len(extra): e.matmul(
                        ps[pi], lhsT=kk[:, g, ks_], rhs=q4, start=True, stop=(ne == 0)), [sqB_, qtB[p]], [pb[pi]])
                    for ei, (ml, mr, mB) in enumerate(extra):
                        P.op("pe", lambda e, pi=pi, ml=ml, mr=mr, last=(ei == len(extra) - 1): e.matmul(
                            ps[pi], lhsT=ml, rhs=mr.unsqueeze(1).broadcast_to([mr.shape[0], 4, 128]), start=False, stop=last), [kB, mB], [pb[pi]])
                    P.op("act", lambda e, pi=pi: e.activation(out=pT[pi], in_=ps[pi].rearrange("p (r q) -> p r q", r=4), func=AF.Exp),
                         [pb[pi]], [pTB[pi]])
                    return pi

                def issue_pv(br, kt, pi):
                    ob = 2 if br == "sel" else 3
                    va_ = vsa if br == "sel" else vwa
                    for r in range(4):
                        first = (r == 0 and ((br == "sel" and kt == 0) or (br == "win" and kt == firstwin)))
                        last = (r == 3 and kt == i)
                        P.op("pe", lambda e, r=r, pi=pi, ob=ob, va_=va_, kt=kt, first=first, last=last, g=g: e.matmul(
                            ps[ob][:, r * 65:(r + 1) * 65], lhsT=pT[pi][:, r, :], rhs=va_[:, kt, g, :], start=first, stop=last,
                            skip_group_check=True), [pTB[pi], sqB_], [pb[ob]])

                prev = None
                for (br, kt) in visits:
                    pi = issue_scores(br, kt)
                    if prev is not None:
                        issue_pv(*prev)
                    prev = (br, kt, pi)
                    yield
                issue_pv(*prev)
                yield
                gv = gts[:, i, g * 12:(g + 1) * 12].rearrange("p (r b) -> p b r", b=3)
                c2 = ps[2][:, 0:260].rearrange("p (r c) -> p r c", r=4)
                c3 = ps[3][:, 0:260].rearrange("p (r c) -> p r c", r=4)
                og_ = otk[:, g * 256:(g + 1) * 256].rearrange("p (r d) -> p r d", r=4)
                for bi_, cc, bk in ((0, c2, 2), (1, c3, 3)):
                    P.op("dve", lambda e, bi_=bi_, cc=cc: e.reciprocal(out=zz[:, bi_, :], in_=cc[:, :, 64]), [pb[bk], zzB], [zzB])
                    P.op("dve", lambda e, bi_=bi_, gv=gv: e.tensor_tensor(out=zz[:, bi_, :], in0=zz[:, bi_, :], in1=gv[:, bi_ + 1, :], op=ALU.mult),
                         [zzB, sqB_], [zzB])
                P.op("dve", lambda e, c2=c2: e.tensor_tensor(out=tmp, in0=c2[:, :, 0:64], in1=zz[:, 0, :].unsqueeze(2).broadcast_to([128, 4, 64]),
                                                           op=ALU.mult), [pb[2], zzB], [tmpB])
                P.op("dve", lambda e, c3=c3: e.tensor_tensor(out=tmp2, in0=c3[:, :, 0:64], in1=zz[:, 1, :].unsqueeze(2).broadcast_to([128, 4, 64]),
                                                            op=ALU.mult), [pb[3], zzB], [tmp2B])
                me_ = "dve"
                P.op(me_, lambda e, g=g, p=p: e.tensor_tensor(out=tmp, in0=tmp, in1=ocs[p][:, g], op=ALU.add), [tmpB, ocsB[p]], [tmpB])
                P.op(me_, lambda e, og_=og_: e.tensor_tensor(out=og_, in0=tmp, in1=tmp2, op=ALU.add), [tmpB, tmp2B], [otkB])
                yield

        def stage_c(sq_, i):
            p = i % 2
            xt, xtB = xt2[p], xt2B[p]
            tok0 = sq_ * S + i * 128
            P.dma("sp", xt, Xv[:, :, tok0:tok0 + 128], xtB, C.XB)
            P.op("act", lambda e: e.activation(out=otb, in_=otk, func=AF.Copy), [otkB], [otbB])
            yield
            for yc in range(8):
                P.op("pe", lambda e, yc=yc: e.transpose(out=p6b[:, yc * 128:(yc + 1) * 128], in_=otb[:, yc * 128:(yc + 1) * 128], identity=ident),
                     [otbB, kB], [pb[6]])
            P.op("act", lambda e: e.activation(out=oT, in_=p6b[:, 0:1024].rearrange("p (c q) -> p c q", c=8), func=AF.Copy), [pb[6]], [oTB])
            yield
            for dh in range(2):
                b = 6
                for dc in range(4):
                    d_ = dh * 4 + dc
                    for yc in range(8):
                        P.op("pe", lambda e, yc=yc, d_=d_, dc=dc, b=b: e.matmul(ps[b][:, dc * 128:(dc + 1) * 128],
                                                                             lhsT=wout[:, yc, d_ * 128:(d_ + 1) * 128], rhs=oT[:, yc, :],
                                                                             start=(yc == 0 and dc == 0), stop=(yc == 7 and dc == 3),
                                                                             skip_group_check=True), [wB, oTB], [pb[b]])
                    yield
                P.op("dve", lambda e, dh=dh, b=b, xt=xt: e.tensor_tensor(out=xt[:, dh * 4:(dh + 1) * 4, :], in0=xt[:, dh * 4:(dh + 1) * 4, :],
                                                                        in1=ps[b].rearrange("p (c q) -> p c q", c=4), op=ALU.add), [xtB, pb[b]], [xtB])
                yield
            P.dma("sp", Xv[:, :, tok0:tok0 + 128], xt, C.XB, xtB, join=True)
            yield

        for sq_ in range(NSEQ):
            P.dma("sp", ksT[0:64], D_["KsT"][sq_], sqB_, D_["KsTB"], join=True)
            P.dma("sp", kwT[0:64], D_["KwT"][sq_], sqB_, D_["KwTB"], join=True)
            P.dma("sp", kcT, D_["KcT"][sq_], sqB_, D_["KcTB"], join=True)
            P.dma("sp", vcT, D_["VcT"][sq_], sqB_, D_["VcTB"], join=True)
            for g in range(4):
                P.dma("sp", vsa[:, :, g, 0:64], D_["Vs"][sq_, :, g * 64:(g + 1) * 64].rearrange("(k p) d -> p k d", p=128), sqB_, D_["VsB"], join=True)
                P.dma("sp", vwa[:, :, g, 0:64], D_["Vw"][sq_, :, g * 64:(g + 1) * 64].rearrange("(k p) d -> p k d", p=128), sqB_, D_["VwB"], join=True)
            P.dma("sp", gts, D_["G"][sq_].rearrange("(k p) c -> p k c", p=128), sqB_, D_["GB"], join=True)
            if sq_ == 0:
                for kv in range(2):
                    for hc in range(2):
                        for l in range(32):
                            P.op("pe", lambda e, kv=kv, hc=hc, l=l: e.matmul(ps[7][:, 2 * kv + hc:2 * kv + hc + 1],
                                                                           lhsT=w1[0:64, kv, l, hc * 128:(hc + 1) * 128], rhs=peT[0:64, kv, l:l + 1],
                                                                           start=(l == 0), stop=(l == 31)), [wB], [pb[7]])
                P.op("dve", lambda e: e.tensor_tensor(out=btot, in0=b1, in1=ps[7][:, 0:4].rearrange("p (a b) -> p a b", a=2), op=ALU.add),
                     [pb[7], cB], [btB])
                for g in range(4):
                    P.dma("pool", vcmp[:, g, 0:32], D_["mseld"], vcmpB, None, join=True)
            kk_ = 0
            for kv, src in enumerate([kcT, vcT]):
                for g in range(4):
                    r0 = (g % 2) * 64
                    ch = g // 2
                    for hc in range(2):
                        b = kk_ % 4
                        kk_ += 1
                        for l in range(32):
                            P.op("pe", lambda e, kv=kv, hc=hc, l=l, r0=r0, ch=ch, src=src, b=b: e.matmul(
                                ps[b][:, 0:127], lhsT=w1[r0:r0 + 64, kv, l, hc * 128:(hc + 1) * 128],
                                rhs=src[r0:r0 + 64, ch, l:l + 16 * 126 + 1:16], start=(l == 0), stop=(l == 31)), [wB, sqB_], [pb[b]])
                        P.op("act", lambda e, kv=kv, hc=hc, b=b: e.activation(out=hT[:, hc, 0:127], in_=ps[b][:, 0:127], func=AF.Gelu_apprx_tanh,
                                                                           bias=btot[:, kv, hc:hc + 1]), [pb[b], btB], [hTB])
                    if kv == 0:
                        for hc in range(2):
                            P.op("pe", lambda e, hc=hc: e.matmul(ps[7][0:64, 0:127], lhsT=w2k[:, hc, :], rhs=hT[:, hc, 0:127],
                                                                 start=(hc == 0), stop=(hc == 1)), [wB, hTB], [pb[7]])
                        P.op("act", lambda e, g=g: e.activation(out=kcmp[0:64, g, 0:127], in_=ps[7][0:64, 0:127], func=AF.Copy), [pb[7]], [kcmpB])
                    else:
                        for hc in range(2):
                            P.op("pe", lambda e, hc=hc: e.matmul(ps[7][0:127, 0:64], lhsT=hT[:, hc, 0:127], rhs=w2v[:, hc, :],
                                                                 start=(hc == 0), stop=(hc == 1)), [wB, hTB], [pb[7]])
                        P.op("act", lambda e, g=g: e.activation(out=vcmp[0:127, g, 32:96], in_=ps[7][0:127, 0:64], func=AF.Copy), [pb[7]], [vcmpB])
            dbg = int(os.environ.get("NSADBG", "9"))
            if dbg >= 1:
                interleave([stage_a(sq_, 0)])
            for i in range(NQ):
                if dbg == 1:
                    interleave([stage_a(sq_, i + 1) if i + 1 < NQ else None])
                elif dbg == 2:
                    interleave([stage_b(sq_, i)])
                    interleave([stage_a(sq_, i + 1) if i + 1 < NQ else None])
                elif dbg >= 3:
                    interleave([stage_c(sq_, i - 1) if i >= 1 else None, stage_b(sq_, i), stage_a(sq_, i + 1) if i + 1 < NQ else None])
            if dbg >= 3:
                interleave([stage_c(sq_, NQ - 1)])
        P.barrier()


def perm_gu(w):
    wg = w[:, :DFF].reshape(8, 128, NF, 128)
    wu = w[:, DFF:].reshape(8, 128, NF, 128)
    o = np.stack([wg, wu], axis=3)
    o = o.transpose(2, 1, 0, 3, 4)
    return np.ascontiguousarray(o.reshape(NF, 128, 8 * 256))


def perm_vec(v):
    return np.ascontiguousarray(v.reshape(8, 128).T)


def build(upto="all"):
    nc = bass.Bass("TRN2", target_bir_lowering=False)
    P = Prog(nc)
    C = Ctx()
    C.P, C.nc = P, nc
    dram_in = lambda n, shp: nc.dram_tensor(n, list(shp), F32, kind="ExternalInput").ap()
    xin = dram_in("xin", [8, 128, NTOK])
    nrm_d = dram_in("nrm", [128, 56])
    wgu_d = [dram_in("wgu%d" % i, [NF, 128, 2048]) for i in range(4)]
    wd_d = [dram_in("wd%d" % i, [NF, 128, D]) for i in range(4)]
    C.out = nc.dram_tensor("out", [8, 128, NTOK], F32, kind="ExternalOutput").ap()
    C.outB = P.buf("out", keep=True)
    C.X = nc.dram_tensor("X", [8, 128, NTOK], F32, kind="Internal").ap()
    C.XB = P.buf("X", keep=True)

    with ExitStack() as es0:
        C.ps = [es0.enter_context(nc.psum_tensor("ps%d" % i, [128, 512], F32)).ap() for i in range(8)]
        C.pb = [P.buf("ps%d" % i, keep=True) for i in range(8)]
        C.constB = P.buf("const", keep=True)
        C.ones_bf = sb(es0, nc, "ones_bf", [128, 128], BF16)
        C.eps_t = sb(es0, nc, "eps_t", [128, 1], F32)
        C.nrm = sb(es0, nc, "nrm_sb", [128, 56], F32)
        P.op("dve", lambda e: e.memset(C.ones_bf, 1.0), [], [C.constB])
        P.op("dve", lambda e: e.memset(C.eps_t, EPS), [C.constB], [C.constB])
        nrmB = P.buf("nrmld")
        P.dma("sp", C.nrm, nrm_d, nrmB, None)
        P.barrier()
        C.constB.w = None
        C.constB.rd = []
        g = lambda i: C.nrm[:, i * 8:(i + 1) * 8]
        D_ = {}
        for n, shp in [("ab_win", [128, 8, 3080]), ("ab_wout", [128, 8, D]), ("rgw", [128, 4, 2, 128]), ("rgc", [128, 4, 8]),
                       ("mlc", [128, 8, 5]), ("mlrow", [128, 520])]:
            D_[n] = dram_in(n, shp)
        for n, shp in [("nsa_win", [128, 8, NCOLN]), ("nsa_wout", [128, 8, D]), ("w1d", [128, 2, 32, 256]), ("w2kd", [128, 2, 64]),
                       ("w2vd", [128, 2, 64]), ("peTd", [128, 2, 32]), ("b1d", [128, 2, 2]), ("mseld", [128, 32]),
                       ("selA", [128, 16, 32]), ("selB", [128, 16, 32]), ("bgrow", [128, 48])]:
            D_[n] = dram_in(n, shp)
        for n, shp, dt in [("Qs", [NSEQ, 64, 16, S], BF16), ("KsT", [NSEQ, 64, 4, S], BF16), ("KwT", [NSEQ, 64, 4, S], BF16),
                           ("KcT", [NSEQ, 128, 2, S], BF16), ("VcT", [NSEQ, 128, 2, S], BF16), ("Vs", [NSEQ, S, 256], BF16),
                           ("Vw", [NSEQ, S, 256], BF16), ("G", [NSEQ, S, 48], F32)]:
            D_[n] = nc.dram_tensor("scr_" + n, shp, dt, kind="Internal").ap()
            D_[n + "B"] = P.buf("scr_" + n, keep=True)
        stages = ["ffn1", "mix0", "ffn2", "ffn1b", "mix1", "all"]
        if upto in ("n1only", "n2only", "abonly"):
            if upto == "abonly":
                ab_phase(C, g(1), D_)
            else:
                nsa_proj_phase(C, g(4), D_)
                if upto == "n2only":
                    nsa_core_phase(C, D_)
            final_phase(C, g(6))
            P.emit([C.outB])
            return nc, P
        ns = stages.index(upto) + 1
        ffn_phase(C, es0, [(g(0), wgu_d[0], wd_d[0], (g(6) if ns == 1 else None), xin)])
        if ns >= 2:
            ab_phase(C, g(1), D_)
        if ns >= 4:
            ffn_phase(C, es0, [(g(2), wgu_d[1], wd_d[1], None, C.X), (g(3), wgu_d[2], wd_d[2], None, C.X)])
        elif ns >= 3:
            ffn_phase(C, es0, [(g(2), wgu_d[1], wd_d[1], None, C.X)])
        if ns >= 5:
            nsa_proj_phase(C, g(4), D_)
            nsa_core_phase(C, D_)
        if ns >= 6:
            ffn_phase(C, es0, [(g(5), wgu_d[3], wd_d[3], g(6), C.X)])
        elif ns >= 2:
            final_phase(C, g(6))
        P.emit([C.outB])
    return nc, P


_CACHE = {}


def kernel(upto="all", **inp):
    x = np.asarray(inp["x"], np.float32)
    B = x.shape[0]
    ncores = B // NSEQ
    if upto not in _CACHE:
        _CACHE[upto] = build(upto)
    nc, P = _CACHE[upto]
    nrm = np.concatenate([perm_vec(inp["ffn1_norm"][0]), perm_vec(inp["mix_norm"][0]), perm_vec(inp["ffn2_norm"][0]),
                          perm_vec(inp["ffn1_norm"][1]), perm_vec(inp["mix_norm"][1]), perm_vec(inp["ffn2_norm"][1]),
                          perm_vec(inp["final_norm"])], axis=1).astype(np.float32)
    shared = {"nrm": np.ascontiguousarray(nrm)}
    ffns = [("ffn1", 0), ("ffn2", 0), ("ffn1", 1), ("ffn2", 1)]
    for i, (nm, l) in enumerate(ffns):
        shared["wgu%d" % i] = perm_gu(np.asarray(inp[nm + "_w_gu"][l], np.float32))
        shared["wd%d" % i] = np.ascontiguousarray(np.asarray(inp[nm + "_w_down"][l], np.float32).reshape(NF, 128, D))
    f32 = lambda a: np.ascontiguousarray(np.asarray(a, np.float32))
    shared["ab_win"] = f32(inp["ab_w_in"][0].reshape(8, 128, 3080).transpose(1, 0, 2))
    shared["ab_wout"] = f32(inp["ab_w_out"][0].reshape(8, 128, D).transpose(1, 0, 2))
    rgw = np.zeros((128, 4, 2, 128), np.float32)
    for c in range(4):
        for k, nm in enumerate(["rg_w_r", "rg_w_i"]):
            rgw[0:64, c, k, 0:64] = inp[nm][0][2 * c]
            rgw[64:128, c, k, 64:128] = inp[nm][0][2 * c + 1]
    shared["rgw"] = rgw
    pc = lambda v, n: np.asarray(v, np.float32).reshape(n, 128).T
    rgc = np.stack([pc(inp["rg_conv_w"][0][j], 4) for j in range(4)] + [pc(inp["rg_conv_b"][0], 4), pc(inp["rg_b_r"][0], 4),
                   pc(inp["rg_b_i"][0], 4), pc(inp["rg_lambda"][0], 4)], axis=2)
    shared["rgc"] = f32(rgc)
    mlc = np.stack([pc(inp["ml_conv_w"][0][j], 8) for j in range(4)] + [pc(inp["ml_conv_b"][0], 8)], axis=2)
    shared["mlc"] = f32(mlc)
    row = np.concatenate([inp["ml_b_i"][0], inp["ml_b_f"][0], inp["ml_norm"][0]]).astype(np.float32)
    shared["mlrow"] = f32(np.tile(row[None, :], (128, 1)))
    W = np.asarray(inp["nsa_w_in"][0], np.float32)
    kcW, vcW, ksW, vsW, kwW, vwW = [W[:, 1024 + 256 * i_:1280 + 256 * i_] for i_ in range(6)]
    Wp = np.concatenate([W[:, 0:1024], ksW, kwW, kcW, vcW, vsW, vwW, W[:, 2560:2608]], axis=1)
    assert Wp.shape == (1024, NCOLN)
    shared["nsa_win"] = f32(Wp.reshape(8, 128, NCOLN).transpose(1, 0, 2))
    shared["nsa_wout"] = f32(inp["nsa_w_out"][0].reshape(8, 128, D).transpose(1, 0, 2))
    w1d = np.zeros((128, 2, 32, 256), np.float32)
    peTd = np.zeros((128, 2, 32), np.float32)
    b1d = np.zeros((128, 2, 2), np.float32)
    for kv_, (w1n, pen, b1n) in enumerate([("nsa_k_w1", "nsa_pe_k", "nsa_k_b1"), ("nsa_v_w1", "nsa_pe_v", "nsa_v_b1")]):
        w1_ = np.asarray(inp[w1n][0], np.float32).reshape(32, 64, 256).transpose(1, 0, 2)
        w1d[0:64, kv_] = w1_
        w1d[64:128, kv_] = w1_
        pe_ = np.asarray(inp[pen][0], np.float32).T
        peTd[0:64, kv_] = pe_
        peTd[64:128, kv_] = pe_
        b1d[:, kv_, :] = np.asarray(inp[b1n][0], np.float32).reshape(2, 128).T
    shared["w1d"], shared["peTd"], shared["b1d"] = w1d, peTd, b1d
    w2k_ = np.asarray(inp["nsa_k_w2"][0], np.float32).reshape(2, 128, 64).transpose(1, 0, 2)
    shared["w2kd"] = f32(w2k_)
    shared["w2vd"] = f32(np.asarray(inp["nsa_v_w2"][0], np.float32).reshape(2, 128, 64).transpose(1, 0, 2))
    ncmp = (S - 32) // 16 + 1
    cs_ = np.arange(ncmp)[:, None] * 16
    ss_ = np.arange(32)[None, :] * 64
    ov = np.clip(np.minimum(cs_ + 32, ss_ + 64) - np.maximum(cs_, ss_), 0, None) / 32.0
    msel = np.zeros((128, 32), np.float32)
    msel[:ncmp] = ov
    shared["mseld"] = msel
    tpos = (np.arange(16)[None, :] * 128 + np.arange(128)[:, None])[:, :, None]
    jj = np.arange(32)[None, None, :]
    cur = tpos // 64
    valid = (jj * 64 <= tpos)
    forced = ((jj == 0) | (jj == cur) | (jj == cur - 1)) & valid
    shared["selA"] = f32((valid & ~forced).astype(np.float32))
    shared["selB"] = f32(1e6 * forced.astype(np.float32) - (1.0 - valid.astype(np.float32)))
    shared["bgrow"] = f32(np.tile(np.asarray(inp["nsa_b_gate"][0], np.float32)[None, :], (128, 1)))
    in_maps = []
    for c in range(ncores):
        xs = x[c * NSEQ:(c + 1) * NSEQ].reshape(NTOK, 8, 128).transpose(1, 2, 0)
        m = dict(shared)
        m["xin"] = np.ascontiguousarray(xs)
        in_maps.append(m)
    res = run_bass_kernel_spmd(nc, in_maps, core_ids=list(range(ncores)))
    outs = []
    for c in range(ncores):
        o = res.results[c]["out"]
        outs.append(o.transpose(2, 0, 1).reshape(NSEQ, S, D))
    return np.ascontiguousarray(np.concatenate(outs, axis=0))
```

```python
import os
from contextlib import ExitStack
import numpy as np
import concourse.bass as bass
import concourse.mybir as mybir
from concourse.bass_utils import run_bass_kernel_spmd

F32 = mybir.dt.float32
BF16 = mybir.dt.bfloat16
ALU = mybir.AluOpType
AF = mybir.ActivationFunctionType
AX = mybir.AxisListType

D = 1024
S = 2048
NSEQ = 2
NTOK = NSEQ * S
DFF = 2816
NF = DFF // 128
NCORES = 8
EPS = 1e-6


class Buf:
    __slots__ = ("name", "w", "rd", "sem", "cnt", "keep", "q")

    def __init__(self, name, keep=False):
        self.name = name
        self.keep = keep
        self.w = None
        self.rd = []
        self.sem = None
        self.cnt = 0


class Op:
    __slots__ = ("eng", "fn", "waits", "marked", "semval", "dma")

    def __init__(self, eng, fn):
        self.eng = eng
        self.fn = fn
        self.waits = []
        self.marked = False
        self.semval = None
        self.dma = None


class Prog:
    ENGS = ("pe", "act", "dve", "pool", "sp")
    CENGS = ("pe", "act", "dve", "pool")

    def __init__(self, nc):
        self.nc = nc
        self.lists = {e: [] for e in self.ENGS}
        self.esem = {e: nc.alloc_semaphore("es_" + e) for e in self.CENGS}
        self.lastc = {e: None for e in self.CENGS}
        self.bufs = []
        self.nsem = 0
        self.sempool = {}

    def buf(self, name="b", keep=False):
        b = Buf(name, keep)
        self.bufs.append(b)
        return b

    def bufs_n(self, n, name="b"):
        return [self.buf("%s%d" % (name, i)) for i in range(n)]

    def _deps(self, o, reads, writes):
        toks = []
        for b in reads:
            if b.w is not None:
                toks.append(b.w)
        for b in writes:
            if b.w is not None:
                toks.append(b.w)
            toks.extend(b.rd)
        seen = set()
        for t in toks:
            if id(t) in seen:
                continue
            seen.add(id(t))
            if t[0] == "e":
                p = t[1]
                if p is o:
                    continue
                if p.eng == "pe" and o.eng == "pe":
                    continue
                p.marked = True
            o.waits.append(t)

    def op(self, eng, fn, reads=(), writes=()):
        o = Op(eng, fn)
        self._deps(o, reads, writes)
        tok = ("e", o)
        for b in reads:
            b.rd.append(tok)
        for b in writes:
            b.w = tok
            b.rd = []
        self.lists[eng].append(o)
        self.lastc[eng] = o
        return o

    def dma(self, q, out, in_, dst, src, join=False, **kw):
        o = Op(q, None)
        if dst.sem is None:
            pool_ = self.sempool.setdefault(q, [])
            if pool_ and not os.environ.get("NOPOOL"):
                dst.sem, dst.cnt = pool_.pop()
                dst.q = q
            else:
                dst.sem = self.nc.alloc_semaphore("ds_%d" % self.nsem)
                self.nsem += 1
                dst.q = q
        assert dst.q == q, "one DMA queue per buffer"
        srcs = [src] if src is not None else []
        if join and dst.w is not None and dst.w[0] == "d" and dst.w[1] is dst:
            saved = dst.w
            dst.w = None
            self._deps(o, srcs, [dst])
            dst.w = saved
        else:
            self._deps(o, srcs, [dst])
        dst.cnt += 16
        tok = ("d", dst, dst.cnt)
        dst.w = tok
        dst.rd = []
        if src is not None:
            src.rd.append(tok)
        o.dma = (out, in_, dst, kw)
        self.lists[q].append(o)
        return o

    def barrier(self):
        toks = []
        for e in self.CENGS:
            if self.lastc[e] is not None:
                self.lastc[e].marked = True
                toks.append(("e", self.lastc[e]))
        for b in self.bufs:
            if b.sem is not None and b.cnt > 0:
                toks.append(("d", b, b.cnt))
        for e in self.ENGS:
            o = Op(e, None)
            o.waits = [t for t in toks if not (t[0] == "e" and t[1].eng == e)]
            self.lists[e].append(o)
        kept = []
        for b in self.bufs:
            if b.keep:
                kept.append(b)
            elif b.sem is not None:
                self.sempool.setdefault(b.q, []).append((b.sem, b.cnt))
        self.bufs = kept

    def emit(self, final_bufs=()):
        nc = self.nc
        for e in self.CENGS:
            c = 0
            for o in self.lists[e]:
                if o.marked:
                    c += 1
                    o.semval = c
        fin = Op("sp", None)
        for b in final_bufs:
            if b.w is not None:
                fin.waits.append(b.w)
        self.lists["sp"].append(fin)

        def run(engname, eng):
            seen = {}
            for o in self.lists[engname]:
                for t in o.waits:
                    if t[0] == "e":
                        sem, val = self.esem[t[1].eng], t[1].semval
                    else:
                        sem, val = t[1].sem, t[2]
                    if seen.get(sem.num, 0) < val:
                        eng.wait_ge(sem, val)
                        seen[sem.num] = val
                if o.dma is not None:
                    out, in_, dst, kw = o.dma
                    eng.dma_start(out=out, in_=in_, **kw).then_inc(dst.sem, 16)
                elif o.fn is not None:
                    ins = o.fn(eng)
                    if o.marked:
                        ins.then_inc(self.esem[engname], 1)

        with nc.Block() as block:
            @block.tensor
            def _(eng):
                run("pe", eng)

            @block.scalar
            def _(eng):
                run("act", eng)

            @block.vector
            def _(eng):
                run("dve", eng)

            @block.gpsimd
            def _(eng):
                run("pool", eng)

            @block.sync
            def _(eng):
                run("sp", eng)

    def stats(self):
        return {e: len(l) for e, l in self.lists.items()}


class Ctx:
    pass


_SBN = [0]


def sb(es, nc, name, shape, dt):
    _SBN[0] += 1
    return es.enter_context(nc.sbuf_tensor("s%d_%s" % (_SBN[0], name), list(shape), dt)).ap()


def rmsnorm_fm(C, x3, xB, g2, out3, outB, sq3, sqB, T):
    P, ps, pb = C.P, C.ps, C.pb
    rs, rstd, rsB, rstdB = C.rs, C.rstd, C.rsB, C.rstdB
    nh = T // 512
    for kc in range(8):
        P.op("act", lambda e, kc=kc: e.activation(out=sq3[:, kc, 0:T], in_=x3[:, kc, 0:T], func=AF.Square),
             [xB], [sqB[kc]])
    for hf in range(nh):
        bank = 6 + (hf % 2)
        for kc in range(8):
            P.op("pe", lambda e, kc=kc, hf=hf, bank=bank: e.matmul(
                ps[bank], lhsT=C.ones_bf, rhs=sq3[:, kc, hf * 512:(hf + 1) * 512], start=(kc == 0), stop=(kc == 7)),
                [sqB[kc], C.constB], [pb[bank]])
        P.op("act", lambda e, hf=hf, bank=bank: e.activation(
            out=rs[:, hf * 512:(hf + 1) * 512], in_=ps[bank], func=AF.Sqrt, scale=1.0 / D, bias=C.eps_t[:, 0:1]),
            [pb[bank], C.constB], [rsB])
    P.op("dve", lambda e: e.reciprocal(out=rstd[:, 0:T], in_=rs[:, 0:T]), [rsB], [rstdB])
    for kc in range(8):
        P.op("dve", lambda e, kc=kc: e.scalar_tensor_tensor(
            out=out3[:, kc, 0:T], in0=x3[:, kc, 0:T], scalar=g2[:, kc:kc + 1], in1=rstd[:, 0:T],
            op0=ALU.mult, op1=ALU.mult), [xB, rstdB, C.constB], [outB[kc] if isinstance(outB, list) else outB])


def rmsnorm_fm_h(C, x3, xB, g2, out3, outB, sq3, sqB, T):
    for _ in rmsnorm_fm_g(C, x3, xB, g2, out3, outB, sq3, sqB, T):
        pass


def rmsnorm_fm_g(C, x3, xB, g2, out3, outB, sq3, sqB, T):
    P, ps, pb = C.P, C.ps, C.pb
    rs, rstd, rsB, rstdB = C.rs, C.rstd, C.rsB, C.rstdB
    for hf in range(T // 512):
        cs = slice(hf * 512, (hf + 1) * 512)
        bank = 6 + (hf % 2)
        for kc in range(8):
            P.op("act", lambda e, kc=kc, cs=cs: e.activation(out=sq3[:, kc, :], in_=x3[:, kc, cs], func=AF.Square), [xB], [sqB[kc]])
            if kc % 4 == 3:
                yield
        for kc in range(8):
            P.op("pe", lambda e, kc=kc, bank=bank: e.matmul(ps[bank], lhsT=C.ones_bf, rhs=sq3[:, kc, :], start=(kc == 0), stop=(kc == 7)),
                 [sqB[kc], C.constB], [pb[bank]])
        P.op("act", lambda e, cs=cs, bank=bank: e.activation(out=rs[:, cs], in_=ps[bank], func=AF.Sqrt, scale=1.0 / D, bias=C.eps_t[:, 0:1]),
             [pb[bank], C.constB], [rsB])
        yield
        P.op("dve", lambda e, cs=cs: e.reciprocal(out=rstd[:, cs], in_=rs[:, cs]), [rsB], [rstdB])
        yield
        for kc in range(8):
            P.op("dve", lambda e, kc=kc, cs=cs: e.scalar_tensor_tensor(
                out=out3[:, kc, cs], in0=x3[:, kc, cs], scalar=g2[:, kc:kc + 1], in1=rstd[:, cs],
                op0=ALU.mult, op1=ALU.mult), [xB, rstdB, C.constB], [outB[kc] if isinstance(outB, list) else outB])
            if kc % 2 == 1:
                yield


def ffn_phase(C, es0, cfgs):
    P, nc, ps, pb = C.P, C.nc, C.ps, C.pb
    T = 1024
    NT = NTOK // T
    with ExitStack() as es:
        xt = [sb(es, nc, "xt%d" % i, [128, 8, T], F32) for i in range(2)]
        xtB = P.bufs_n(2, "xt")
        xn = sb(es, nc, "xn", [128, 8, T], BF16)
        xnB = P.bufs_n(8, "xn")
        h = sb(es, nc, "h", [128, NF, T], BF16)
        hB = P.bufs_n(NF, "h")
        wd = sb(es, nc, "wd", [128, NF, D], BF16)
        wdB = P.bufs_n(NF, "wd")
        NR = 4
        wg = [sb(es, nc, "wg%d" % i, [128, 2048], BF16) for i in range(NR)]
        wgB = P.bufs_n(NR, "wg")
        sl = [sb(es, nc, "sl%d" % i, [128, 512], F32) for i in range(2)]
        slB = P.bufs_n(2, "sl")
        nsq = sb(es, nc, "nsq", [128, 8, 512], BF16)
        nsqB = P.bufs_n(8, "nsq")
        C.rs = sb(es, nc, "rs", [128, T], F32)
        C.rstd = sb(es, nc, "rstd", [128, T], F32)
        C.rsB, C.rstdB = P.buf("rs"), P.buf("rstd")

        def run_one(gain, wgu_d, wd_d, final, xsrc):
            Xv = C.X.rearrange("c p n -> p c n")
            Sv = xsrc.rearrange("c p n -> p c n")
            sB = C.XB if xsrc is C.X else None
            Ov = C.out.rearrange("c p n -> p c n")
            def load(t):
                P.dma("sp", xt[t % 2], Sv[:, :, t * T:(t + 1) * T], xtB[t % 2], sB)

            load(0)
            load(1)
            gi = 0
            for t in range(NT):
                x3, xB = xt[t % 2], xtB[t % 2]
                if t == 0:
                    rmsnorm_fm_h(C, x3, xB, gain, xn, xnB, nsq, nsqB, T)
                for fi in range(NF):
                    slot = gi % NR
                    gi += 1
                    P.dma("pool", wg[slot], wgu_d[fi], wgB[slot], None)
                    if t == 0 and fi == NR - 1:
                        for fj in range(NF):
                            P.dma("pool", wd[:, fj, :], wd_d[fj], wdB[fj], None)
                    for hf in range(2):
                        pg, pu = 2 * hf, 2 * hf + 1
                        cs = slice(hf * 512, (hf + 1) * 512)
                        for kc in range(8):
                            P.op("pe", lambda e, kc=kc, slot=slot, cs=cs, pg=pg: e.matmul(
                                ps[pg], lhsT=wg[slot][:, kc * 256:kc * 256 + 128], rhs=xn[:, kc, cs],
                                start=(kc == 0), stop=(kc == 7)), [wgB[slot], xnB[kc]], [pb[pg]])
                        for kc in range(8):
                            P.op("pe", lambda e, kc=kc, slot=slot, cs=cs, pu=pu: e.matmul(
                                ps[pu], lhsT=wg[slot][:, kc * 256 + 128:kc * 256 + 256], rhs=xn[:, kc, cs],
                                start=(kc == 0), stop=(kc == 7)), [wgB[slot], xnB[kc]], [pb[pu]])
                        P.op("act", lambda e, hf=hf, pg=pg: e.activation(out=sl[hf], in_=ps[pg], func=AF.Silu),
                             [pb[pg]], [slB[hf]])
                        P.op("dve", lambda e, hf=hf, pu=pu, fi=fi, cs=cs: e.tensor_tensor(
                            out=h[:, fi, cs], in0=sl[hf], in1=ps[pu], op=ALU.mult), [slB[hf], pb[pu]], [hB[fi]])
                def down_gen(x3=x3, xB=xB):
                    k = 0
                    for dc in range(8):
                        for hf in range(2):
                            po = 4 + (k % 2)
                            k += 1
                            cs = slice(hf * 512, (hf + 1) * 512)
                            for fi in range(NF):
                                P.op("pe", lambda e, fi=fi, dc=dc, cs=cs, po=po: e.matmul(
                                    ps[po], lhsT=wd[:, fi, dc * 128:(dc + 1) * 128], rhs=h[:, fi, cs],
                                    start=(fi == 0), stop=(fi == NF - 1)), [wdB[fi], hB[fi]], [pb[po]])
                            P.op("dve", lambda e, dc=dc, cs=cs, po=po, x3=x3: e.scalar_tensor_tensor(
                                out=x3[:, dc, cs], in0=ps[po], scalar=0.5, in1=x3[:, dc, cs], op0=ALU.mult, op1=ALU.add),
                                [pb[po], xB], [xB])
                            yield

                ngen = None
                if t + 1 < NT:
                    ngen = rmsnorm_fm_g(C, xt[(t + 1) % 2], xtB[(t + 1) % 2], gain, xn, xnB, nsq, nsqB, T)
                interleave([down_gen(), ngen])
                if final is None:
                    P.dma("sp", Xv[:, :, t * T:(t + 1) * T], x3, C.XB, xB, join=True)
                else:
                    rmsnorm_fm_h(C, x3, xB, final, x3, xB, nsq, nsqB, T)
                    P.dma("sp", Ov[:, :, t * T:(t + 1) * T], x3, C.outB, xB, join=True)
                if t + 2 < NT:
                    load(t + 2)

        for cfg in cfgs:
            run_one(*cfg)
        P.barrier()


def ab_phase(C, gain, D_):
    P, nc, ps, pb = C.P, C.nc, C.ps, C.pb
    T = 512
    NT = NTOK // T
    TPS = S // T
    QS = 128.0 ** -0.5
    with ExitStack() as es:
        A = lambda name, shape, dt: sb(es, nc, name, shape, dt)
        win = A("win", [128, 8, 3080], BF16)
        wout = A("wout", [128, 8, D], BF16)
        rgw = A("rgw", [128, 4, 2, 128], BF16)
        rgc = A("rgc", [128, 4, 8], F32)
        mlc = A("mlc", [128, 8, 5], F32)
        mlrow = A("mlrow", [128, 8 + 512], F32)
        cneg = A("cneg", [128, 4], F32)
        U = A("U", [128, 128], BF16)
        mneg = A("mneg", [128, 128], BF16)
        ident = A("ident", [128, 128], BF16)
        onesf = A("onesf", [128, 128], F32)
        zerof = A("zerof", [128, 128], F32)
        wB = P.buf("abw")
        for kc in range(8):
            P.dma("pool", win[:, kc, :], D_["ab_win"][:, kc, :], wB, None, join=True)
        P.dma("pool", wout, D_["ab_wout"], wB, None, join=True)
        P.dma("pool", rgw, D_["rgw"], wB, None, join=True)
        cB = P.buf("abc")
        P.dma("sp", rgc, D_["rgc"], cB, None, join=True)
        P.dma("sp", mlc, D_["mlc"], cB, None, join=True)
        P.dma("sp", mlrow, D_["mlrow"], cB, None, join=True)
        kB = P.buf("abk")
        P.op("pool", lambda e: e.memset(onesf, 1.0), [], [kB])
        P.op("pool", lambda e: e.memset(zerof, 0.0), [kB], [kB])
        P.op("pool", lambda e: e.affine_select(out=U, in_=onesf, pattern=[[1, 128]], compare_op=ALU.is_ge, fill=0.0,
                                              base=0, channel_multiplier=-1), [kB], [kB])
        P.op("pool", lambda e: e.affine_select(out=mneg, in_=zerof, pattern=[[1, 128]], compare_op=ALU.is_ge,
                                              fill=-30000.0, base=0, channel_multiplier=-1), [kB], [kB])
        P.op("pool", lambda e: e.affine_select(out=ident, in_=onesf, pattern=[[1, 128]], compare_op=ALU.is_equal,
                                              fill=0.0, base=0, channel_multiplier=-1), [kB], [kB])
        P.op("act", lambda e: e.activation(out=cneg, in_=rgc[:, :, 7], func=AF.Exp, scale=-1.0), [cB], [kB])
        P.op("act", lambda e: e.activation(out=cneg, in_=cneg, func=AF.Ln, bias=1.0), [kB], [kB])
        P.op("dve", lambda e: e.tensor_scalar(out=cneg, in0=cneg, scalar1=-8.0, scalar2=None, op0=ALU.mult), [kB], [kB])

        xt2 = [A("xt%d" % i_, [128, 8, T], F32) for i_ in range(2)]; xt2B = P.bufs_n(2, "xt")
        xn = A("xn", [128, 8, T], BF16); xnB = P.bufs_n(8, "xn")
        yT = A("yT", [128, 8, T], BF16); yTB = P.bufs_n(8, "yT")
        C.rs = A("rs", [128, T], F32); C.rstd = A("rstd", [128, T], F32)
        C.rsB, C.rstdB = P.buf("rs"), P.buf("rstd")
        xa = A("xa", [128, 4, T + 3], F32); xaB = P.bufs_n(4, "xa")
        qk = A("qk", [128, 8, T + 3], F32); qkB = P.bufs_n(8, "qk")
        xc = A("xc", [128, T], F32); xcB = P.buf("xc")
        xcb = A("xcb", [128, T], BF16); xcbB = P.buf("xcb")
        rr = A("rr", [128, T], F32); rrB = P.buf("rr")
        ig = A("ig", [128, T], F32); igB = P.buf("ig")
        aa = A("aa", [128, T], F32); aaB = P.buf("aa")
        uu = A("uu", [128, T], F32); uuB = P.buf("uu")
        hs = A("hs", [128, T], F32); hsB = P.buf("hs")
        gg = A("gg", [128, T], F32); ggB = P.buf("gg")
        hst = A("hst", [128, 4], F32); hstB = P.bufs_n(4, "hst")
        qc = A("qc", [128, T], F32); qcB = P.buf("qc")
        qT = A("qT", [128, 4, T], BF16); qTB = P.bufs_n(4, "qT")
        kT = A("kT", [128, 4, T], BF16); kTB = P.bufs_n(4, "kT")
        va = A("va", [128, 4, 4, 129], BF16); vaB = P.bufs_n(4, "va")
        og = A("og", [128, 4, 512], F32); ogB = P.bufs_n(4, "og")
        gt = A("gt", [128, 4, 8], F32); gtB = P.bufs_n(4, "gt")
        lhi = A("lhi", [128, 4], BF16); llo = A("llo", [128, 4], BF16); lhf = A("lhf", [128, 4], F32)
        lB = P.buf("lhl")
        Lh = A("Lh", [128, 4, 128], BF16); Ll = A("Ll", [128, 4, 128], BF16); LB = P.buf("L")
        sm = A("sm", [128, 24], F32); smB = P.buf("sm")
        DT = A("DT", [128, 4, 128], F32); DTB = P.buf("DT")
        AT = A("AT", [128, 4, 128], BF16); ATB = P.buf("AT")
        kw = A("kw", [128, 4, 128], BF16); kwB = P.buf("kw")
        itr = A("itr", [128, 4, 129], F32); itrB = P.buf("itr")
        tot = A("tot", [128, 4, 129], F32); totB = P.buf("tot")
        hh = A("hh", [128, 4, 128], F32); hhB = P.buf("hh")
        h2 = A("h2", [128, 4, 128], F32); h2B = P.buf("h2")
        ytk = A("ytk", [128, 512], BF16); ytkB = P.buf("ytk")
        Cst = A("Cst", [128, 4, 129], F32); CstB = P.buf("Cst")
        Cbf = A("Cbf", [128, 4, 129], BF16); CbfB = P.buf("Cbf")
        P.op("pool", lambda e: e.memset(va, 1.0), [], vaB)
        Xv = C.X.rearrange("c p n -> p c n")
        bank_rr = [0]

        def nb():
            bank_rr[0] = (bank_rr[0] + 1) % 6
            return bank_rr[0]

        def proj_fm(col0, t, bank=None):
            b = nb() if bank is None else bank
            for kc in range(8):
                P.op("pe", lambda e, kc=kc, b=b: e.matmul(ps[b], lhsT=win[:, kc, col0:col0 + 128], rhs=xn[:, kc, :],
                                                          start=(kc == 0), stop=(kc == 7)), [wB, xnB[kc]], [pb[b]])
            return b

        for t in range(NT):
            first = (t % TPS == 0)
            xt, xtB = xt2[t % 2], xt2B[t % 2]
            if t == 0:
                P.dma("sp", xt, Xv[:, :, 0:T], xtB, C.XB)
            if t + 1 < NT:
                P.dma("sp", xt2[(t + 1) % 2], Xv[:, :, (t + 1) * T:(t + 2) * T], xt2B[(t + 1) % 2], C.XB)
            rmsnorm_fm(C, xt, xtB, gain, xn, xnB, yT, yTB, T)
            if first:
                P.op("pool", lambda e: e.memset(Cst, 0.0), [], [CstB])
                P.op("pool", lambda e: e.memset(Cbf, 0.0), [], [CbfB])
            for c in range(8):
                if first:
                    P.op("pool", lambda e, c=c: e.memset(qk[:, c, 0:3], 0.0), [], [qkB[c]])
                else:
                    P.op("pool", lambda e, c=c: e.tensor_copy(out=qk[:, c, 0:3], in_=qk[:, c, T:T + 3]), [qkB[c]], [qkB[c]])
                b = proj_fm(1024 + c * 128, t)
                P.op("act", lambda e, c=c, b=b: e.activation(out=qk[:, c, 3:T + 3], in_=ps[b], func=AF.Copy), [pb[b]], [qkB[c]])
                P.op("dve", lambda e, c=c: e.tensor_scalar(out=qc, in0=qk[:, c, 0:T], scalar1=mlc[:, c, 0:1],
                                                          scalar2=mlc[:, c, 4:5], op0=ALU.mult, op1=ALU.add), [qkB[c], cB], [qcB])
                for j in range(1, 4):
                    P.op("dve", lambda e, c=c, j=j: e.scalar_tensor_tensor(
                        out=qc, in0=qk[:, c, j:j + T], scalar=mlc[:, c, j:j + 1], in1=qc, op0=ALU.mult, op1=ALU.add),
                        [qkB[c], cB, qcB], [qcB])
                if c < 4:
                    P.op("act", lambda e: e.activation(out=qc, in_=qc, func=AF.Silu), [qcB], [qcB])
                    P.op("dve", lambda e, c=c: e.tensor_scalar(out=qT[:, c, :], in0=qc, scalar1=QS, scalar2=None, op0=ALU.mult),
                         [qcB], [qTB[c]])
                else:
                    P.op("act", lambda e, c=c: e.activation(out=kT[:, c - 4, :], in_=qc, func=AF.Silu), [qcB], [kTB[c - 4]])
            for j in range(4):
                ts_ = slice(j * 128, (j + 1) * 128)
                b = nb()
                for kc in range(8):
                    P.op("pe", lambda e, kc=kc, b=b, ts_=ts_: e.matmul(ps[b], lhsT=xn[:, kc, ts_], rhs=win[:, kc, 2048:2560],
                                                                     start=(kc == 0), stop=(kc == 7)), [wB, xnB[kc]], [pb[b]])
                P.op("act", lambda e, j=j, b=b: e.activation(out=va[:, j, :, 0:128], in_=ps[b].rearrange("p (h v) -> p h v", h=4),
                                                             func=AF.Copy), [pb[b]], [vaB[j]])
                b = nb()
                for kc in range(8):
                    P.op("pe", lambda e, kc=kc, b=b, ts_=ts_: e.matmul(ps[b], lhsT=xn[:, kc, ts_], rhs=win[:, kc, 2560:3072],
                                                                     start=(kc == 0), stop=(kc == 7)), [wB, xnB[kc]], [pb[b]])
                P.op("act", lambda e, j=j, b=b: e.activation(out=og[:, j, :], in_=ps[b], func=AF.Sigmoid), [pb[b]], [ogB[j]])
                b = nb()
                for kc in range(8):
                    P.op("pe", lambda e, kc=kc, b=b, ts_=ts_: e.matmul(ps[b][:, 0:8], lhsT=xn[:, kc, ts_], rhs=win[:, kc, 3072:3080],
                                                                     start=(kc == 0), stop=(kc == 7)), [wB, xnB[kc]], [pb[b]])
                P.op("dve", lambda e, j=j, b=b: e.tensor_tensor(out=gt[:, j, :], in0=ps[b][:, 0:8], in1=mlrow[:, 0:8], op=ALU.add),
                     [pb[b], cB], [gtB[j]])
                P.op("act", lambda e, j=j: e.activation(out=gt[:, j, 4:8], in_=gt[:, j, 4:8], func=AF.Exp, scale=-1.0), [gtB[j]], [gtB[j]])
                P.op("act", lambda e, j=j: e.activation(out=gt[:, j, 4:8], in_=gt[:, j, 4:8], func=AF.Ln, bias=1.0), [gtB[j]], [gtB[j]])
                P.op("dve", lambda e, j=j: e.tensor_scalar(out=gt[:, j, 4:8], in0=gt[:, j, 4:8], scalar1=-1.0, scalar2=None, op0=ALU.mult),
                     [gtB[j]], [gtB[j]])
            def rg_stream():
                for c in range(4):
                    if first:
                        P.op("pool", lambda e, c=c: e.memset(xa[:, c, 0:3], 0.0), [], [xaB[c]])
                    else:
                        P.op("pool", lambda e, c=c: e.tensor_copy(out=xa[:, c, 0:3], in_=xa[:, c, T:T + 3]), [xaB[c]], [xaB[c]])
                    b = proj_fm(c * 128, t, 5)
                    P.op("act", lambda e, c=c, b=b: e.activation(out=xa[:, c, 3:T + 3], in_=ps[b], func=AF.Copy), [pb[b]], [xaB[c]])
                    P.op("dve", lambda e, c=c: e.tensor_scalar(out=xc, in0=xa[:, c, 0:T], scalar1=rgc[:, c, 0:1],
                                                              scalar2=rgc[:, c, 4:5], op0=ALU.mult, op1=ALU.add), [xaB[c], cB], [xcB])
                    for j in range(1, 4):
                        P.op("dve", lambda e, c=c, j=j: e.scalar_tensor_tensor(
                            out=xc, in0=xa[:, c, j:j + T], scalar=rgc[:, c, j:j + 1], in1=xc, op0=ALU.mult, op1=ALU.add),
                            [xaB[c], cB, xcB], [xcB])
                    P.op("act", lambda e: e.activation(out=xcb, in_=xc, func=AF.Copy), [xcB], [xcbB])
                    yield
                    br = 5
                    P.op("pe", lambda e, c=c, br=br: e.matmul(ps[br], lhsT=rgw[:, c, 0, :], rhs=xcb, start=True, stop=True),
                         [wB, xcbB], [pb[br]])
                    P.op("act", lambda e, c=c, br=br: e.activation(out=rr, in_=ps[br], func=AF.Sigmoid, bias=rgc[:, c, 5:6]),
                         [pb[br], cB], [rrB])
                    yield
                    bi = 5
                    P.op("pe", lambda e, c=c, bi=bi: e.matmul(ps[bi], lhsT=rgw[:, c, 1, :], rhs=xcb, start=True, stop=True),
                         [wB, xcbB], [pb[bi]])
                    P.op("act", lambda e, c=c, bi=bi: e.activation(out=ig, in_=ps[bi], func=AF.Sigmoid, bias=rgc[:, c, 6:7]),
                         [pb[bi], cB], [igB])
                    P.op("act", lambda e, c=c: e.activation(out=aa, in_=rr, func=AF.Exp, scale=cneg[:, c:c + 1]), [rrB, kB], [aaB])
                    yield
                    P.op("act", lambda e: e.activation(out=rr, in_=aa, func=AF.Square), [aaB], [rrB])
                    P.op("act", lambda e: e.activation(out=rr, in_=rr, func=AF.Sqrt, scale=-1.0, bias=onesf[:, 0:1]), [rrB, kB], [rrB])
                    P.op("dve", lambda e: e.tensor_tensor(out=uu, in0=ig, in1=xc, op=ALU.mult), [igB, xcB], [uuB])
                    P.op("dve", lambda e: e.tensor_tensor(out=uu, in0=uu, in1=rr, op=ALU.mult), [uuB, rrB], [uuB])
                    yield
                    if first:
                        P.op("dve", lambda e: e.tensor_tensor_scan(out=hs, data0=aa, data1=uu, initial=0.0, op0=ALU.mult, op1=ALU.add),
                             [aaB, uuB], [hsB])
                    else:
                        P.op("dve", lambda e, c=c: e.tensor_tensor_scan(out=hs, data0=aa, data1=uu, initial=hst[:, c:c + 1],
                                                                       op0=ALU.mult, op1=ALU.add), [aaB, uuB, hstB[c]], [hsB])
                    P.op("dve", lambda e, c=c: e.tensor_copy(out=hst[:, c:c + 1], in_=hs[:, T - 1:T]), [hsB], [hstB[c]])
                    yield
                    bg = proj_fm(512 + c * 128, t, 5)
                    P.op("act", lambda e, bg=bg: e.activation(out=gg, in_=ps[bg], func=AF.Gelu_apprx_tanh), [pb[bg]], [ggB])
                    P.op("dve", lambda e, c=c: e.tensor_tensor(out=yT[:, c, :], in0=gg, in1=hs, op=ALU.mult), [ggB, hsB], [yTB[c]])
                    yield
            def ml_stream():
                for j in range(4):
                    ts_ = slice(j * 128, (j + 1) * 128)
                    logi = gt[:, j, 0:4]
                    logf = gt[:, j, 4:8]
                    P.op("dve", lambda e, logf=logf: e.tensor_copy(out=lhi, in_=logf), [gtB[j]], [lB])
                    P.op("dve", lambda e: e.tensor_copy(out=lhf, in_=lhi), [lB], [lB])
                    P.op("dve", lambda e, logf=logf: e.tensor_tensor(out=lhf, in0=logf, in1=lhf, op=ALU.subtract), [gtB[j], lB], [lB])
                    P.op("dve", lambda e: e.tensor_copy(out=llo, in_=lhf), [lB], [lB])
                    P.op("dve", lambda e: e.tensor_copy(out=Lh, in_=lhi.unsqueeze(2).broadcast_to([128, 4, 128])), [lB], [LB])
                    P.op("dve", lambda e: e.tensor_copy(out=Ll, in_=llo.unsqueeze(2).broadcast_to([128, 4, 128])), [lB, LB], [LB])
                    P.op("pe", lambda e: e.matmul(ps[7][:, 264:268], lhsT=U, rhs=lhi, start=True, stop=False), [kB, lB], [pb[7]])
                    P.op("pe", lambda e: e.matmul(ps[7][:, 264:268], lhsT=U, rhs=llo, start=False, stop=True), [kB, lB], [pb[7]])
                    P.op("pe", lambda e: e.matmul(ps[7][:, 268:272], lhsT=C.ones_bf, rhs=lhi, start=True, stop=False), [kB, lB], [pb[7]])
                    P.op("pe", lambda e: e.matmul(ps[7][:, 268:272], lhsT=C.ones_bf, rhs=llo, start=False, stop=True), [kB, lB], [pb[7]])
                    for hd in range(4):
                        o_ = ps[0][:, hd * 128:(hd + 1) * 128]
                        P.op("pe", lambda e, hd=hd, o_=o_: e.matmul(o_, lhsT=Lh[:, hd, :], rhs=U, start=True, stop=False), [LB, kB], [pb[0]])
                        P.op("pe", lambda e, hd=hd, o_=o_: e.matmul(o_, lhsT=Ll[:, hd, :], rhs=U, start=False, stop=False), [LB, kB], [pb[0]])
                        P.op("pe", lambda e, hd=hd, o_=o_: e.matmul(o_, lhsT=ident, rhs=mneg, start=False, stop=True), [kB], [pb[0]])
                        P.op("pe", lambda e, hd=hd, ts_=ts_: e.matmul(ps[1][:, hd * 128:(hd + 1) * 128], lhsT=kT[:, hd, ts_], rhs=qT[:, hd, ts_],
                                                                     start=True, stop=True), [kTB[hd], qTB[hd]], [pb[1]])
                    yield
                    bcol = ps[7][:, 264:268]
                    gcol = ps[7][:, 268:272]
                    P.op("dve", lambda e, logi=logi, bcol=bcol: e.tensor_tensor(out=sm[:, 0:4], in0=logi, in1=bcol, op=ALU.subtract),
                         [gtB[j], pb[7]], [smB])
                    P.op("dve", lambda e, gcol=gcol: e.tensor_tensor(out=sm[:, 8:12], in0=sm[:, 0:4], in1=gcol, op=ALU.add),
                         [smB, pb[7]], [smB])
                    P.op("act", lambda e, bcol=bcol: e.activation(out=sm[:, 4:8], in_=bcol, func=AF.Exp), [pb[7], smB], [smB])
                    P.op("act", lambda e: e.activation(out=sm[:, 8:12], in_=sm[:, 8:12], func=AF.Exp), [smB], [smB])
                    P.op("act", lambda e, gcol=gcol: e.activation(out=sm[:, 12:16], in_=gcol, func=AF.Exp), [pb[7], smB], [smB])
                    yield
                    for hd in range(4):
                        P.op("act", lambda e, hd=hd: e.activation(out=DT[:, hd, :], in_=ps[0][:, hd * 128:(hd + 1) * 128], func=AF.Exp,
                                                                  bias=sm[:, hd:hd + 1]), [pb[0], smB], [DTB])
                    P.op("dve", lambda e: e.tensor_tensor(out=AT, in0=DT, in1=ps[1].rearrange("p (h k) -> p h k", h=4), op=ALU.mult),
                         [DTB, pb[1]], [ATB])
                    yield
                    for hd in range(4):
                        bi_ = 2 + hd // 2
                        cs = slice((hd % 2) * 129, (hd % 2) * 129 + 129)
                        P.op("pe", lambda e, hd=hd, bi_=bi_, cs=cs, ts_=ts_: e.matmul(ps[bi_][:, cs], lhsT=qT[:, hd, ts_], rhs=Cbf[:, hd, :],
                                                                                    start=True, stop=True), [qTB[hd], CbfB], [pb[bi_]])
                    for hp in range(2):
                        for hd in (2 * hp, 2 * hp + 1):
                            cs = slice((hd % 2) * 129, (hd % 2) * 129 + 129)
                            P.op("pe", lambda e, hd=hd, cs=cs, j=j: e.matmul(ps[4][:, cs], lhsT=AT[:, hd, :], rhs=va[:, j, hd, :],
                                                                           start=True, stop=True), [ATB, vaB[j]], [pb[4]])
                        P.op("act", lambda e, hp=hp: e.activation(out=itr[:, 2 * hp:2 * hp + 2, :],
                                                                  in_=ps[4][:, 0:258].rearrange("p (h v) -> p h v", h=2), func=AF.Copy),
                             [pb[4]], [itrB])
                    yield
                    for hd in range(4):
                        bi_ = 2 + hd // 2
                        cs = slice((hd % 2) * 129, (hd % 2) * 129 + 129)
                        P.op("dve", lambda e, hd=hd, bi_=bi_, cs=cs: e.scalar_tensor_tensor(
                            out=tot[:, hd, :], in0=ps[bi_][:, cs], scalar=sm[:, 4 + hd:5 + hd], in1=itr[:, hd, :], op0=ALU.mult, op1=ALU.add),
                            [pb[bi_], smB, itrB], [totB])
                    yield
                    P.op("dve", lambda e: e.tensor_scalar(out=sm[:, 16:20], in0=tot[:, :, 128], scalar1=-1.0, scalar2=1.0,
                                                          op0=ALU.mult, op1=ALU.max), [totB, smB], [smB])
                    P.op("dve", lambda e: e.tensor_tensor(out=sm[:, 16:20], in0=sm[:, 16:20], in1=tot[:, :, 128], op=ALU.max),
                         [totB, smB], [smB])
                    P.op("dve", lambda e: e.reciprocal(out=sm[:, 16:20], in_=sm[:, 16:20]), [smB], [smB])
                    P.op("dve", lambda e: e.tensor_tensor(out=hh, in0=tot[:, :, 0:128],
                                                          in1=sm[:, 16:20].unsqueeze(2).broadcast_to([128, 4, 128]), op=ALU.mult),
                         [totB, smB], [hhB])
                    P.op("dve", lambda e: e.tensor_tensor(out=h2, in0=hh, in1=hh, op=ALU.mult), [hhB], [h2B])
                    P.op("dve", lambda e: e.tensor_reduce(out=sm[:, 20:24], in_=h2, axis=AX.X, op=ALU.add), [h2B, smB], [smB])
                    P.op("act", lambda e: e.activation(out=sm[:, 20:24], in_=sm[:, 20:24], func=AF.Sqrt, scale=1.0 / 128, bias=C.eps_t[:, 0:1]),
                         [smB], [smB])
                    P.op("dve", lambda e: e.reciprocal(out=sm[:, 20:24], in_=sm[:, 20:24]), [smB], [smB])
                    P.op("dve", lambda e: e.tensor_tensor(out=hh, in0=hh, in1=sm[:, 20:24].unsqueeze(2).broadcast_to([128, 4, 128]),
                                                          op=ALU.mult), [hhB, smB], [hhB])
                    P.op("dve", lambda e: e.tensor_tensor(out=hh, in0=hh, in1=mlrow[:, 8:520].rearrange("p (h v) -> p h v", h=4),
                                                          op=ALU.mult), [hhB, cB], [hhB])
                    P.op("dve", lambda e, j=j: e.tensor_tensor(out=ytk.rearrange("p (h v) -> p h v", h=4), in0=hh,
                                                               in1=og[:, j, :].rearrange("p (h v) -> p h v", h=4), op=ALU.mult),
                         [hhB, ogB[j]], [ytkB])
                    yield
                    p6b = ps[6].bitcast(BF16)
                    for hd in range(4):
                        P.op("pe", lambda e, hd=hd, p6b=p6b: e.transpose(out=p6b[:, hd * 128:(hd + 1) * 128], in_=ytk[:, hd * 128:(hd + 1) * 128],
                                                                       identity=ident), [ytkB, kB], [pb[6]])
                    for hd in range(4):
                        P.op("act", lambda e, hd=hd, p6b=p6b, ts_=ts_: e.activation(out=yT[:, 4 + hd, ts_], in_=p6b[:, hd * 128:(hd + 1) * 128],
                                                                                  func=AF.Copy), [pb[6]], [yTB[4 + hd]])
                    yield
                    p7b = ps[7].bitcast(BF16)
                    for hd in range(4):
                        P.op("pe", lambda e, hd=hd, p7b=p7b, ts_=ts_: e.transpose(out=p7b[:, 512 + hd * 128:512 + (hd + 1) * 128],
                                                                                in_=kT[:, hd, ts_], identity=ident), [kTB[hd], kB], [pb[7]])
                    for hd in range(4):
                        P.op("dve", lambda e, hd=hd, p7b=p7b: e.tensor_scalar(out=kw[:, hd, :], in0=p7b[:, 512 + hd * 128:512 + (hd + 1) * 128],
                                                                            scalar1=sm[:, 8 + hd:9 + hd], scalar2=None, op0=ALU.mult),
                             [pb[7], smB], [kwB])
                    for hd in range(4):
                        bs_ = 2 + hd // 2
                        cs = slice((hd % 2) * 129, (hd % 2) * 129 + 129)
                        P.op("pe", lambda e, hd=hd, bs_=bs_, cs=cs, j=j: e.matmul(ps[bs_][:, cs], lhsT=kw[:, hd, :], rhs=va[:, j, hd, :],
                                                                                start=True, stop=True), [kwB, vaB[j]], [pb[bs_]])
                    for hd in range(4):
                        bs_ = 2 + hd // 2
                        cs = slice((hd % 2) * 129, (hd % 2) * 129 + 129)
                        P.op("dve", lambda e, hd=hd, bs_=bs_, cs=cs: e.scalar_tensor_tensor(
                            out=Cst[:, hd, :], in0=Cst[:, hd, :], scalar=sm[:, 12 + hd:13 + hd], in1=ps[bs_][:, cs], op0=ALU.mult, op1=ALU.add),
                            [CstB, smB, pb[bs_]], [CstB])
                    P.op("act", lambda e: e.activation(out=Cbf, in_=Cst, func=AF.Copy), [CstB], [CbfB])
                    yield
            interleave([rg_stream(), ml_stream()])
            for dc in range(8):
                b = nb()
                for yc in range(8):
                    P.op("pe", lambda e, yc=yc, dc=dc, b=b: e.matmul(ps[b], lhsT=wout[:, yc, dc * 128:(dc + 1) * 128], rhs=yT[:, yc, :],
                                                                   start=(yc == 0), stop=(yc == 7)), [wB, yTB[yc]], [pb[b]])
                P.op("dve", lambda e, dc=dc, b=b, xt=xt: e.tensor_tensor(out=xt[:, dc, :], in0=xt[:, dc, :], in1=ps[b], op=ALU.add),
                     [xtB, pb[b]], [xtB])
            P.dma("sp", Xv[:, :, t * T:(t + 1) * T], xt, C.XB, xtB, join=True)
        P.barrier()


def final_phase(C, gain):
    P, nc = C.P, C.nc
    T = 512
    with ExitStack() as es:
        xt = sb(es, nc, "fxt", [128, 8, T], F32); xtB = P.buf("fxt")
        sq = sb(es, nc, "fsq", [128, 8, T], BF16); sqB = P.bufs_n(8, "fsq")
        C.rs = sb(es, nc, "rs", [128, T], F32); C.rstd = sb(es, nc, "rstd", [128, T], F32)
        C.rsB, C.rstdB = P.buf("rs"), P.buf("rstd")
        Xv = C.X.rearrange("c p n -> p c n")
        Ov = C.out.rearrange("c p n -> p c n")
        for t in range(NTOK // T):
            P.dma("sp", xt, Xv[:, :, t * T:(t + 1) * T], xtB, C.XB)
            rmsnorm_fm(C, xt, xtB, gain, xt, xtB, sq, sqB, T)
            P.dma("sp", Ov[:, :, t * T:(t + 1) * T], xt, C.outB, xtB, join=True)
        P.barrier()


NCOLN = 2608


def interleave(gens):
    gens = [g for g in gens if g is not None]
    while gens:
        for g in list(gens):
            try:
                next(g)
            except StopIteration:
                gens.remove(g)


def nsa_proj_phase(C, gain, D_):
    P, nc, ps, pb = C.P, C.nc, C.ps, C.pb
    T = 512
    NT = NTOK // T
    TPS = S // T
    with ExitStack() as es:
        A = lambda name, shape, dt: sb(es, nc, name, shape, dt)
        win = A("nwin", [128, 8, NCOLN], BF16)
        wB = P.buf("nw")
        for kc in range(8):
            P.dma("pool", win[:, kc, :], D_["nsa_win"][:, kc, :], wB, None, join=True)
        bg = A("bg", [128, 48], F32)
        cB = P.buf("nc")
        P.dma("sp", bg, D_["bgrow"], cB, None)
        xt = [A("xt%d" % i, [128, 8, T], F32) for i in range(2)]; xtB = P.bufs_n(2, "xt")
        xn2 = [A("xn%d" % i, [128, 8, T], BF16) for i in range(2)]; xn2B = [P.bufs_n(8, "xn%d_" % i) for i in range(2)]
        sq2 = [A("sq%d" % i, [128, 8, T], BF16) for i in range(2)]; sq2B = [P.bufs_n(8, "sq%d_" % i) for i in range(2)]
        C.rs = A("rs", [128, T], F32); C.rstd = A("rstd", [128, T], F32)
        C.rsB, C.rstdB = P.buf("rs"), P.buf("rstd")
        stg = [A("stg%d" % i, [128, 16, T], BF16) for i in range(2)]; stgB = [P.bufs_n(5, "stg%d_" % i) for i in range(2)]
        vtk = [A("vtk%d" % i, [128, 4, 512], BF16) for i in range(2)]; vtkB = P.bufs_n(2, "vtk")
        gtk = [A("gtk%d" % i, [128, 4, 48], F32) for i in range(2)]; gtkB = P.bufs_n(2, "gtk")
        Xv = C.X.rearrange("c p n -> p c n")
        k = 0
        groups = [("Qs", 0, 8, 0.125, 0), ("KsT", 8, 2, 1.0, 1024), ("KwT", 10, 2, 1.0, 1280), ("KcT", 12, 2, 1.0, 1536),
                  ("VcT", 14, 2, 1.0, 1792)]
        P.dma("sp", xt[0], Xv[:, :, 0:T], xtB[0], C.XB)
        for t in range(NT):
            sq_, t0 = t // TPS, (t % TPS) * T
            pr = t % 2
            if t + 1 < NT:
                P.dma("sp", xt[(t + 1) % 2], Xv[:, :, (t + 1) * T:(t + 2) * T], xtB[(t + 1) % 2], C.XB)
            xn, xnB, sq, sqB = xn2[pr], xn2B[pr], sq2[pr], sq2B[pr]
            if t == 0:
                rmsnorm_fm(C, xt[pr], xtB[pr], gain, xn, xnB, sq, sqB, T)
            for gi, (nm, c0, ncnk, scl, cb) in enumerate(groups):
                if gi == 1 and t + 1 < NT:
                    pn = (t + 1) % 2
                    rmsnorm_fm(C, xt[pn], xtB[pn], gain, xn2[pn], xn2B[pn], sq2[pn], sq2B[pn], T)
                for cc in range(ncnk):
                    b = k % 6
                    k += 1
                    col0 = cb + cc * 128
                    for kc in range(8):
                        P.op("pe", lambda e, kc=kc, b=b, col0=col0, xn=xn: e.matmul(ps[b], lhsT=win[:, kc, col0:col0 + 128], rhs=xn[:, kc, :],
                                                                          start=(kc == 0), stop=(kc == 7)), [wB, xnB[kc]], [pb[b]])
                    if (c0 + cc) % 2 == 0:
                        P.op("act", lambda e, b=b, c=c0 + cc, scl=scl, pr=pr: e.activation(out=stg[pr][:, c, :], in_=ps[b], func=AF.Copy, scale=scl),
                             [pb[b]], [stgB[pr][gi]])
                    else:
                        P.op("dve", lambda e, b=b, c=c0 + cc, scl=scl, pr=pr: e.tensor_scalar(out=stg[pr][:, c, :], in0=ps[b], scalar1=scl,
                                                                                          scalar2=None, op0=ALU.mult), [pb[b]], [stgB[pr][gi]])
                if nm in ("Qs", "KsT", "KwT"):
                    nh = 2 * ncnk
                    P.dma("sp", D_[nm][sq_, :, 0:nh:2, t0:t0 + T], stg[pr][0:64, c0:c0 + ncnk, :], D_[nm + "B"], stgB[pr][gi], join=True)
                    P.dma("sp", D_[nm][sq_, :, 1:nh:2, t0:t0 + T], stg[pr][64:128, c0:c0 + ncnk, :], D_[nm + "B"], stgB[pr][gi], join=True)
                else:
                    P.dma("sp", D_[nm][sq_, :, :, t0:t0 + T], stg[pr][:, c0:c0 + ncnk, :], D_[nm + "B"], stgB[pr][gi], join=True)
            for j in range(4):
                ts_ = slice(j * 128, (j + 1) * 128)
                b = k % 6
                k += 1
                for kc in range(8):
                    P.op("pe", lambda e, kc=kc, b=b, ts_=ts_, xn=xn: e.matmul(ps[b], lhsT=xn[:, kc, ts_], rhs=win[:, kc, 2048:2560],
                                                                     start=(kc == 0), stop=(kc == 7)), [wB, xnB[kc]], [pb[b]])
                P.op("act", lambda e, b=b, j=j, pr=pr: e.activation(out=vtk[pr][:, j, :], in_=ps[b], func=AF.Copy), [pb[b]], [vtkB[pr]])
                b = k % 6
                k += 1
                for kc in range(8):
                    P.op("pe", lambda e, kc=kc, b=b, ts_=ts_, xn=xn: e.matmul(ps[b][:, 0:48], lhsT=xn[:, kc, ts_], rhs=win[:, kc, 2560:2608],
                                                                     start=(kc == 0), stop=(kc == 7)), [wB, xnB[kc]], [pb[b]])
                P.op("dve", lambda e, b=b, j=j, pr=pr: e.tensor_tensor(out=gtk[pr][:, j, :], in0=ps[b][:, 0:48], in1=bg, op=ALU.add),
                     [pb[b], cB], [gtkB[pr]])
            P.op("act", lambda e, pr=pr: e.activation(out=gtk[pr], in_=gtk[pr], func=AF.Sigmoid), [gtkB[pr]], [gtkB[pr]])
            P.dma("sp", D_["Vs"][sq_, t0:t0 + T, :].rearrange("(j p) c -> p j c", p=128), vtk[pr][:, :, 0:256], D_["VsB"], vtkB[pr], join=True)
            P.dma("sp", D_["Vw"][sq_, t0:t0 + T, :].rearrange("(j p) c -> p j c", p=128), vtk[pr][:, :, 256:512], D_["VwB"], vtkB[pr], join=True)
            P.dma("sp", D_["G"][sq_, t0:t0 + T, :].rearrange("(j p) c -> p j c", p=128), gtk[pr], D_["GB"], gtkB[pr], join=True)
        P.barrier()


def nsa_core_phase(C, D_):
    P, nc, ps, pb = C.P, C.nc, C.ps, C.pb
    NQ = S // 128
    NEG = -30000.0
    with ExitStack() as es:
        A = lambda name, shape, dt: sb(es, nc, name, shape, dt)
        wout = A("nwout", [128, 8, D], BF16)
        w1 = A("w1", [128, 2, 32, 256], BF16)
        w2k = A("w2k", [128, 2, 64], BF16)
        w2v = A("w2v", [128, 2, 64], BF16)
        peT = A("peT", [128, 2, 32], BF16)
        b1 = A("b1", [128, 2, 2], F32)
        selA = A("selA", [128, 16, 32], F32)
        selB = A("selB", [128, 16, 32], F32)
        wB = P.buf("n2w")
        P.dma("pool", wout, D_["nsa_wout"], wB, None, join=True)
        P.dma("pool", w1, D_["w1d"], wB, None, join=True)
        P.dma("pool", w2k, D_["w2kd"], wB, None, join=True)
        P.dma("pool", w2v, D_["w2vd"], wB, None, join=True)
        P.dma("pool", peT, D_["peTd"], wB, None, join=True)
        cB = P.buf("n2c")
        P.dma("sp", b1, D_["b1d"], cB, None, join=True)
        P.dma("sp", selA, D_["selA"], cB, None, join=True)
        P.dma("sp", selB, D_["selB"], cB, None, join=True)
        kB = P.buf("n2k")
        onesf = A("onesf", [128, 2048], F32)
        zerof = A("zerof", [128, 2048], F32)
        ident = A("ident", [128, 128], BF16)
        mneg = A("mneg", [128, 128], BF16)
        mneg2 = A("mneg2", [128, 128], BF16)
        cmask = A("cmask", [128, 16, 128], BF16)
        E = A("E", [32, 16, 128], BF16)
        E0 = A("E0", [32, 16, 128], F32)
        P.op("pool", lambda e: e.memset(onesf, 1.0), [], [kB])
        P.op("pool", lambda e: e.memset(zerof, 0.0), [kB], [kB])
        P.op("pool", lambda e: e.affine_select(out=ident, in_=onesf[:, 0:128], pattern=[[1, 128]], compare_op=ALU.is_equal, fill=0.0,
                                              base=0, channel_multiplier=-1), [kB], [kB])
        P.op("pool", lambda e: e.affine_select(out=mneg, in_=zerof[:, 0:128], pattern=[[1, 128]], compare_op=ALU.is_ge, fill=NEG,
                                              base=0, channel_multiplier=-1), [kB], [kB])
        P.op("pool", lambda e: e.affine_select(out=mneg2, in_=zerof[:, 0:128], pattern=[[-1, 128]], compare_op=ALU.is_ge, fill=NEG,
                                              base=-1, channel_multiplier=1), [kB], [kB])
        for i in range(16):
            P.op("pool", lambda e, i=i: e.affine_select(out=cmask[:, i, :], in_=zerof[:, 0:128], pattern=[[1, 128]], compare_op=ALU.is_ge,
                                                       fill=NEG, base=128 * i - 31, channel_multiplier=-16), [kB], [kB])
        P.op("pool", lambda e: e.affine_select(out=E0, in_=onesf[0:32, :].rearrange("p (a b) -> p a b", a=16), pattern=[[128, 16], [1, 128]],
                                              compare_op=ALU.is_ge, fill=0.0, base=0, channel_multiplier=-64), [kB], [kB])
        P.op("pool", lambda e: e.affine_select(out=E, in_=E0, pattern=[[-128, 16], [-1, 128]], compare_op=ALU.is_ge, fill=0.0,
                                              base=63, channel_multiplier=64), [kB], [kB])

        sqB_ = P.buf("seqdata")
        ksT = A("ksT", [128, 4, S], BF16)
        kwT = A("kwT", [128, 4, S], BF16)
        kcT = A("kcT", [128, 2, S], BF16)
        vcT = A("vcT", [128, 2, S], BF16)
        vsa = A("vsa", [128, 16, 4, 65], BF16)
        vwa = A("vwa", [128, 16, 4, 65], BF16)
        gts = A("gts", [128, 16, 48], F32)
        hT = A("hT", [128, 2, 128], BF16); hTB = P.buf("hT")
        btot = A("btot", [128, 2, 2], F32); btB = P.buf("btot")
        kcmp = A("kcmp", [128, 4, 128], BF16); kcmpB = P.buf("kcmp")
        P.op("dve", lambda e: e.memset(kcmp, 0.0), [], [kcmpB])
        vcmp = A("vcmp", [128, 4, 97], BF16); vcmpB = P.buf("vcmp")
        qt = [A("qt%d" % i, [128, 16, 128], BF16) for i in range(2)]; qtB = P.bufs_n(2, "qt")
        selT = [A("selT%d" % i, [128, 4, 128], BF16) for i in range(2)]; selTB = P.bufs_n(2, "selT")
        E128 = A("E128", [128, 16, 128], BF16)
        P.op("dve", lambda e: e.memset(E128, 0.0), [], [kB])
        for i_ in range(2):
            P.op("dve", lambda e, i_=i_: e.memset(qt[i_], 0.0), [], [qtB[i_]])
            P.op("dve", lambda e, i_=i_: e.memset(selT[i_], 0.0), [], [selTB[i_]])
        P.op("dve", lambda e: e.tensor_copy(out=E128[0:32], in_=E), [kB], [kB])
        P.op("dve", lambda e: e.memset(ksT, 0.0), [], [sqB_])
        P.op("dve", lambda e: e.memset(kwT, 0.0), [sqB_], [sqB_])
        ocs = [A("ocs%d" % i, [128, 4, 4, 64], F32) for i in range(2)]; ocsB = P.bufs_n(2, "ocs")
        xt2 = [A("xt2_%d" % i, [128, 8, 128], F32) for i in range(2)]; xt2B = P.bufs_n(2, "xt2")
        pT = [A("pT%d" % i, [128, 4, 128], BF16) for i in range(2)]; pTB = P.bufs_n(2, "pT")
        pTa = A("pTa", [128, 4, 128], BF16); pTaB = P.buf("pTa")
        sc4 = A("sc4", [128, 4, 32], F32); sc4B = P.buf("sc4")
        scr = A("scr", [128, 32], F32); scrB = P.buf("scr")
        top8 = A("top8", [128, 8], F32)
        seln = A("seln", [128, 32], BF16); selnB = P.buf("seln")
        zza = A("zza", [128, 4], F32); zzaB = P.buf("zza")
        zz = A("zz", [128, 2, 4], F32); zzB = P.buf("zz")
        otk = A("otk", [128, 1024], F32); otkB = P.buf("otk")
        tmp = A("tmpo", [128, 4, 64], F32); tmpB = P.buf("tmpo")
        tmp2 = A("tmpo2", [128, 4, 64], F32); tmp2B = P.buf("tmpo2")
        otb = A("otb", [128, 1024], BF16); otbB = P.buf("otb")
        oT = A("oT", [128, 8, 128], BF16); oTB = P.buf("oT")
        P.op("dve", lambda e: e.memset(vsa, 1.0), [], [sqB_])
        P.op("dve", lambda e: e.memset(vwa, 1.0), [sqB_], [sqB_])
        P.op("dve", lambda e: e.memset(vcmp, 1.0), [], [vcmpB])
        Xv = C.X.rearrange("c p n -> p c n")
        p6b = ps[6].bitcast(BF16)
        p7b = ps[7].bitcast(BF16)

        def stage_a(sq_, i):
            p = i % 2
            P.dma("sp", qt[p][0:64], D_["Qs"][sq_, :, :, i * 128:(i + 1) * 128], qtB[p], D_["QsB"])
            for g in range(4):
                P.op("pe", lambda e, g=g, p=p: e.matmul(ps[4][0:127, :], lhsT=kcmp[:, g, 0:127], rhs=qt[p][:, 4 * g:4 * g + 4, :],
                                                       start=True, stop=False), [kcmpB, qtB[p]], [pb[4]])
                P.op("pe", lambda e, i=i: e.matmul(ps[4][0:127, :], lhsT=ident[0:127, 0:127],
                                                   rhs=cmask[0:127, i, :].unsqueeze(1).broadcast_to([127, 4, 128]), start=False, stop=True),
                     [kB], [pb[4]])
                P.op("act", lambda e: e.activation(out=pTa[0:127], in_=ps[4][0:127, :].rearrange("p (r q) -> p r q", r=4), func=AF.Exp),
                     [pb[4]], [pTaB])
                yield
                for r in range(4):
                    P.op("pe", lambda e, r=r, g=g: e.matmul(ps[5][:, r * 97:(r + 1) * 97], lhsT=pTa[0:127, r, :], rhs=vcmp[0:127, g, :],
                                                           start=(r == 0), stop=(r == 3), skip_group_check=True), [pTaB, vcmpB], [pb[5]])
                yield
                c4 = ps[5][:, 0:388].rearrange("p (r c) -> p r c", r=4)
                P.op("dve", lambda e, c4=c4: e.tensor_scalar(out=zza, in0=c4[:, :, 96], scalar1=1e-30, scalar2=None, op0=ALU.max), [pb[5]], [zzaB])
                P.op("dve", lambda e: e.reciprocal(out=zza, in_=zza), [zzaB], [zzaB])
                P.op("dve", lambda e, c4=c4: e.tensor_tensor(out=sc4, in0=c4[:, :, 0:32], in1=zza.unsqueeze(2).broadcast_to([128, 4, 32]),
                                                           op=ALU.mult), [pb[5], zzaB], [sc4B])
                P.op("dve", lambda e: e.tensor_reduce(out=scr, in_=sc4.rearrange("p r j -> p j r"), axis=AX.X, op=ALU.add), [sc4B], [scrB])
                yield
                P.op("dve", lambda e, i=i: e.tensor_tensor(out=scr, in0=scr, in1=selA[:, i, :], op=ALU.mult), [scrB, cB], [scrB])
                P.op("dve", lambda e, i=i: e.tensor_tensor(out=scr, in0=scr, in1=selB[:, i, :], op=ALU.add), [scrB, cB], [scrB])
                P.op("dve", lambda e: e.max(out=top8, in_=scr), [scrB], [scrB])
                P.op("dve", lambda e: e.tensor_scalar(out=seln, in0=scr, scalar1=top8[:, 7:8], scalar2=1.0, op0=ALU.is_ge, op1=ALU.subtract),
                     [scrB], [selnB])
                yield
                P.op("pe", lambda e: e.transpose(out=p7b[0:32, 0:128], in_=seln, identity=ident), [selnB, kB], [pb[7]])
                gv = gts[:, i, g * 12:(g + 1) * 12].rearrange("p (r b) -> p b r", b=3)
                P.op("dve", lambda e, gv=gv: e.tensor_tensor(out=zza, in0=zza, in1=gv[:, 0, :], op=ALU.mult), [zzaB, sqB_], [zzaB])
                P.op("dve", lambda e, c4=c4, g=g, p=p: e.tensor_tensor(out=ocs[p][:, g], in0=c4[:, :, 32:96],
                                                                     in1=zza.unsqueeze(2).broadcast_to([128, 4, 64]), op=ALU.mult),
                     [pb[5], zzaB], [ocsB[p]])
                P.op("act", lambda e, g=g, p=p: e.activation(out=selT[p][0:32, g, :], in_=p7b[0:32, 0:128], func=AF.Copy, scale=-NEG),
                     [pb[7]], [selTB[p]])
                yield

        def stage_b(sq_, i):
            p = i % 2
            vcount = [0]
            for g in range(4):
                q4 = qt[p][:, 4 * g:4 * g + 4, :]
                visits = [("sel", kt) for kt in range(i + 1)] + [("win", kt) for kt in (i - 2, i - 1, i) if kt >= 0]
                firstwin = min(kt for br, kt in visits if br == "win")

                def issue_scores(br, kt):
                    pi = vcount[0] % 2
                    vcount[0] += 1
                    ks_ = slice(kt * 128, (kt + 1) * 128)
                    kk = ksT if br == "sel" else kwT
                    extra = []
                    if br == "sel":
                        extra.append((E128[:, kt, :], selT[p][:, g, :], selTB[p]))
                    if kt == i:
                        extra.append((ident, mneg, kB))
                    if br == "win" and kt == i - 2:
                        extra.append((ident, mneg2, kB))
                    P.op("pe", lambda e, pi=pi, kk=kk, ks_=ks_, g=g, q4=q4, ne=len(extra): e.matmul(
                        ps[pi], lhsT=kk[:, g, ks_], rhs=q4, start=True, stop=(ne == 0)), [sqB_, qtB[p]], [pb[pi]])
                    for ei, (ml, mr, mB) in enumerate(extra):
                        P.op("pe", lambda e, pi=pi, ml=ml, mr=mr, last=(ei == len(extra) - 1): e.matmul(
                            ps[pi], lhsT=ml, rhs=mr.unsqueeze(1).broadcast_to([mr.shape[0], 4, 128]), start=False, stop=last), [kB, mB], [pb[pi]])
                    P.op("act", lambda e, pi=pi: e.activation(out=pT[pi], in_=ps[pi].rearrange("p (r q) -> p r q", r=4), func=AF.Exp),
                         [pb[pi]], [pTB[pi]])
                    return pi

                def issue_pv(br, kt, pi):
                    ob = 2 if br == "sel" else 3
                    va_ = vsa if br == "sel" else vwa
                    for r in range(4):
                        first = (r == 0 and ((br == "sel" and kt == 0) or (br == "win" and kt == firstwin)))
                        last = (r == 3 and kt == i)
                        P.op("pe", lambda e, r=r, pi=pi, ob=ob, va_=va_, kt=kt, first=first, last=last, g=g: e.matmul(
                            ps[ob][:, r * 65:(r + 1) * 65], lhsT=pT[pi][:, r, :], rhs=va_[:, kt, g, :], start=first, stop=last,
                            skip_group_check=True), [pTB[pi], sqB_], [pb[ob]])

                prev = None
                for (br, kt) in visits:
                    pi = issue_scores(br, kt)
                    if prev is not None:
                        issue_pv(*prev)
                    prev = (br, kt, pi)
                    yield
                issue_pv(*prev)
                yield
                gv = gts[:, i, g * 12:(g + 1) * 12].rearrange("p (r b) -> p b r", b=3)
                c2 = ps[2][:, 0:260].rearrange("p (r c) -> p r c", r=4)
                c3 = ps[3][:, 0:260].rearrange("p (r c) -> p r c", r=4)
                og_ = otk[:, g * 256:(g + 1) * 256].rearrange("p (r d) -> p r d", r=4)
                for bi_, cc, bk in ((0, c2, 2), (1, c3, 3)):
                    P.op("dve", lambda e, bi_=bi_, cc=cc: e.reciprocal(out=zz[:, bi_, :], in_=cc[:, :, 64]), [pb[bk], zzB], [zzB])
                    P.op("dve", lambda e, bi_=bi_, gv=gv: e.tensor_tensor(out=zz[:, bi_, :], in0=zz[:, bi_, :], in1=gv[:, bi_ + 1, :], op=ALU.mult),
                         [zzB, sqB_], [zzB])
                P.op("dve", lambda e, c2=c2: e.tensor_tensor(out=tmp, in0=c2[:, :, 0:64], in1=zz[:, 0, :].unsqueeze(2).broadcast_to([128, 4, 64]),
                                                           op=ALU.mult), [pb[2], zzB], [tmpB])
                P.op("dve", lambda e, c3=c3: e.tensor_tensor(out=tmp2, in0=c3[:, :, 0:64], in1=zz[:, 1, :].unsqueeze(2).broadcast_to([128, 4, 64]),
                                                            op=ALU.mult), [pb[3], zzB], [tmp2B])
                me_ = "dve"
                P.op(me_, lambda e, g=g, p=p: e.tensor_tensor(out=tmp, in0=tmp, in1=ocs[p][:, g], op=ALU.add), [tmpB, ocsB[p]], [tmpB])
                P.op(me_, lambda e, og_=og_: e.tensor_tensor(out=og_, in0=tmp, in1=tmp2, op=ALU.add), [tmpB, tmp2B], [otkB])
                yield

        def stage_c(sq_, i):
            p = i % 2
            xt, xtB = xt2[p], xt2B[p]
            tok0 = sq_ * S + i * 128
            P.dma("sp", xt, Xv[:, :, tok0:tok0 + 128], xtB, C.XB)
            P.op("act", lambda e: e.activation(out=otb, in_=otk, func=AF.Copy), [otkB], [otbB])
            yield
            for yc in range(8):
                P.op("pe", lambda e, yc=yc: e.transpose(out=p6b[:, yc * 128:(yc + 1) * 128], in_=otb[:, yc * 128:(yc + 1) * 128], identity=ident),
                     [otbB, kB], [pb[6]])
            P.op("act", lambda e: e.activation(out=oT, in_=p6b[:, 0:1024].rearrange("p (c q) -> p c q", c=8), func=AF.Copy), [pb[6]], [oTB])
            yield
            for dh in range(2):
                b = 6
                for dc in range(4):
                    d_ = dh * 4 + dc
                    for yc in range(8):
                        P.op("pe", lambda e, yc=yc, d_=d_, dc=dc, b=b: e.matmul(ps[b][:, dc * 128:(dc + 1) * 128],
                                                                             lhsT=wout[:, yc, d_ * 128:(d_ + 1) * 128], rhs=oT[:, yc, :],
                                                                             start=(yc == 0 and dc == 0), stop=(yc == 7 and dc == 3),
                                                                             skip_group_check=True), [wB, oTB], [pb[b]])
                    yield
                P.op("dve", lambda e, dh=dh, b=b, xt=xt: e.tensor_tensor(out=xt[:, dh * 4:(dh + 1) * 4, :], in0=xt[:, dh * 4:(dh + 1) * 4, :],
                                                                        in1=ps[b].rearrange("p (c q) -> p c q", c=4), op=ALU.add), [xtB, pb[b]], [xtB])
                yield
            P.dma("sp", Xv[:, :, tok0:tok0 + 128], xt, C.XB, xtB, join=True)
            yield

        for sq_ in range(NSEQ):
            P.dma("sp", ksT[0:64], D_["KsT"][sq_], sqB_, D_["KsTB"], join=True)
            P.dma("sp", kwT[0:64], D_["KwT"][sq_], sqB_, D_["KwTB"], join=True)
            P.dma("sp", kcT, D_["KcT"][sq_], sqB_, D_["KcTB"], join=True)
            P.dma("sp", vcT, D_["VcT"][sq_], sqB_, D_["VcTB"], join=True)
            for g in range(4):
                P.dma("sp", vsa[:, :, g, 0:64], D_["Vs"][sq_, :, g * 64:(g + 1) * 64].rearrange("(k p) d -> p k d", p=128), sqB_, D_["VsB"], join=True)
                P.dma("sp", vwa[:, :, g, 0:64], D_["Vw"][sq_, :, g * 64:(g + 1) * 64].rearrange("(k p) d -> p k d", p=128), sqB_, D_["VwB"], join=True)
            P.dma("sp", gts, D_["G"][sq_].rearrange("(k p) c -> p k c", p=128), sqB_, D_["GB"], join=True)
            if sq_ == 0:
                for kv in range(2):
                    for hc in range(2):
                        for l in range(32):
                            P.op("pe", lambda e, kv=kv, hc=hc, l=l: e.matmul(ps[7][:, 2 * kv + hc:2 * kv + hc + 1],
                                                                           lhsT=w1[0:64, kv, l, hc * 128:(hc + 1) * 128], rhs=peT[0:64, kv, l:l + 1],
                                                                           start=(l == 0), stop=(l == 31)), [wB], [pb[7]])
                P.op("dve", lambda e: e.tensor_tensor(out=btot, in0=b1, in1=ps[7][:, 0:4].rearrange("p (a b) -> p a b", a=2), op=ALU.add),
                     [pb[7], cB], [btB])
                for g in range(4):
                    P.dma("pool", vcmp[:, g, 0:32], D_["mseld"], vcmpB, None, join=True)
            kk_ = 0
            for kv, src in enumerate([kcT, vcT]):
                for g in range(4):
                    r0 = (g % 2) * 64
                    ch = g // 2
                    for hc in range(2):
                        b = kk_ % 4
                        kk_ += 1
                        for l in range(32):
                            P.op("pe", lambda e, kv=kv, hc=hc, l=l, r0=r0, ch=ch, src=src, b=b: e.matmul(
                                ps[b][:, 0:127], lhsT=w1[r0:r0 + 64, kv, l, hc * 128:(hc + 1) * 128],
                                rhs=src[r0:r0 + 64, ch, l:l + 16 * 126 + 1:16], start=(l == 0), stop=(l == 31)), [wB, sqB_], [pb[b]])
                        P.op("act", lambda e, kv=kv, hc=hc, b=b: e.activation(out=hT[:, hc, 0:127], in_=ps[b][:, 0:127], func=AF.Gelu_apprx_tanh,
                                                                           bias=btot[:, kv, hc:hc + 1]), [pb[b], btB], [hTB])
                    if kv == 0:
                        for hc in range(2):
                            P.op("pe", lambda e, hc=hc: e.matmul(ps[7][0:64, 0:127], lhsT=w2k[:, hc, :], rhs=hT[:, hc, 0:127],
                                                                 start=(hc == 0), stop=(hc == 1)), [wB, hTB], [pb[7]])
                        P.op("act", lambda e, g=g: e.activation(out=kcmp[0:64, g, 0:127], in_=ps[7][0:64, 0:127], func=AF.Copy), [pb[7]], [kcmpB])
                    else:
                        for hc in range(2):
                            P.op("pe", lambda e, hc=hc: e.matmul(ps[7][0:127, 0:64], lhsT=hT[:, hc, 0:127], rhs=w2v[:, hc, :],
                                                                 start=(hc == 0), stop=(hc == 1)), [wB, hTB], [pb[7]])
                        P.op("act", lambda e, g=g: e.activation(out=vcmp[0:127, g, 32:96], in_=ps[7][0:127, 0:64], func=AF.Copy), [pb[7]], [vcmpB])
            dbg = int(os.environ.get("NSADBG", "9"))
            if dbg >= 1:
                interleave([stage_a(sq_, 0)])
            for i in range(NQ):
                if dbg == 1:
                    interleave([stage_a(sq_, i + 1) if i + 1 < NQ else None])
                elif dbg == 2:
                    interleave([stage_b(sq_, i)])
                    interleave([stage_a(sq_, i + 1) if i + 1 < NQ else None])
                elif dbg >= 3:
                    interleave([stage_c(sq_, i - 1) if i >= 1 else None, stage_b(sq_, i), stage_a(sq_, i + 1) if i + 1 < NQ else None])
            if dbg >= 3:
                interleave([stage_c(sq_, NQ - 1)])
        P.barrier()


def perm_gu(w):
    wg = w[:, :DFF].reshape(8, 128, NF, 128)
    wu = w[:, DFF:].reshape(8, 128, NF, 128)
    o = np.stack([wg, wu], axis=3)
    o = o.transpose(2, 1, 0, 3, 4)
    return np.ascontiguousarray(o.reshape(NF, 128, 8 * 256))


def perm_vec(v):
    return np.ascontiguousarray(v.reshape(8, 128).T)


def build(upto="all"):
    nc = bass.Bass("TRN2", target_bir_lowering=False)
    P = Prog(nc)
    C = Ctx()
    C.P, C.nc = P, nc
    dram_in = lambda n, shp: nc.dram_tensor(n, list(shp), F32, kind="ExternalInput").ap()
    xin = dram_in("xin", [8, 128, NTOK])
    nrm_d = dram_in("nrm", [128, 56])
    wgu_d = [dram_in("wgu%d" % i, [NF, 128, 2048]) for i in range(4)]
    wd_d = [dram_in("wd%d" % i, [NF, 128, D]) for i in range(4)]
    C.out = nc.dram_tensor("out", [8, 128, NTOK], F32, kind="ExternalOutput").ap()
    C.outB = P.buf("out", keep=True)
    C.X = nc.dram_tensor("X", [8, 128, NTOK], F32, kind="Internal").ap()
    C.XB = P.buf("X", keep=True)

    with ExitStack() as es0:
        C.ps = [es0.enter_context(nc.psum_tensor("ps%d" % i, [128, 512], F32)).ap() for i in range(8)]
        C.pb = [P.buf("ps%d" % i, keep=True) for i in range(8)]
        C.constB = P.buf("const", keep=True)
        C.ones_bf = sb(es0, nc, "ones_bf", [128, 128], BF16)
        C.eps_t = sb(es0, nc, "eps_t", [128, 1], F32)
        C.nrm = sb(es0, nc, "nrm_sb", [128, 56], F32)
        P.op("dve", lambda e: e.memset(C.ones_bf, 1.0), [], [C.constB])
        P.op("dve", lambda e: e.memset(C.eps_t, EPS), [C.constB], [C.constB])
        nrmB = P.buf("nrmld")
        P.dma("sp", C.nrm, nrm_d, nrmB, None)
        P.barrier()
        C.constB.w = None
        C.constB.rd = []
        g = lambda i: C.nrm[:, i * 8:(i + 1) * 8]
        D_ = {}
        for n, shp in [("ab_win", [128, 8, 3080]), ("ab_wout", [128, 8, D]), ("rgw", [128, 4, 2, 128]), ("rgc", [128, 4, 8]),
                       ("mlc", [128, 8, 5]), ("mlrow", [128, 520])]:
            D_[n] = dram_in(n, shp)
        for n, shp in [("nsa_win", [128, 8, NCOLN]), ("nsa_wout", [128, 8, D]), ("w1d", [128, 2, 32, 256]), ("w2kd", [128, 2, 64]),
                       ("w2vd", [128, 2, 64]), ("peTd", [128, 2, 32]), ("b1d", [128, 2, 2]), ("mseld", [128, 32]),
                       ("selA", [128, 16, 32]), ("selB", [128, 16, 32]), ("bgrow", [128, 48])]:
            D_[n] = dram_in(n, shp)
        for n, shp, dt in [("Qs", [NSEQ, 64, 16, S], BF16), ("KsT", [NSEQ, 64, 4, S], BF16), ("KwT", [NSEQ, 64, 4, S], BF16),
                           ("KcT", [NSEQ, 128, 2, S], BF16), ("VcT", [NSEQ, 128, 2, S], BF16), ("Vs", [NSEQ, S, 256], BF16),
                           ("Vw", [NSEQ, S, 256], BF16), ("G", [NSEQ, S, 48], F32)]:
            D_[n] = nc.dram_tensor("scr_" + n, shp, dt, kind="Internal").ap()
            D_[n + "B"] = P.buf("scr_" + n, keep=True)
        stages = ["ffn1", "mix0", "ffn2", "ffn1b", "mix1", "all"]
        if upto in ("n1only", "n2only", "abonly"):
            if upto == "abonly":
                ab_phase(C, g(1), D_)
            else:
                nsa_proj_phase(C, g(4), D_)
                if upto == "n2only":
                    nsa_core_phase(C, D_)
            final_phase(C, g(6))
            P.emit([C.outB])
            return nc, P
        ns = stages.index(upto) + 1
        ffn_phase(C, es0, [(g(0), wgu_d[0], wd_d[0], (g(6) if ns == 1 else None), xin)])
        if ns >= 2:
            ab_phase(C, g(1), D_)
        if ns >= 3:
            ffn_phase(C, es0, [(g(2), wgu_d[1], wd_d[1], None, C.X)])
        if ns >= 4:
            ffn_phase(C, es0, [(g(3), wgu_d[2], wd_d[2], None, C.X)])
        if ns >= 5:
            nsa_proj_phase(C, g(4), D_)
            nsa_core_phase(C, D_)
        if ns >= 6:
            ffn_phase(C, es0, [(g(5), wgu_d[3], wd_d[3], g(6), C.X)])
        elif ns >= 2:
            final_phase(C, g(6))
        P.emit([C.outB])
    return nc, P


_CACHE = {}


def kernel(upto="all", **inp):
    x = np.asarray(inp["x"], np.float32)
    B = x.shape[0]
    ncores = B // NSEQ
    if upto not in _CACHE:
        _CACHE[upto] = build(upto)
    nc, P = _CACHE[upto]
    nrm = np.concatenate([perm_vec(inp["ffn1_norm"][0]), perm_vec(inp["mix_norm"][0]), perm_vec(inp["ffn2_norm"][0]),
                          perm_vec(inp["ffn1_norm"][1]), perm_vec(inp["mix_norm"][1]), perm_vec(inp["ffn2_norm"][1]),
                          perm_vec(inp["final_norm"])], axis=1).astype(np.float32)
    shared = {"nrm": np.ascontiguousarray(nrm)}
    ffns = [("ffn1", 0), ("ffn2", 0), ("ffn1", 1), ("ffn2", 1)]
    for i, (nm, l) in enumerate(ffns):
        shared["wgu%d" % i] = perm_gu(np.asarray(inp[nm + "_w_gu"][l], np.float32))
        shared["wd%d" % i] = np.ascontiguousarray(np.asarray(inp[nm + "_w_down"][l], np.float32).reshape(NF, 128, D))
    f32 = lambda a: np.ascontiguousarray(np.asarray(a, np.float32))
    shared["ab_win"] = f32(inp["ab_w_in"][0].reshape(8, 128, 3080).transpose(1, 0, 2))
    shared["ab_wout"] = f32(inp["ab_w_out"][0].reshape(8, 128, D).transpose(1, 0, 2))
    rgw = np.zeros((128, 4, 2, 128), np.float32)
    for c in range(4):
        for k, nm in enumerate(["rg_w_r", "rg_w_i"]):
            rgw[0:64, c, k, 0:64] = inp[nm][0][2 * c]
            rgw[64:128, c, k, 64:128] = inp[nm][0][2 * c + 1]
    shared["rgw"] = rgw
    pc = lambda v, n: np.asarray(v, np.float32).reshape(n, 128).T
    rgc = np.stack([pc(inp["rg_conv_w"][0][j], 4) for j in range(4)] + [pc(inp["rg_conv_b"][0], 4), pc(inp["rg_b_r"][0], 4),
                   pc(inp["rg_b_i"][0], 4), pc(inp["rg_lambda"][0], 4)], axis=2)
    shared["rgc"] = f32(rgc)
    mlc = np.stack([pc(inp["ml_conv_w"][0][j], 8) for j in range(4)] + [pc(inp["ml_conv_b"][0], 8)], axis=2)
    shared["mlc"] = f32(mlc)
    row = np.concatenate([inp["ml_b_i"][0], inp["ml_b_f"][0], inp["ml_norm"][0]]).astype(np.float32)
    shared["mlrow"] = f32(np.tile(row[None, :], (128, 1)))
    W = np.asarray(inp["nsa_w_in"][0], np.float32)
    kcW, vcW, ksW, vsW, kwW, vwW = [W[:, 1024 + 256 * i_:1280 + 256 * i_] for i_ in range(6)]
    Wp = np.concatenate([W[:, 0:1024], ksW, kwW, kcW, vcW, vsW, vwW, W[:, 2560:2608]], axis=1)
    assert Wp.shape == (1024, NCOLN)
    shared["nsa_win"] = f32(Wp.reshape(8, 128, NCOLN).transpose(1, 0, 2))
    shared["nsa_wout"] = f32(inp["nsa_w_out"][0].reshape(8, 128, D).transpose(1, 0, 2))
    w1d = np.zeros((128, 2, 32, 256), np.float32)
    peTd = np.zeros((128, 2, 32), np.float32)
    b1d = np.zeros((128, 2, 2), np.float32)
    for kv_, (w1n, pen, b1n) in enumerate([("nsa_k_w1", "nsa_pe_k", "nsa_k_b1"), ("nsa_v_w1", "nsa_pe_v", "nsa_v_b1")]):
        w1_ = np.asarray(inp[w1n][0], np.float32).reshape(32, 64, 256).transpose(1, 0, 2)
        w1d[0:64, kv_] = w1_
        w1d[64:128, kv_] = w1_
        pe_ = np.asarray(inp[pen][0], np.float32).T
        peTd[0:64, kv_] = pe_
        peTd[64:128, kv_] = pe_
        b1d[:, kv_, :] = np.asarray(inp[b1n][0], np.float32).reshape(2, 128).T
    shared["w1d"], shared["peTd"], shared["b1d"] = w1d, peTd, b1d
    w2k_ = np.asarray(inp["nsa_k_w2"][0], np.float32).reshape(2, 128, 64).transpose(1, 0, 2)
    shared["w2kd"] = f32(w2k_)
    shared["w2vd"] = f32(np.asarray(inp["nsa_v_w2"][0], np.float32).reshape(2, 128, 64).transpose(1, 0, 2))
    ncmp = (S - 32) // 16 + 1
    cs_ = np.arange(ncmp)[:, None] * 16
    ss_ = np.arange(32)[None, :] * 64
    ov = np.clip(np.minimum(cs_ + 32, ss_ + 64) - np.maximum(cs_, ss_), 0, None) / 32.0
    msel = np.zeros((128, 32), np.float32)
    msel[:ncmp] = ov
    shared["mseld"] = msel
    tpos = (np.arange(16)[None, :] * 128 + np.arange(128)[:, None])[:, :, None]
    jj = np.arange(32)[None, None, :]
    cur = tpos // 64
    valid = (jj * 64 <= tpos)
    forced = ((jj == 0) | (jj == cur) | (jj == cur - 1)) & valid
    shared["selA"] = f32((valid & ~forced).astype(np.float32))
    shared["selB"] = f32(1e6 * forced.astype(np.float32) - (1.0 - valid.astype(np.float32)))
    shared["bgrow"] = f32(np.tile(np.asarray(inp["nsa_b_gate"][0], np.float32)[None, :], (128, 1)))
    in_maps = []
    for c in range(ncores):
        xs = x[c * NSEQ:(c + 1) * NSEQ].reshape(NTOK, 8, 128).transpose(1, 2, 0)
        m = dict(shared)
        m["xin"] = np.ascontiguousarray(xs)
        in_maps.append(m)
    res = run_bass_kernel_spmd(nc, in_maps, core_ids=list(range(ncores)))
    outs = []
    for c in range(ncores):
        o = res.results[c]["out"]
        outs.append(o.transpose(2, 0, 1).reshape(NSEQ, S, D))
    return np.ascontiguousarray(np.concatenate(outs, axis=0))
```
